# Optimizing a Trainium2 kernel written in Bass

```python
import jax, jax.numpy as jnp
from jax import lax
import numpy as np

D_MODEL = 4096
BATCH = 4
SEQ = 4096
DEPTH = 1
DEC_BATCH = 4
DEC_SEQ = 2048
PAST_LEN = 128

N_HEADS = 16
N_KV_HEADS = 4
HEAD_DIM = 128
Q_BLOCK = 128
ROPE_THETA = 10000.0
GRID_W = 64
FOURIER_CH = 2048
ATTN_W = N_HEADS * HEAD_DIM
KV_W = N_KV_HEADS * HEAD_DIM
IN_W = ATTN_W + 2 * KV_W + FOURIER_CH
N_BRANCH = 2
MEM_LEN = 256
CA_HEADS = 4
CA_HEAD_DIM = 256
CA_W = CA_HEADS * CA_HEAD_DIM
N_KEYS = 128
N_EXPERTS = N_KEYS * N_KEYS
PEER_HEADS = 8
PEER_TOPK = 16
PEER_QDIM = 256
PEER_HALF = PEER_QDIM // 2
PEER_TOK_BLOCK = 32
EPS = 1e-6

kernel_name = 'hybrid_fourier_gqa_peer_encoder'

F32 = jnp.float32


def rmsnorm(x, g):
    xf = x.astype(F32)
    y = xf * lax.rsqrt(jnp.mean(xf * xf, axis=-1, keepdims=True) + EPS)
    return (y * g.astype(F32)).astype(x.dtype)


def axial_rope_tables(seq_len):
    rows = seq_len // GRID_W
    row = jnp.repeat(jnp.arange(rows), GRID_W)
    col = jnp.tile(jnp.arange(GRID_W), rows)
    half = HEAD_DIM // 2
    inv_freq = ROPE_THETA ** (-jnp.arange(0, half, 2, dtype=F32) / half)
    ang = jnp.stack([row, col], axis=-1).astype(F32)[:, :, None] * inv_freq
    return jnp.cos(ang), jnp.sin(ang)


def apply_axial_rope(x, cos, sin):
    B, S, H, _ = x.shape
    xf = x.astype(F32).reshape(B, S, H, 2, 2, HEAD_DIM // 4)
    x1, x2 = xf[..., 0, :], xf[..., 1, :]
    c = cos[None, :, None]
    s = sin[None, :, None]
    out = jnp.stack([x1 * c - x2 * s, x2 * c + x1 * s], axis=-2)
    return out.reshape(B, S, H, HEAD_DIM).astype(x.dtype)


def gqa_attention(q, k, v):
    B, S, _, _ = q.shape
    G = N_HEADS // N_KV_HEADS
    nblk = S // Q_BLOCK
    qb = q.reshape(B, nblk, Q_BLOCK, N_KV_HEADS, G, HEAD_DIM).transpose(1, 0, 2, 3, 4, 5)
    scale = HEAD_DIM ** -0.5

    def one_block(qi):
        s = jnp.einsum('bqkgd,bskd->bkgqs', qi, k, preferred_element_type=F32) * scale
        p = jax.nn.softmax(s, axis=-1).astype(v.dtype)
        return jnp.einsum('bkgqs,bskd->bqkgd', p, v)

    o = lax.map(one_block, qb)
    return o.transpose(1, 0, 2, 3, 4, 5).reshape(B, S, ATTN_W)


def fourier_mix(u):
    z = jnp.fft.fft2(u.astype(F32), axes=(1, 2), norm='ortho')
    return jnp.real(z).astype(u.dtype)


def memory_cross_attention(h, mem, mem_norm, w_cq, w_ckv, w_co):
    B, S, _ = h.shape
    M = mem.shape[1]
    m = rmsnorm(mem, mem_norm)
    q = (h @ w_cq).reshape(B, S, CA_HEADS, CA_HEAD_DIM)
    kv = (m @ w_ckv).reshape(B, M, 2, CA_HEADS, CA_HEAD_DIM)
    k, v = kv[:, :, 0], kv[:, :, 1]
    s = jnp.einsum('bqhd,bmhd->bhqm', q, k, preferred_element_type=F32) * (CA_HEAD_DIM ** -0.5)
    p = jax.nn.softmax(s, axis=-1).astype(v.dtype)
    o = jnp.einsum('bhqm,bmhd->bqhd', p, v).reshape(B, S, CA_W)
    return o @ w_co


def peer(h, w_pq, sub_keys, expert_u, expert_v):
    B, S, D = h.shape
    q = (h @ w_pq).reshape(B, S, PEER_HEADS, 2, PEER_HALF).astype(F32)
    sk = jnp.einsum('bshcd,cnd->bshcn', q, sub_keys.astype(F32))
    s_top, i_top = lax.top_k(sk, PEER_TOPK)
    cand_s = (s_top[..., 0, :, None] + s_top[..., 1, None, :]).reshape(B, S, PEER_HEADS, PEER_TOPK * PEER_TOPK)
    cand_i = (i_top[..., 0, :, None] * N_KEYS + i_top[..., 1, None, :]).reshape(B, S, PEER_HEADS, PEER_TOPK * PEER_TOPK)
    best_s, best_pos = lax.top_k(cand_s, PEER_TOPK)
    ids = jnp.take_along_axis(cand_i, best_pos, axis=-1)
    gates = jax.nn.softmax(best_s, axis=-1)
    T = B * S
    nb = T // PEER_TOK_BLOCK
    E = PEER_HEADS * PEER_TOPK
    hb = h.reshape(nb, PEER_TOK_BLOCK, D)
    ib = ids.reshape(nb, PEER_TOK_BLOCK, E)
    gb = gates.reshape(nb, PEER_TOK_BLOCK, E).astype(h.dtype)

    def block(args):
        hx, ix, gx = args
        u = expert_u[ix]
        a = jax.nn.gelu(jnp.einsum('td,ted->te', hx, u), approximate=False)
        v = expert_v[ix]
        return jnp.einsum('te,ted->td', gx * a, v)

    out = lax.map(block, (hb, ib, gb))
    return out.reshape(B, S, D)


def encoder_layer(x, mem, norm_mix, w_in, q_norm, k_norm, w_attn_br, w_four_br, w_gate, b_gate, w_out,
                  norm_ca, mem_norm, w_cq, w_ckv, w_co, norm_ffn, w_pq, sub_keys, expert_u, expert_v):
    B, S, D = x.shape
    h = rmsnorm(x, norm_mix)
    proj = h @ w_in
    q, k, v, f = jnp.split(proj, [ATTN_W, ATTN_W + KV_W, ATTN_W + 2 * KV_W], axis=-1)
    q = rmsnorm(q.reshape(B, S, N_HEADS, HEAD_DIM), q_norm)
    k = rmsnorm(k.reshape(B, S, N_KV_HEADS, HEAD_DIM), k_norm)
    v = v.reshape(B, S, N_KV_HEADS, HEAD_DIM)
    cos, sin = axial_rope_tables(S)
    q = apply_axial_rope(q, cos, sin)
    k = apply_axial_rope(k, cos, sin)
    attn_br = gqa_attention(q, k, v) @ w_attn_br
    four_br = fourier_mix(f) @ w_four_br
    g = jax.nn.sigmoid((h @ w_gate + b_gate).astype(F32)).astype(x.dtype).reshape(B, S, N_BRANCH, D)
    merged = g[:, :, 0] * attn_br + g[:, :, 1] * four_br
    x = x + merged @ w_out
    x = x + memory_cross_attention(rmsnorm(x, norm_ca), mem, mem_norm, w_cq, w_ckv, w_co)
    x = x + peer(rmsnorm(x, norm_ffn), w_pq, sub_keys, expert_u, expert_v)
    return x


def trunk(x, mem, layer_params, final_norm):
    (norm_mix, w_in, q_norm, k_norm, w_attn_br, w_four_br, w_gate, b_gate, w_out,
     norm_ca, mem_norm, w_cq, w_ckv, w_co, norm_ffn, w_pq, sub_keys, expert_u, expert_v) = layer_params
    for l in range(DEPTH):
        x = encoder_layer(x, mem, norm_mix[l], w_in[l], q_norm[l], k_norm[l], w_attn_br[l], w_four_br[l],
                          w_gate[l], b_gate[l], w_out[l], norm_ca[l], mem_norm[l], w_cq[l], w_ckv[l], w_co[l],
                          norm_ffn[l], w_pq[l], sub_keys[l], expert_u[l], expert_v[l])
    return rmsnorm(x, final_norm)


def setup_inputs(seed: int = 0) -> dict:
    key = jax.random.key(seed)
    ks = jax.random.split(key, 26)
    L, D = DEPTH, D_MODEL

    def nrm(k, shape, scale):
        return jax.random.normal(k, shape, F32) * scale

    def gain(k, shape):
        return 1.0 + 0.02 * jax.random.normal(k, shape, F32)

    return {
        'x_prompt': nrm(ks[0], (BATCH, SEQ, D), 1.0),
        'x_sample': nrm(ks[1], (DEC_BATCH, DEC_SEQ, D), 1.0),
        'mem_prompt': nrm(ks[2], (BATCH, MEM_LEN, D), 1.0),
        'mem_sample': nrm(ks[3], (DEC_BATCH, MEM_LEN, D), 1.0),
        'norm_mix': gain(ks[4], (L, D)),
        'w_in': nrm(ks[5], (L, D, IN_W), D ** -0.5),
        'q_norm': gain(ks[6], (L, HEAD_DIM)),
        'k_norm': gain(ks[7], (L, HEAD_DIM)),
        'w_attn_br': nrm(ks[8], (L, ATTN_W, D), ATTN_W ** -0.5),
        'w_four_br': nrm(ks[9], (L, FOURIER_CH, D), FOURIER_CH ** -0.5),
        'w_gate': nrm(ks[10], (L, D, N_BRANCH * D), D ** -0.5),
        'b_gate': nrm(ks[11], (L, N_BRANCH * D), 0.02),
        'w_out': nrm(ks[12], (L, D, D), D ** -0.5),
        'norm_ca': gain(ks[13], (L, D)),
        'mem_norm': gain(ks[14], (L, D)),
        'w_cq': nrm(ks[15], (L, D, CA_W), D ** -0.5),
        'w_ckv': nrm(ks[16], (L, D, 2 * CA_W), D ** -0.5),
        'w_co': nrm(ks[17], (L, CA_W, D), CA_W ** -0.5),
        'norm_ffn': gain(ks[18], (L, D)),
        'w_pq': nrm(ks[19], (L, D, PEER_HEADS * PEER_QDIM), D ** -0.5),
        'sub_keys': nrm(ks[20], (L, 2, N_KEYS, PEER_HALF), PEER_HALF ** -0.5),
        'expert_u': nrm(ks[21], (L, N_EXPERTS, D), D ** -0.5),
        'expert_v': nrm(ks[22], (L, N_EXPERTS, D), PEER_HEADS ** -0.5),
        'final_norm': gain(ks[23], (D,)),
    }


def reference(x_prompt, x_sample, mem_prompt, mem_sample, norm_mix, w_in, q_norm, k_norm, w_attn_br,
              w_four_br, w_gate, b_gate, w_out, norm_ca, mem_norm, w_cq, w_ckv, w_co, norm_ffn, w_pq,
              sub_keys, expert_u, expert_v, final_norm):
    layer_params = (norm_mix, w_in, q_norm, k_norm, w_attn_br, w_four_br, w_gate, b_gate, w_out,
                    norm_ca, mem_norm, w_cq, w_ckv, w_co, norm_ffn, w_pq, sub_keys, expert_u, expert_v)
    y_prompt = trunk(x_prompt, mem_prompt, layer_params, final_norm)
    y_sample = trunk(x_sample, mem_sample, layer_params, final_norm)
    return (y_prompt, y_sample)
```

```python
import numpy as np
import ml_dtypes
from contextlib import ExitStack
import concourse.bass as bass
import concourse.mybir as mybir
from concourse.bass_utils import run_bass_kernel_spmd

F32 = mybir.dt.float32
BF16 = mybir.dt.bfloat16
AF = mybir.ActivationFunctionType
ALU = mybir.AluOpType
AX = mybir.AxisListType
BF = ml_dtypes.bfloat16

D = 4096
EPS = 1e-6
NPO = 6
NPA = 12
TOWN = 3072
TALL = 6144
ATT_SCALE = 128 ** -0.5
CA_SCALE = 256 ** -0.5


class Obj:
    _n = 0

    def __init__(self, name=""):
        Obj._n += 1
        self.id = Obj._n
        self.name = name
        self.w = {}
        self.r = {}


class Buf:
    def __init__(self, nc, st, name, shape, dtype):
        self.t = st.enter_context(nc.sbuf_tensor(name, list(shape), dtype))
        self.o = Obj(name)


class Ring:
    def __init__(self, items):
        self.items = items
        self.i = 0

    def next(self):
        it = self.items[self.i % len(self.items)]
        self.i += 1
        return it


class Sched:
    ENG = ("pe", "act", "dve", "pool", "sp")
    BLK = {"pe": "tensor", "act": "scalar", "dve": "vector", "pool": "gpsimd", "sp": "sync"}

    def __init__(self, nc, semstack):
        self.nc = nc
        self.semstack = semstack
        self.sems = {}
        self.ops = {e: [] for e in self.ENG}
        self.cnt = {}
        self.known = {e: {} for e in self.ENG}
        self.unsig = {e: False for e in self.ENG}
        self.nops = 0
        self.bufphys = {}
        self.free_phys = []
        self.nphys = 0

    def _collect(self, reads, writes, mykey, dma):
        d = {}
        for o in reads:
            for k, v in o.w.items():
                if v > d.get(k, 0):
                    d[k] = v
        for o in writes:
            for k, v in o.w.items():
                if dma and k == mykey:
                    continue
                if v > d.get(k, 0):
                    d[k] = v
            for k, v in o.r.items():
                if v > d.get(k, 0):
                    d[k] = v
        return d

    def _emit_waits(self, eng, deps, skipkey=None):
        kn = self.known[eng]
        lst = self.ops[eng]
        for k, v in deps.items():
            if k == skipkey:
                continue
            if kn.get(k, 0) >= v:
                continue
            lst.append((0, k, v))
            kn[k] = v

    def _record(self, reads, writes, key, c, merge=False):
        for o in reads:
            if o.r.get(key, 0) < c:
                o.r[key] = c
        for o in writes:
            if merge and key in o.w:
                o.w[key] = c
            else:
                o.w = {key: c}
            o.r = {}

    def op(self, eng, name, kw, reads=(), writes=(), signal=True, nosame=False):
        key = ("e", eng)
        deps = self._collect(reads, writes, key, False)
        self._emit_waits(eng, deps, skipkey=key if (eng == "pe" or nosame) else None)
        c = self.cnt.get(key, 0) + 1
        if signal:
            self.cnt[key] = c
            self.ops[eng].append((1, name, kw, key))
            self.unsig[eng] = False
        else:
            self.ops[eng].append((1, name, kw, None))
            self.unsig[eng] = True
        self._record(reads, writes, key, c)
        self.nops += 1

    def dma(self, q, out_ap, in_ap, semobj, reads=(), writes=()):
        oid = getattr(semobj, "o", semobj).id
        if oid not in self.bufphys:
            if self.free_phys:
                self.bufphys[oid] = self.free_phys.pop()
            else:
                self.bufphys[oid] = self.nphys
                self.nphys += 1
        key = ("b", self.bufphys[oid])
        deps = self._collect(reads, writes, key, True)
        self._emit_waits(q, deps)
        c = self.cnt.get(key, 0) + 16
        self.cnt[key] = c
        self.ops[q].append((2, out_ap, in_ap, key))
        self._record(reads, writes, key, c, merge=True)
        self.nops += 1

    def barrier(self):
        for e in self.ENG:
            assert not self.unsig[e], e
            self._emit_waits(e, dict(self.cnt))

    def flush(self, st):
        nc = self.nc
        for k in self.cnt:
            if k not in self.sems:
                self.sems[k] = self.semstack.enter_context(nc.semaphore("s%d" % len(self.sems)))
        sems = self.sems
        block = st.enter_context(nc.Block())
        for e in self.ENG:
            lst = self.ops[e]
            if not lst:
                continue

            def body(engine, lst=lst):
                for it in lst:
                    if it[0] == 0:
                        engine.wait_ge(sems[it[1]], it[2])
                    elif it[0] == 1:
                        ins = getattr(engine, it[1])(**it[2])
                        if it[3] is not None:
                            ins.then_inc(sems[it[3]], 1)
                    else:
                        engine.dma_start(out=it[1], in_=it[2]).then_inc(sems[it[3]], 16)

            getattr(block, self.BLK[e])(body)
        self.ops = {e: [] for e in self.ENG}
        self.free_phys = list(range(self.nphys))
        self.bufphys = {}


def build(stop_after=99, debug_out=()):
    nc = bass.Bass("TRN2", target_bir_lowering=False)

    def inp(name, shape, dt=F32):
        return nc.dram_tensor(name, list(shape), dt, kind="ExternalInput").ap()

    def scr(name, shape, dt=BF16):
        kind = "ExternalOutput" if name in debug_out else "Internal"
        return nc.dram_tensor(name, list(shape), dt, kind=kind).ap()

    xT = inp("xT", [NPA, 128, 32, 512])
    memT = inp("memT", [128, 32, 512])
    w_in = inp("w_in", [10, 128, 32, 512])
    w_attn = inp("w_attn", [16, 128, 16, 256])
    w_four = inp("w_four", [16, 128, 16, 256])
    w_gate = inp("w_gate", [32, 128, 32, 256])
    w_out = inp("w_out", [8, 128, 32, 512])
    w_cq = inp("w_cq", [2, 128, 32, 512])
    w_ckv = inp("w_ckv", [4, 128, 32, 512])
    w_co = inp("w_co", [8, 128, 8, 512])
    w_pq = inp("w_pq", [4, 128, 32, 512])
    uT = inp("uT", [32, 128, 32, 512])
    ev = inp("ev", [8, 4, 128, 32, 512])
    g_mix = inp("g_mix", [128, 32])
    g_ca = inp("g_ca", [128, 32])
    g_mem = inp("g_mem", [128, 32])
    g_ffn = inp("g_ffn", [128, 32])
    g_fin = inp("g_fin", [128, 32])
    qn = inp("qn", [128, 1])
    kn = inp("kn", [128, 1])
    bg = inp("bg", [128, 64])
    subkT = inp("subkT", [128, 2, 128])
    cosT = inp("cosT", [128, TALL])
    sinT = inp("sinT", [128, TALL])
    rmat = inp("rmat", [128, 128], BF16)
    ones_d = inp("ones", [128, 128], BF16)
    ident = inp("ident", [128, 128], BF16)
    ccs = inp("ccs", [8, 128, 16, 512], BF16)
    csp = inp("csp", [2, 4, 128, 32, 512], BF16)
    css = inp("css", [2, 128, 32, 512], BF16)
    yT = nc.dram_tensor("yT", [NPO, 128, 32, 512], F32, kind="ExternalOutput").ap()

    HT = scr("HT", [NPA, 128, 32, 512])
    QT = scr("QT", [16, 128, TOWN])
    KT = scr("KT", [4, 128, TALL])
    VS = scr("VS", [TALL, 512])
    FT = scr("FT", [NPA, 128, 16, 512])
    PQ = scr("PQ", [4, 2, TALL, 512])
    ZT = scr("ZT", [NPO, 128, 16, 512])
    AT = scr("AT", [NPO, 128, 16, 512])
    MT = scr("MT", [NPO, 128, 32, 512])
    X1T = scr("X1T", [NPO, 128, 32, 512], F32)
    HCT = scr("HCT", [NPO, 128, 32, 512])
    MNT = scr("MNT", [1, 128, 32, 512])
    QCT = scr("QCT", [8, 128, TOWN])
    KCT = scr("KCT", [8, 128, 512])
    VC = scr("VC", [512, 1024])
    OCT = scr("OCT", [NPO, 128, 8, 512])
    X2T = scr("X2T", [NPO, 128, 32, 512], F32)
    HPT = scr("HPT", [NPO, 128, 32, 512])
    QPT = scr("QPT", [16, 128, TOWN])
    WTp = scr("WTp", [NPO, 4, 128, 32, 512])
    GAT = scr("GAT", [NPO, 4, 128, 32, 512])
    X3T = scr("X3T", [NPO, 128, 32, 512], F32)

    outer = ExitStack()
    S = Sched(nc, outer)
    cnt = [0]

    def mkbuf(st, shape, dt, name="b"):
        cnt[0] += 1
        return Buf(nc, st, "%s%d" % (name, cnt[0]), shape, dt)

    def mkps(st, n, dt=F32, cols=512):
        out = []
        for i in range(n):
            cnt[0] += 1
            t = st.enter_context(nc.psum_tensor("ps%d" % cnt[0], [128, cols], dt))
            out.append((t, Obj("ps")))
        return out

    def mm(ps, po, pairs, reads, start=True, stop=True):
        n = len(pairs)
        for i, (l, r) in enumerate(pairs):
            last = i == n - 1
            S.op("pe", "matmul", dict(out=ps, lhsT=l, rhs=r, start=(start and i == 0), stop=(stop and last)),
                 reads=reads, writes=[po], signal=last)

    def load_w(buf, wpanel, kc):
        step = 8 if kc >= 8 else kc
        for c0 in range(0, kc, step):
            S.dma("pool", buf.t[:, c0:c0 + step, :], wpanel[:, c0:c0 + step, :], buf, writes=[buf.o])

    def consts(st, need_ones=True):
        ON = mkbuf(st, [128, 128], BF16, "ones")
        S.dma("sp", ON.t[:], ones_d, ON, writes=[ON.o])
        EP = mkbuf(st, [128, 1], F32, "eps")
        S.op("dve", "memset", dict(ap=EP.t[:], constant=EPS), writes=[EP.o])
        return ON, EP

    phase_idx = [0]

    def run_phase(fn):
        phase_idx[0] += 1
        if phase_idx[0] > stop_after:
            return
        with ExitStack() as st:
            fn(st)
            S.barrier()
            S.flush(st)

    def norm_phase(srcs, gain_ap, dsts, out_dt):
        def fn(st):
            ON, EP = consts(st)
            Xr = Ring([mkbuf(st, [128, 32, 256], F32, "nx") for _ in range(2)])
            SQr = Ring([mkbuf(st, [128, 32, 256], BF16, "nsq") for _ in range(2)])
            Or = Ring([mkbuf(st, [128, 32, 256], out_dt, "no") for _ in range(2)])
            RSr = Ring([mkbuf(st, [128, 256], F32, "nrs") for _ in range(2)])
            G = mkbuf(st, [128, 32], F32, "ng")
            PSr = Ring(mkps(st, 2))
            S.dma("sp", G.t[:], gain_ap, G, writes=[G.o])
            for src, dst in zip(srcs, dsts):
                for hf in range(2):
                    cs = slice(hf * 256, (hf + 1) * 256)
                    X, SQ, O, RS = Xr.next(), SQr.next(), Or.next(), RSr.next()
                    ps, po = PSr.next()
                    S.dma("sp", X.t[:], src[:, :, cs], X, writes=[X.o])
                    for q2 in range(2):
                        S.op("act", "activation", dict(out=SQ.t[:, q2 * 16:(q2 + 1) * 16, :], in_=X.t[:, q2 * 16:(q2 + 1) * 16, :],
                                                       func=AF.Square), reads=[X.o], writes=[SQ.o])
                    mm(ps[:, 0:256], po, [(ON.t[:], SQ.t[:, c, :]) for c in range(32)], reads=[ON.o, SQ.o])
                    S.op("act", "activation", dict(out=RS.t[:], in_=ps[:, 0:256], func=AF.Sqrt, bias=EP.t[:, 0:1], scale=1.0 / D),
                         reads=[po, EP.o], writes=[RS.o])
                    S.op("dve", "reciprocal", dict(out=RS.t[:], in_=RS.t[:]), reads=[RS.o], writes=[RS.o])
                    for q4 in range(4):
                        c8 = slice(q4 * 8, (q4 + 1) * 8)
                        S.op("pool", "tensor_tensor",
                             dict(out=X.t[:, c8, :], in0=X.t[:, c8, :],
                                  in1=G.t[:, c8].unsqueeze(2).broadcast_to([128, 8, 256]), op=ALU.mult),
                             reads=[X.o, G.o], writes=[X.o], nosame=(q4 > 0))
                    for q4 in range(4):
                        c8 = slice(q4 * 8, (q4 + 1) * 8)
                        S.op("dve", "tensor_tensor",
                             dict(out=O.t[:, c8, :], in0=X.t[:, c8, :],
                                  in1=RS.t[:].unsqueeze(1).broadcast_to([128, 8, 256]), op=ALU.mult),
                             reads=[X.o, RS.o], writes=[O.o], nosame=(q4 > 0))
                    S.dma("act", dst[:, :, cs], O.t[:], O, reads=[O.o])
        return fn

    def p2(st):
        ON, EP = consts(st)
        A = [mkbuf(st, [128, 32, 512], BF16, "A") for _ in range(2)]
        HB = Ring([mkbuf(st, [128, 32, 512], BF16, "HB") for _ in range(2)])
        CS = Ring([mkbuf(st, [128, 2, 512], F32, "CS") for _ in range(2)])
        RM = mkbuf(st, [128, 128], BF16, "RM")
        QN = mkbuf(st, [128, 1], F32, "QN")
        KN = mkbuf(st, [128, 1], F32, "KN")
        S.dma("sp", RM.t[:], rmat, RM, writes=[RM.o])
        S.dma("sp", QN.t[:], qn, QN, writes=[QN.o])
        S.dma("sp", KN.t[:], kn, KN, writes=[KN.o])
        EV = Ring([mkbuf(st, [128, 512], BF16, "EV") for _ in range(3)])
        XQ = Ring([mkbuf(st, [128, 512], F32, "XQ") for _ in range(4)])
        SQ = Ring([mkbuf(st, [128, 512], BF16, "SQ") for _ in range(4)])
        RS = Ring([mkbuf(st, [128, 512], F32, "RS") for _ in range(2)])
        XN = Ring([mkbuf(st, [128, 512], BF16, "XN") for _ in range(4)])
        T1 = Ring([mkbuf(st, [128, 512], F32, "T1") for _ in range(2)])
        T2 = Ring([mkbuf(st, [128, 512], F32, "T2") for _ in range(2)])
        OB = Ring([mkbuf(st, [128, 512], BF16, "OB") for _ in range(3)])
        pss = mkps(st, 8)
        PSM = Ring(pss[0:4])
        PS2 = Ring(pss[4:6])
        PS3 = Ring(pss[6:8])

        def stage_a(job):
            ps, po = job["ps"], job["po"]
            xq, sq = XQ.next(), SQ.next()
            S.op("act", "activation", dict(out=xq.t[:], in_=ps[:], func=AF.Copy), reads=[po], writes=[xq.o])
            S.op("act", "activation", dict(out=sq.t[:], in_=ps[:], func=AF.Square), reads=[po], writes=[sq.o])
            job["xq"], job["sq"] = xq, sq

        def stage_b(job):
            xq, sq, gain = job["xq"], job["sq"], job["gain"]
            ps2, po2 = PS2.next()
            mm(ps2[:], po2, [(ON.t[:], sq.t[:])], reads=[ON.o, sq.o])
            rs, xn = RS.next(), XN.next()
            S.op("act", "activation", dict(out=rs.t[:], in_=ps2[:], func=AF.Sqrt, bias=EP.t[:, 0:1], scale=1.0 / 128),
                 reads=[po2, EP.o], writes=[rs.o])
            S.op("dve", "reciprocal", dict(out=rs.t[:], in_=rs.t[:]), reads=[rs.o], writes=[rs.o])
            S.op("dve", "scalar_tensor_tensor", dict(out=xn.t[:], in0=xq.t[:], scalar=gain.t[:, 0:1], in1=rs.t[:],
                                                     op0=ALU.mult, op1=ALU.mult),
                 reads=[xq.o, gain.o, rs.o], writes=[xn.o])
            job["xn"] = xn

        def stage_c(job):
            xn, CSb, dst = job["xn"], job["cs"], job["dst"]
            ps3, po3 = PS3.next()
            mm(ps3[:], po3, [(RM.t[:], xn.t[:])], reads=[RM.o, xn.o])
            t1, t2, ob = T1.next(), T2.next(), OB.next()
            S.op("dve", "tensor_tensor", dict(out=t1.t[:], in0=xn.t[:], in1=CSb.t[:, 0, :], op=ALU.mult),
                 reads=[xn.o, CSb.o], writes=[t1.o])
            S.op("dve", "tensor_tensor", dict(out=t2.t[:], in0=ps3[:], in1=CSb.t[:, 1, :], op=ALU.mult),
                 reads=[po3, CSb.o], writes=[t2.o])
            S.op("pool", "tensor_tensor", dict(out=ob.t[:], in0=t1.t[:], in1=t2.t[:], op=ALU.add),
                 reads=[t1.o, t2.o], writes=[ob.o])
            S.dma("pool", dst, ob.t[:], ob, reads=[ob.o])

        pend = []

        def advance(newjob):
            for job in list(pend):
                job["age"] += 1
                if job["age"] == 1:
                    stage_b(job)
                elif job["age"] == 2:
                    stage_c(job)
                    pend.remove(job)
            if newjob is not None:
                stage_a(newjob)
                newjob["age"] = 0
                pend.append(newjob)

        load_w(A[0], w_in[0], 32)
        for ap in range(10):
            kind = "q" if ap < 4 else "k" if ap == 4 else "v" if ap == 5 else "f"
            Ab = A[ap % 2]
            if ap + 1 < 10:
                load_w(A[(ap + 1) % 2], w_in[ap + 1], 32)
            nbs = range(NPO) if kind == "q" else range(NPA)
            for nb in nbs:
                Hb = HB.next()
                S.dma("sp", Hb.t[:], HT[nb], Hb, writes=[Hb.o])
                if kind in "qk":
                    CSb = CS.next()
                    S.dma("sp", CSb.t[:, 0, :], cosT[:, nb * 512:(nb + 1) * 512], CSb, writes=[CSb.o])
                    S.dma("sp", CSb.t[:, 1, :], sinT[:, nb * 512:(nb + 1) * 512], CSb, writes=[CSb.o])
                for m in range(4):
                    ps, po = PSM.next()
                    if kind == "v":
                        mm(ps[:], po, [(Hb.t[:, c, m * 128:(m + 1) * 128], Ab.t[:, c, :]) for c in range(32)],
                           reads=[Hb.o, Ab.o])
                        advance(None)
                        Eb = EV.next()
                        S.op("act", "activation", dict(out=Eb.t[:], in_=ps[:], func=AF.Copy), reads=[po], writes=[Eb.o])
                        S.dma("pool", VS[nb * 512 + m * 128:nb * 512 + (m + 1) * 128, :], Eb.t[:], Eb, reads=[Eb.o])
                        continue
                    mm(ps[:], po, [(Ab.t[:, c, m * 128:(m + 1) * 128], Hb.t[:, c, :]) for c in range(32)],
                       reads=[Hb.o, Ab.o])
                    if kind == "f":
                        advance(None)
                        Eb = EV.next()
                        S.op("act", "activation", dict(out=Eb.t[:], in_=ps[:], func=AF.Copy), reads=[po], writes=[Eb.o])
                        S.dma("pool", FT[nb, :, (ap - 6) * 4 + m, :], Eb.t[:], Eb, reads=[Eb.o])
                        continue
                    gain = QN if kind == "q" else KN
                    dst = QT[ap * 4 + m, :, nb * 512:(nb + 1) * 512] if kind == "q" else KT[m, :, nb * 512:(nb + 1) * 512]
                    advance(dict(ps=ps, po=po, gain=gain, dst=dst, cs=CSb))
        while pend:
            advance(None)

    def p3(st):
        Bp = [mkbuf(st, [128, 16, 512], BF16, "Bp") for _ in range(2)]
        Fb = Ring([mkbuf(st, [128, 16, 512], BF16, "Fb") for _ in range(2)])
        EV = Ring([mkbuf(st, [128, 512], BF16, "EV") for _ in range(4)])
        PSM = Ring(mkps(st, 4))
        k = 0
        for pi in range(8):
            pq, cb = pi // 4, pi % 4
            B_ = Bp[pi % 2]
            S.dma("sp", B_.t[:], ccs[pi], B_, writes=[B_.o])
            for nb in range(NPA):
                F_ = Fb.next()
                S.dma("sp", F_.t[:], FT[nb], F_, writes=[F_.o])
                for m in range(4):
                    ps, po = PSM.next()
                    mm(ps[:], po, [(F_.t[:, c, m * 128:(m + 1) * 128], B_.t[:, c, :]) for c in range(16)],
                       reads=[F_.o, B_.o])
                    Eb = EV.next()
                    k += 1
                    if k % 2:
                        S.op("act", "activation", dict(out=Eb.t[:], in_=ps[:], func=AF.Copy), reads=[po], writes=[Eb.o])
                    else:
                        S.op("dve", "tensor_copy", dict(out=Eb.t[:], in_=ps[:]), reads=[po], writes=[Eb.o])
                    r0 = nb * 512 + m * 128
                    S.dma("pool", PQ[cb, pq, r0:r0 + 128, :], Eb.t[:], Eb, reads=[Eb.o])

    def p4(st):
        Aa = [mkbuf(st, [128, 32, 512], BF16, "Aa") for _ in range(2)]
        Bb = Ring([mkbuf(st, [128, 32, 512], BF16, "Bb") for _ in range(3)])
        EV = Ring([mkbuf(st, [128, 512], BF16, "EV") for _ in range(4)])
        pss = mkps(st, 8)
        GR = Ring([pss[0:4], pss[4:8]])
        k = 0

        def rows(cb, pq, r0, n):
            return PQ[cb, pq, r0:r0 + n, :].rearrange("(c p) n -> p c n", p=128)

        def epi(grp, nbp, cb):
            nonlocal k
            for m in range(4):
                ps, po = grp[m]
                Eb = EV.next()
                k += 1
                if k % 2:
                    S.op("act", "activation", dict(out=Eb.t[:], in_=ps[:], func=AF.Copy), reads=[po], writes=[Eb.o])
                else:
                    S.op("dve", "tensor_copy", dict(out=Eb.t[:], in_=ps[:]), reads=[po], writes=[Eb.o])
                S.dma("pool", ZT[nbp, :, cb * 4 + m, :], Eb.t[:], Eb, reads=[Eb.o])

        for cb in range(4):
            for kp in range(2):
                S.dma("sp", Aa[kp].t[:, 0:16, :], rows(cb, kp, 0, 2048), Aa[kp], writes=[Aa[kp].o])
                S.dma("sp", Aa[kp].t[:, 16:32, :], rows(cb, kp, 3072, 2048), Aa[kp], writes=[Aa[kp].o])
            for nb in range(4):
                grp = GR.next()
                for kp in range(2):
                    Bk = Bb.next()
                    S.dma("sp", Bk.t[:], csp[kp, nb], Bk, writes=[Bk.o])
                    for m in range(4):
                        ps, po = grp[m]
                        mm(ps[:], po, [(Aa[kp].t[:, c, m * 128:(m + 1) * 128], Bk.t[:, c, :]) for c in range(32)],
                           reads=[Aa[kp].o, Bk.o], start=(kp == 0), stop=(kp == 1))
                epi(grp, nb, cb)
        for cb in range(4):
            A0 = Aa[cb % 2]
            S.dma("sp", A0.t[:, 0:8, :], rows(cb, 0, 2048, 1024), A0, writes=[A0.o])
            S.dma("sp", A0.t[:, 8:16, :], rows(cb, 0, 5120, 1024), A0, writes=[A0.o])
            S.dma("sp", A0.t[:, 16:24, :], rows(cb, 1, 2048, 1024), A0, writes=[A0.o])
            S.dma("sp", A0.t[:, 24:32, :], rows(cb, 1, 5120, 1024), A0, writes=[A0.o])
            for nb in range(2):
                grp = GR.next()
                Bk = Bb.next()
                S.dma("sp", Bk.t[:], css[nb], Bk, writes=[Bk.o])
                for m in range(4):
                    ps, po = grp[m]
                    mm(ps[:], po, [(A0.t[:, c, m * 128:(m + 1) * 128], Bk.t[:, c, :]) for c in range(32)],
                       reads=[A0.o, Bk.o])
                epi(grp, 4 + nb, cb)

    def p5(st):
        ON, EP = consts(st)
        KTg = mkbuf(st, [128, 4096], BF16, "KTg")
        Vg = mkbuf(st, [128, 32, 128], BF16, "Vg")
        QG = mkbuf(st, [128, 4, 2048], BF16, "QG")
        ER = Ring([mkbuf(st, [128, 512], BF16, "E") for _ in range(3)])
        RZ = Ring([mkbuf(st, [128, 512], F32, "RZ") for _ in range(2)])
        OB = Ring([mkbuf(st, [128, 4, 128], BF16, "OB") for _ in range(2)])
        pss = mkps(st, 6)
        PSS = Ring(pss[0:2])
        PSO = Ring(pss[2:4])
        PSZ = Ring(pss[4:6])
        seqs = [((0, 2048), (3072, 5120), 0, 2048, 0), ((2048, 3072), (5120, 6144), 2048, 1024, 4)]
        for (o0, o1), (t0, t1), q0, nq, pan0 in seqs:
            hk = o1 - o0
            nch = 2 * hk // 128
            for g in range(4):
                S.dma("sp", KTg.t[:, 0:hk], KT[g, :, o0:o1], KTg, writes=[KTg.o])
                S.dma("sp", KTg.t[:, hk:2 * hk], KT[g, :, t0:t1], KTg, writes=[KTg.o])
                S.dma("sp", Vg.t[:, 0:nch // 2, :], VS[o0:o1, g * 128:(g + 1) * 128].rearrange("(c p) d -> p c d", p=128),
                      Vg, writes=[Vg.o])
                S.dma("sp", Vg.t[:, nch // 2:nch, :], VS[t0:t1, g * 128:(g + 1) * 128].rearrange("(c p) d -> p c d", p=128),
                      Vg, writes=[Vg.o])
                S.dma("sp", QG.t[:, :, 0:nq], QT[g * 4:(g + 1) * 4, :, q0:q0 + nq].rearrange("h d q -> d h q"),
                      QG, writes=[QG.o])
                for qb in range(nq // 128):
                    rhsq = QG.t[:, :, qb * 128:(qb + 1) * 128]
                    pso, poo = PSO.next()
                    psz, poz = PSZ.next()

                    def issue_s(sc):
                        ps, po = PSS.next()
                        mm(ps[:], po, [(KTg.t[:, sc * 128:(sc + 1) * 128], rhsq)], reads=[KTg.o, QG.o])
                        Eb = ER.next()
                        S.op("act", "activation", dict(out=Eb.t[:], in_=ps[:], func=AF.Exp, scale=ATT_SCALE),
                             reads=[po], writes=[Eb.o])
                        return Eb

                    Ecur = issue_s(0)
                    for sc in range(nch):
                        Enext = issue_s(sc + 1) if sc + 1 < nch else None
                        mm(pso[:], poo, [(Vg.t[:, sc, :], Ecur.t[:])], reads=[Vg.o, Ecur.o],
                           start=(sc == 0), stop=(sc == nch - 1))
                        mm(psz[:], poz, [(ON.t[:], Ecur.t[:])], reads=[ON.o, Ecur.o],
                           start=(sc == 0), stop=(sc == nch - 1))
                        Ecur = Enext
                    rz, ob = RZ.next(), OB.next()
                    S.op("dve", "reciprocal", dict(out=rz.t[:], in_=psz[:]), reads=[poz], writes=[rz.o])
                    S.op("dve", "tensor_tensor", dict(out=ob.t[:].rearrange("p h q -> p (h q)"), in0=pso[:], in1=rz.t[:],
                                                      op=ALU.mult), reads=[poo, rz.o], writes=[ob.o])
                    tq = qb * 128
                    S.dma("pool", AT[pan0 + tq // 512, :, g * 4:(g + 1) * 4, tq % 512:tq % 512 + 128], ob.t[:], ob,
                          reads=[ob.o])

    def p6(st):
        Wa = mkbuf(st, [128, 16, 256], BF16, "Wa")
        Wf = mkbuf(st, [128, 16, 256], BF16, "Wf")
        Wg0 = mkbuf(st, [128, 32, 256], BF16, "Wg0")
        Wg1 = mkbuf(st, [128, 32, 256], BF16, "Wg1")
        ATb = Ring([mkbuf(st, [128, 16, 512], BF16, "ATb") for _ in range(2)])
        ZTb = Ring([mkbuf(st, [128, 16, 512], BF16, "ZTb") for _ in range(2)])
        HBb = Ring([mkbuf(st, [128, 32, 512], BF16, "HBb") for _ in range(2)])
        BG = mkbuf(st, [128, 64], F32, "BG")
        S.dma("sp", BG.t[:], bg, BG, writes=[BG.o])
        S0 = Ring([mkbuf(st, [128, 512], F32, "S0") for _ in range(2)])
        S1 = Ring([mkbuf(st, [128, 512], F32, "S1") for _ in range(2)])
        MO = Ring([mkbuf(st, [128, 512], BF16, "MO") for _ in range(2)])
        pss = mkps(st, 8)
        GR = Ring([pss[0:4], pss[4:8]])
        for mb2 in range(16):
            load_w(Wa, w_attn[mb2], 16)
            load_w(Wf, w_four[mb2], 16)
            load_w(Wg0, w_gate[mb2], 32)
            load_w(Wg1, w_gate[16 + mb2], 32)
            for nb in range(NPO):
                a_, z_, h_ = ATb.next(), ZTb.next(), HBb.next()
                S.dma("sp", a_.t[:], AT[nb], a_, writes=[a_.o])
                S.dma("sp", z_.t[:], ZT[nb], z_, writes=[z_.o])
                S.dma("sp", h_.t[:], HT[nb], h_, writes=[h_.o])
                for mi in range(2):
                    m = mb2 * 2 + mi
                    (pa, oa), (pf, of), (pg0, og0), (pg1, og1) = GR.next()
                    cs = slice(mi * 128, (mi + 1) * 128)
                    mm(pa[:], oa, [(Wa.t[:, c, cs], a_.t[:, c, :]) for c in range(16)], reads=[Wa.o, a_.o])
                    mm(pf[:], of, [(Wf.t[:, c, cs], z_.t[:, c, :]) for c in range(16)], reads=[Wf.o, z_.o])
                    mm(pg0[:], og0, [(Wg0.t[:, c, cs], h_.t[:, c, :]) for c in range(32)], reads=[Wg0.o, h_.o])
                    mm(pg1[:], og1, [(Wg1.t[:, c, cs], h_.t[:, c, :]) for c in range(32)], reads=[Wg1.o, h_.o])
                    s0, s1, mo = S0.next(), S1.next(), MO.next()
                    t0, t1 = s0, s1
                    S.op("act", "activation", dict(out=s0.t[:], in_=pg0[:], func=AF.Sigmoid, bias=BG.t[:, m:m + 1], scale=1.0),
                         reads=[og0, BG.o], writes=[s0.o])
                    S.op("act", "activation", dict(out=s1.t[:], in_=pg1[:], func=AF.Sigmoid, bias=BG.t[:, 32 + m:33 + m], scale=1.0),
                         reads=[og1, BG.o], writes=[s1.o])
                    S.op("dve", "tensor_tensor", dict(out=t0.t[:], in0=pa[:], in1=s0.t[:], op=ALU.mult),
                         reads=[oa, s0.o], writes=[t0.o])
                    S.op("dve", "tensor_tensor", dict(out=t1.t[:], in0=pf[:], in1=s1.t[:], op=ALU.mult),
                         reads=[of, s1.o], writes=[t1.o])
                    S.op("pool", "tensor_tensor", dict(out=mo.t[:], in0=t0.t[:], in1=t1.t[:], op=ALU.add),
                         reads=[t0.o, t1.o], writes=[mo.o])
                    S.dma("pool", MT[nb, :, m, :], mo.t[:], mo, reads=[mo.o])

    def resid_gemm(W, kcw, Bsrc, resid, dst):
        def fn(st):
            A = [mkbuf(st, [128, kcw, 512], BF16, "A") for _ in range(2)]
            Bb = Ring([mkbuf(st, [128, kcw, 512], BF16, "B") for _ in range(2)])
            XR = Ring([mkbuf(st, [128, 512], F32, "XR") for _ in range(3)])
            XO = Ring([mkbuf(st, [128, 512], F32, "XO") for _ in range(3)])
            PSM = Ring(mkps(st, 4))
            load_w(A[0], W[0], kcw)
            for ap in range(8):
                Ab = A[ap % 2]
                if ap + 1 < 8:
                    load_w(A[(ap + 1) % 2], W[ap + 1], kcw)
                for nb in range(NPO):
                    B_ = Bb.next()
                    S.dma("sp", B_.t[:], Bsrc[nb], B_, writes=[B_.o])
                    for mi in range(4):
                        m = ap * 4 + mi
                        ps, po = PSM.next()
                        mm(ps[:], po, [(Ab.t[:, c, mi * 128:(mi + 1) * 128], B_.t[:, c, :]) for c in range(kcw)],
                           reads=[Ab.o, B_.o])
                        xr, xo = XR.next(), XO.next()
                        S.dma("sp", xr.t[:], resid[nb, :, m, :], xr, writes=[xr.o])
                        S.op("dve", "tensor_tensor", dict(out=xo.t[:], in0=ps[:], in1=xr.t[:], op=ALU.add),
                             reads=[po, xr.o], writes=[xo.o])
                        S.dma("pool", dst[nb, :, m, :], xo.t[:], xo, reads=[xo.o])
        return fn

    def proj_fm(W, naps, Bsrc, nbs, dst, woff=0):
        def fn(st):
            A = [mkbuf(st, [128, 32, 512], BF16, "A") for _ in range(2)]
            Bb = Ring([mkbuf(st, [128, 32, 512], BF16, "B") for _ in range(2)])
            EV = Ring([mkbuf(st, [128, 512], BF16, "EV") for _ in range(4)])
            PSM = Ring(mkps(st, 4))
            k = 0
            load_w(A[0], W[woff], 32)
            for ap in range(naps):
                Ab = A[ap % 2]
                if ap + 1 < naps:
                    load_w(A[(ap + 1) % 2], W[woff + ap + 1], 32)
                for nb in range(nbs):
                    B_ = Bb.next()
                    S.dma("sp", B_.t[:], Bsrc[nb], B_, writes=[B_.o])
                    for mi in range(4):
                        ps, po = PSM.next()
                        mm(ps[:], po, [(Ab.t[:, c, mi * 128:(mi + 1) * 128], B_.t[:, c, :]) for c in range(32)],
                           reads=[Ab.o, B_.o])
                        Eb = EV.next()
                        k += 1
                        if k % 2:
                            S.op("act", "activation", dict(out=Eb.t[:], in_=ps[:], func=AF.Copy), reads=[po], writes=[Eb.o])
                        else:
                            S.op("dve", "tensor_copy", dict(out=Eb.t[:], in_=ps[:]), reads=[po], writes=[Eb.o])
                        S.dma("pool", dst[ap * 4 + mi, :, nb * 512:(nb + 1) * 512], Eb.t[:], Eb, reads=[Eb.o])
        return fn

    def p8v(st):
        A = [mkbuf(st, [128, 32, 512], BF16, "A") for _ in range(2)]
        B_ = mkbuf(st, [128, 32, 512], BF16, "B")
        EV = Ring([mkbuf(st, [128, 512], BF16, "EV") for _ in range(4)])
        PSM = Ring(mkps(st, 4))
        S.dma("sp", B_.t[:], MNT[0], B_, writes=[B_.o])
        for ap in range(2):
            load_w(A[ap], w_ckv[2 + ap], 32)
        for ap in range(2):
            for mi in range(4):
                ps, po = PSM.next()
                mm(ps[:], po, [(B_.t[:, c, mi * 128:(mi + 1) * 128], A[ap].t[:, c, :]) for c in range(32)],
                   reads=[A[ap].o, B_.o])
                Eb = EV.next()
                S.op("act", "activation", dict(out=Eb.t[:], in_=ps[:], func=AF.Copy), reads=[po], writes=[Eb.o])
                S.dma("pool", VC[mi * 128:(mi + 1) * 128, ap * 512:(ap + 1) * 512], Eb.t[:], Eb, reads=[Eb.o])

    def p8c(st):
        ON, EP = consts(st)
        KCb = mkbuf(st, [128, 8, 512], BF16, "KCb")
        QCb = mkbuf(st, [128, 8, TOWN], BF16, "QCb")
        VCb = mkbuf(st, [128, 4, 1024], BF16, "VCb")
        S.dma("sp", KCb.t[:], KCT.rearrange("b d m -> d b m"), KCb, writes=[KCb.o])
        S.dma("sp", QCb.t[:], QCT.rearrange("b d t -> d b t"), QCb, writes=[QCb.o])
        S.dma("sp", VCb.t[:], VC.rearrange("(c p) n -> p c n", p=128), VCb, writes=[VCb.o])
        ER = Ring([mkbuf(st, [128, 512], BF16, "E") for _ in range(4)])
        RZ = Ring([mkbuf(st, [128, 512], F32, "RZ") for _ in range(2)])
        OB = Ring([mkbuf(st, [128, 512], BF16, "OB") for _ in range(4)])
        pss = mkps(st, 8)
        PSS = Ring(pss[0:2])
        PSO = Ring(pss[2:6])
        PSZ = Ring(pss[6:8])
        for nb in range(NPO):
            mc0 = 0 if nb < 4 else 2
            for hc in range(4):
                Es = []
                for mc in range(2):
                    ps, po = PSS.next()
                    ms = slice((mc0 + mc) * 128, (mc0 + mc + 1) * 128)
                    mm(ps[:], po, [(KCb.t[:, hc * 2 + db, ms], QCb.t[:, hc * 2 + db, nb * 512:(nb + 1) * 512]) for db in range(2)],
                       reads=[KCb.o, QCb.o])
                    Eb = ER.next()
                    S.op("act", "activation", dict(out=Eb.t[:], in_=ps[:], func=AF.Exp, scale=CA_SCALE), reads=[po], writes=[Eb.o])
                    Es.append(Eb)
                psz, poz = PSZ.next()
                mm(psz[:], poz, [(ON.t[:], Es[mc].t[:]) for mc in range(2)], reads=[ON.o, Es[0].o, Es[1].o])
                rz = RZ.next()
                S.op("dve", "reciprocal", dict(out=rz.t[:], in_=psz[:]), reads=[poz], writes=[rz.o])
                for dvb in range(2):
                    pso, poo = PSO.next()
                    cs = slice(hc * 256 + dvb * 128, hc * 256 + (dvb + 1) * 128)
                    mm(pso[:], poo, [(VCb.t[:, mc0 + mc, cs], Es[mc].t[:]) for mc in range(2)],
                       reads=[VCb.o, Es[0].o, Es[1].o])
                    ob = OB.next()
                    S.op("dve", "tensor_tensor", dict(out=ob.t[:], in0=pso[:], in1=rz.t[:], op=ALU.mult),
                         reads=[poo, rz.o], writes=[ob.o])
                    S.dma("pool", OCT[nb, :, hc * 2 + dvb, :], ob.t[:], ob, reads=[ob.o])

    def p9cd(st):
        SUBK = mkbuf(st, [128, 2, 128], BF16, "SUBK")
        S.dma("pool", SUBK.t[:], subkT, SUBK, writes=[SUBK.o])
        IDB = mkbuf(st, [128, 128], BF16, "IDB")
        S.dma("sp", IDB.t[:], ident, IDB, writes=[IDB.o])
        Q = mkbuf(st, [128, 16, 128], BF16, "Q16")
        SK = mkbuf(st, [128, 16, 128], F32, "SK")
        TOP = mkbuf(st, [128, 16, 16], F32, "TOP")
        B16 = mkbuf(st, [128, 8, 16], F32, "B16")
        JUNK = mkbuf(st, [128, 16], F32, "JUNK")
        NEGM = mkbuf(st, [128, 8], F32, "NEGM")
        ZS = mkbuf(st, [128, 8], F32, "ZS")
        LNZ = mkbuf(st, [128, 8], F32, "LNZ")
        BIAS = mkbuf(st, [128, 8], F32, "BIAS")
        TAUP = mkbuf(st, [128, 8], F32, "TAUP")
        S1B = mkbuf(st, [128, 8, 128], F32, "S1B")
        Xr = Ring([mkbuf(st, [128, 8, 2, 128], F32, "X") for _ in range(3)])
        Er = Ring([mkbuf(st, [128, 8, 256], BF16, "E") for _ in range(3)])
        WSFr = Ring([mkbuf(st, [128, 256], F32, "WSF") for _ in range(8)])
        WSr = Ring([mkbuf(st, [128, 256], BF16, "WS") for _ in range(4)])
        WTSr = Ring([mkbuf(st, [128, 16, 128], BF16, "WTS") for _ in range(2)])
        pssk = mkps(st, 2)
        pstr = Ring(mkps(st, 2, BF16, 256))
        A = [mkbuf(st, [128, 32, 512], BF16, "A") for _ in range(2)]
        Bb = Ring([mkbuf(st, [128, 32, 512], BF16, "B") for _ in range(2)])
        GA = Ring([mkbuf(st, [128, 512], BF16, "GA") for _ in range(4)])
        PSM = Ring(mkps(st, 4))
        LAG = 4

        def gen_c():
            for tt in range(24):
                nb, tsub = tt // 4, tt % 4
                S.dma("sp", Q.t[:], QPT[:, :, tt * 128:(tt + 1) * 128].rearrange("b d t -> d b t"), Q, writes=[Q.o])
                for half in range(2):
                    for h8 in range(8):
                        hc = half * 8 + h8
                        ps, po = pssk[h8 // 4]
                        mm(ps[:, (h8 % 4) * 128:(h8 % 4 + 1) * 128], po, [(Q.t[:, hc, :], SUBK.t[:, hc % 2, :])],
                           reads=[Q.o, SUBK.o])
                    for bk in range(2):
                        ps, po = pssk[bk]
                        c0 = half * 8 + bk * 4
                        S.op("act", "activation", dict(out=SK.t[:, c0:c0 + 4, :].rearrange("p a b -> p (a b)"), in_=ps[:],
                                                       func=AF.Copy), reads=[po], writes=[SK.o])
                yield
                X0, X1 = Xr.items[0], Xr.items[1]
                SKRv = X0.t[:].rearrange("p h i j -> p (h i) j")
                CNv = X1.t[:].rearrange("p h i j -> p h (i j)")
                for hc in range(16):
                    S.op("dve", "max", dict(out=TOP.t[:, hc, 0:8], in_=SK.t[:, hc, :]), reads=[SK.o], writes=[TOP.o], nosame=True)
                for hc in range(16):
                    S.op("dve", "match_replace", dict(out=SKRv[:, hc, :], in_to_replace=TOP.t[:, hc, 0:8], in_values=SK.t[:, hc, :],
                                                      imm_value=-1e30), reads=[SK.o, TOP.o], writes=[X0.o], nosame=(hc > 0))
                yield
                for hc in range(16):
                    S.op("dve", "max", dict(out=TOP.t[:, hc, 8:16], in_=SKRv[:, hc, :]), reads=[X0.o], writes=[TOP.o], nosame=(hc > 0))
                yield
                for hh in range(2):
                    for h4 in range(4):
                        h = hh * 4 + h4
                        a_ = TOP.t[:, 2 * h, :].unsqueeze(2).broadcast_to([128, 16, 16])
                        b_ = TOP.t[:, 2 * h + 1, :].unsqueeze(1).broadcast_to([128, 16, 16])
                        S.op("dve", "tensor_tensor", dict(out=CNv[:, h4, :].rearrange("p (a b) -> p a b", a=16), in0=a_, in1=b_, op=ALU.add),
                             reads=[TOP.o], writes=[X1.o], nosame=(h4 > 0))
                    for h4 in range(4):
                        h = hh * 4 + h4
                        S.op("dve", "max", dict(out=B16.t[:, h, 0:8], in_=CNv[:, h4, :]), reads=[X1.o], writes=[B16.o], nosame=(h4 > 0))
                    for h4 in range(4):
                        h = hh * 4 + h4
                        S.op("dve", "match_replace", dict(out=CNv[:, 4 + h4, :], in_to_replace=B16.t[:, h, 0:8], in_values=CNv[:, h4, :],
                                                          imm_value=-1e30), reads=[X1.o, B16.o], writes=[X1.o], nosame=(h4 > 0))
                    for h4 in range(4):
                        h = hh * 4 + h4
                        S.op("dve", "max", dict(out=B16.t[:, h, 8:16], in_=CNv[:, 4 + h4, :]), reads=[X1.o], writes=[B16.o], nosame=(h4 > 0))
                    yield
                S.op("dve", "tensor_scalar", dict(out=NEGM.t[:], in0=B16.t[:, :, 0], scalar1=-1.0, scalar2=0.0,
                                                  op0=ALU.mult, op1=ALU.add), reads=[B16.o], writes=[NEGM.o])
                S.op("dve", "memset", dict(ap=ZS.t[:], constant=0.0), writes=[ZS.o])
                for h in range(8):
                    S.op("act", "activation", dict(out=JUNK.t[:], in_=B16.t[:, h, :], func=AF.Exp, bias=NEGM.t[:, h:h + 1],
                                                   scale=1.0, accum_out=ZS.t[:, h:h + 1]),
                         reads=[B16.o, NEGM.o], writes=[JUNK.o, ZS.o], nosame=(h > 0))
                S.op("act", "activation", dict(out=LNZ.t[:], in_=ZS.t[:], func=AF.Ln), reads=[ZS.o], writes=[LNZ.o])
                S.op("dve", "tensor_tensor", dict(out=BIAS.t[:], in0=NEGM.t[:], in1=LNZ.t[:], op=ALU.subtract),
                     reads=[NEGM.o, LNZ.o], writes=[BIAS.o])
                S.op("dve", "tensor_tensor", dict(out=TAUP.t[:], in0=B16.t[:, :, 15], in1=BIAS.t[:], op=ALU.add),
                     reads=[B16.o, BIAS.o], writes=[TAUP.o])
                S.op("dve", "tensor_scalar", dict(out=TAUP.t[:], in0=TAUP.t[:], scalar1=1.0, scalar2=-2e-5,
                                                  op0=ALU.mult, op1=ALU.add), reads=[TAUP.o], writes=[TAUP.o])
                skv = SK.t[:].rearrange("p (h c) n -> p h c n", c=2)
                S.op("dve", "tensor_tensor", dict(out=S1B.t[:], in0=skv[:, :, 0, :],
                                                  in1=BIAS.t[:].unsqueeze(2).broadcast_to([128, 8, 128]), op=ALU.add),
                     reads=[SK.o, BIAS.o], writes=[S1B.o])
                s2 = skv[:, :, 1, :]
                yield
                pend = []
                wts = [None]

                def stage2(eb, WSF):
                    if eb % 8 == 0:
                        wts[0] = WTSr.next()
                    WTS = wts[0]
                    WS = WSr.next()
                    S.op("pool", "tensor_copy", dict(out=WS.t[:], in_=WSF.t[:]), reads=[WSF.o], writes=[WS.o])
                    pt, pto = pstr.next()
                    for k in range(2):
                        S.op("pe", "transpose", dict(out=pt[:, k * 128:(k + 1) * 128], in_=WS.t[:, k * 128:(k + 1) * 128],
                                                     identity=IDB.t[:]), reads=[WS.o, IDB.o], writes=[pto], signal=(k == 1))
                    cl = (eb % 8) * 2
                    S.op("dve", "tensor_copy", dict(out=WTS.t[:, cl:cl + 2, :].rearrange("p a b -> p (a b)"), in_=pt[:]),
                         reads=[pto], writes=[WTS.o], nosame=True)
                    if eb % 8 == 7:
                        c16 = ((eb // 8) % 2) * 16
                        S.dma("sp", WTp[nb, eb // 16, :, c16:c16 + 16, tsub * 128:(tsub + 1) * 128], WTS.t[:], WTS, reads=[WTS.o])

                for eb in range(64):
                    i0 = eb * 2
                    Xb, Eb, WSF = Xr.next(), Er.next(), WSFr.next()
                    S.op("pool", "tensor_tensor",
                         dict(out=Xb.t[:], in0=S1B.t[:, :, i0:i0 + 2].unsqueeze(3).broadcast_to([128, 8, 2, 128]),
                              in1=s2.unsqueeze(2).broadcast_to([128, 8, 2, 128]), op=ALU.add),
                         reads=[S1B.o, SK.o], writes=[Xb.o])
                    xv = Xb.t[:].rearrange("p h i j -> p h (i j)")
                    S.op("act", "activation", dict(out=Eb.t[:].rearrange("p h e -> p (h e)"),
                                                   in_=Xb.t[:].rearrange("p h i j -> p (h i j)"), func=AF.Exp),
                         reads=[Xb.o], writes=[Eb.o])
                    for h in range(8):
                        S.op("dve", "scalar_tensor_tensor",
                             dict(out=Eb.t[:, h, :], in0=xv[:, h, :], scalar=TAUP.t[:, h:h + 1], in1=Eb.t[:, h, :],
                                  op0=ALU.is_ge, op1=ALU.mult), reads=[Xb.o, TAUP.o, Eb.o], writes=[Eb.o], nosame=(h > 0))
                    S.op("dve", "tensor_reduce", dict(out=WSF.t[:], in_=Eb.t[:].rearrange("p h e -> p e h"),
                                                      axis=AX.X, op=ALU.add), reads=[Eb.o], writes=[WSF.o])
                    pend.append((eb, WSF))
                    if len(pend) > LAG:
                        stage2(*pend.pop(0))
                    if eb % 2 == 1:
                        yield
                while pend:
                    stage2(*pend.pop(0))
                yield

        def gen_d():
            tiles = [(mb, nb, mi) for mb in range(32) for nb in range(NPO) for mi in range(4)]
            info = {}
            load_w(A[0], uT[0], 32)
            B_ = None
            for j in range(len(tiles) + 4):
                if j < len(tiles):
                    mb, nb, mi = tiles[j]
                    Ab = A[mb % 2]
                    if nb == 0 and mi == 0 and mb + 1 < 32:
                        load_w(A[(mb + 1) % 2], uT[mb + 1], 32)
                    if mi == 0:
                        B_ = Bb.next()
                        S.dma("sp", B_.t[:], HPT[nb], B_, writes=[B_.o])
                    ps, po = PSM.next()
                    mm(ps[:], po, [(Ab.t[:, c, mi * 128:(mi + 1) * 128], B_.t[:, c, :]) for c in range(32)],
                       reads=[Ab.o, B_.o])
                    info[j] = (ps, po, nb, mb * 4 + mi)
                if 0 <= j - 3 < len(tiles):
                    ps, po, nb_, ec = info[j - 3]
                    ga = GA.next()
                    S.op("act", "activation", dict(out=ga.t[:], in_=ps[:], func=AF.Gelu), reads=[po], writes=[ga.o])
                    info[j - 3] = (ga, nb_, ec)
                if 0 <= j - 4 < len(tiles):
                    ga, nb_, ec = info.pop(j - 4)
                    S.dma("sp", GAT[nb_, ec // 32, :, ec % 32, :], ga.t[:], ga, reads=[ga.o])
                yield

        gc, gd = gen_c(), gen_d()
        alive = [True, True]
        while alive[0] or alive[1]:
            for i, g in enumerate((gc, gd)):
                for _ in range(4):
                    if alive[i]:
                        try:
                            next(g)
                        except StopIteration:
                            alive[i] = False

    def p9e(st):
        ACC = mkbuf(st, [128, 32, 512], F32, "ACC")
        A = Ring([mkbuf(st, [128, 32, 512], BF16, "A") for _ in range(2)])
        Bg = mkbuf(st, [128, 32, 512], BF16, "Bg")
        Bw = Ring([mkbuf(st, [128, 8, 512], BF16, "Bw") for _ in range(2)])
        PSM = Ring(mkps(st, 8))
        for nb in range(NPO):
            S.dma("sp", ACC.t[:], X2T[nb], ACC, writes=[ACC.o])
            for kp in range(4):
                S.dma("sp", Bg.t[:], GAT[nb, kp], Bg, writes=[Bg.o])
                for q in range(4):
                    bw = Bw.next()
                    c8 = slice(q * 8, (q + 1) * 8)
                    S.dma("sp", bw.t[:], WTp[nb, kp, :, c8, :], bw, writes=[bw.o])
                    S.op("dve", "tensor_tensor", dict(out=Bg.t[:, c8, :], in0=Bg.t[:, c8, :], in1=bw.t[:], op=ALU.mult),
                         reads=[Bg.o, bw.o], writes=[Bg.o])
                for ap in range(8):
                    Ab = A.next()
                    load_w(Ab, ev[ap, kp], 32)
                    for mi in range(4):
                        m = ap * 4 + mi
                        ps, po = PSM.next()
                        mm(ps[:], po, [(Ab.t[:, c, mi * 128:(mi + 1) * 128], Bg.t[:, c, :]) for c in range(32)],
                           reads=[Ab.o, Bg.o])
                        S.op("dve", "tensor_tensor", dict(out=ACC.t[:, m, :], in0=ps[:], in1=ACC.t[:, m, :], op=ALU.add),
                             reads=[po, ACC.o], writes=[ACC.o], nosame=True)
            S.dma("act", X3T[nb], ACC.t[:], ACC, reads=[ACC.o])

    run_phase(norm_phase([xT[j] for j in range(NPA)], g_mix, [HT[j] for j in range(NPA)], BF16))
    run_phase(p2)
    run_phase(p3)
    run_phase(p4)
    run_phase(p5)
    run_phase(p6)
    run_phase(resid_gemm(w_out, 32, MT, xT, X1T))
    run_phase(norm_phase([X1T[j] for j in range(NPO)], g_ca, [HCT[j] for j in range(NPO)], BF16))
    run_phase(norm_phase([memT], g_mem, [MNT[0]], BF16))
    run_phase(proj_fm(w_cq, 2, HCT, NPO, QCT))
    run_phase(proj_fm(w_ckv, 2, MNT, 1, KCT))
    run_phase(p8v)
    run_phase(p8c)
    run_phase(resid_gemm(w_co, 8, OCT, X1T, X2T))
    run_phase(norm_phase([X2T[j] for j in range(NPO)], g_ffn, [HPT[j] for j in range(NPO)], BF16))
    run_phase(proj_fm(w_pq, 4, HPT, NPO, QPT))
    run_phase(p9cd)
    run_phase(p9e)
    run_phase(norm_phase([X3T[j] for j in range(NPO)], g_fin, [yT[j] for j in range(NPO)], F32))
    outer.close()
    return nc, S.nops


def _panels(x2d, kc):
    n = x2d.shape[0] // 512
    return np.ascontiguousarray(x2d.reshape(n, 512, kc, 128).transpose(0, 3, 2, 1))


def _kpanel(m2d):
    k = m2d.shape[0] // 128
    return np.ascontiguousarray(m2d.reshape(k, 128, m2d.shape[1]).transpose(1, 0, 2))


def _wp(W, wc):
    W = np.asarray(W, np.float32)
    K, N = W.shape
    return np.ascontiguousarray(W.reshape(K // 128, 128, N // wc, wc).transpose(2, 1, 0, 3))


def _gain(g):
    return np.ascontiguousarray(np.asarray(g, np.float32).reshape(-1, 128).T)


def _consts(hf):
    own_p = hf * 2048 + np.arange(2048)
    oth_p = (1 - hf) * 2048 + np.arange(2048)
    own_s = hf * 1024 + np.arange(1024)
    oth_s = (1 - hf) * 1024 + np.arange(1024)
    pos_all = np.concatenate([own_p, own_s, oth_p, oth_s])
    d = np.arange(128)
    a, f = d // 64, d % 32
    inv = 10000.0 ** (-np.arange(0, 64, 2, dtype=np.float64) / 64)
    pa = np.where(a[:, None] == 0, pos_all[None, :] // 64, pos_all[None, :] % 64).astype(np.float64)
    ang = pa * inv[f][:, None]
    cosT = np.cos(ang).astype(np.float32)
    sinT = np.sin(ang).astype(np.float32)

    def dft(rows, cols, n, neg_sin):
        prod = (rows[:, None].astype(np.int64) * cols[None, :].astype(np.int64)) % n
        angm = 2.0 * np.pi * prod / n
        c = np.cos(angm) / np.sqrt(n)
        s = np.sin(angm) / np.sqrt(n)
        return c, (-s if neg_sin else s)

    kp_rows = np.concatenate([own_p, oth_p])
    c, s = dft(kp_rows, own_p, 4096, True)
    csp = np.stack([np.stack([_kpanel(mat[:, nb * 512:(nb + 1) * 512]) for nb in range(4)]) for mat in (c, s)]).astype(BF)
    ks_rows = np.concatenate([own_s, oth_s])
    c, s = dft(ks_rows, own_s, 2048, True)
    mat = np.concatenate([c, s], axis=0)
    css = np.stack([_kpanel(mat[:, nb * 512:(nb + 1) * 512]) for nb in range(2)]).astype(BF)
    return dict(cosT=cosT, sinT=sinT, csp=csp, css=css)


_CACHE = {}


def kernel(x_prompt, x_sample, mem_prompt, mem_sample, norm_mix, w_in, q_norm, k_norm, w_attn_br, w_four_br,
           w_gate, b_gate, w_out, norm_ca, mem_norm, w_cq, w_ckv, w_co, norm_ffn, w_pq, sub_keys, expert_u,
           expert_v, final_norm):
    f32 = lambda a: np.ascontiguousarray(np.asarray(a, np.float32))
    x_prompt, x_sample, mem_prompt, mem_sample = map(f32, (x_prompt, x_sample, mem_prompt, mem_sample))
    if "nc" not in _CACHE:
        _CACHE["nc"] = build()[0]
    nc = _CACHE["nc"]
    ch = np.arange(2048)
    prod = (ch[:, None] * ch[None, :]) % 2048
    angc = 2.0 * np.pi * prod / 2048
    cc = np.cos(angc) / np.sqrt(2048.0)
    sc = np.sin(angc) / np.sqrt(2048.0)
    ccs = np.stack([_kpanel(mat[:, cb * 512:(cb + 1) * 512]) for mat in (cc, sc) for cb in range(4)]).astype(BF)
    rm = np.zeros((128, 128), np.float32)
    for dout in range(128):
        if (dout % 64) < 32:
            rm[dout + 32, dout] = -1.0
        else:
            rm[dout - 32, dout] = 1.0
    shared = dict(
        w_in=_wp(w_in[0], 512), w_attn=_wp(w_attn_br[0], 256), w_four=_wp(w_four_br[0], 256), w_gate=_wp(w_gate[0], 256),
        w_out=_wp(w_out[0], 512), w_cq=_wp(w_cq[0], 512), w_ckv=_wp(w_ckv[0], 512), w_co=_wp(w_co[0], 512),
        w_pq=_wp(w_pq[0], 512),
        uT=np.ascontiguousarray(np.asarray(expert_u[0], np.float32).reshape(32, 512, 32, 128).transpose(0, 3, 2, 1)),
        ev=np.ascontiguousarray(np.asarray(expert_v[0], np.float32).reshape(4, 32, 128, 8, 512).transpose(3, 0, 2, 1, 4)),
        g_mix=_gain(norm_mix[0]), g_ca=_gain(norm_ca[0]), g_mem=_gain(mem_norm[0]), g_ffn=_gain(norm_ffn[0]),
        g_fin=_gain(final_norm), qn=f32(np.asarray(q_norm[0]).reshape(128, 1)), kn=f32(np.asarray(k_norm[0]).reshape(128, 1)),
        bg=_gain(b_gate[0]),
        subkT=np.ascontiguousarray(np.asarray(sub_keys[0], np.float32).transpose(2, 0, 1)),
        rmat=rm.astype(BF), ones=np.ones((128, 128), BF), ident=np.eye(128, dtype=np.float32).astype(BF), ccs=ccs,
    )
    pc = [_consts(0), _consts(1)]
    in_maps = []
    for c in range(8):
        b, hf = c // 2, c % 2
        op_ = slice(hf * 2048, (hf + 1) * 2048)
        tp_ = slice((1 - hf) * 2048, (2 - hf) * 2048)
        os_ = slice(hf * 1024, (hf + 1) * 1024)
        ts_ = slice((1 - hf) * 1024, (2 - hf) * 1024)
        xall = np.concatenate([x_prompt[b, op_], x_sample[b, os_], x_prompt[b, tp_], x_sample[b, ts_]], axis=0)
        mem = np.concatenate([mem_prompt[b], mem_sample[b]], axis=0)
        m = dict(shared)
        m.update(pc[hf])
        m["xT"] = _panels(xall, 32)
        m["memT"] = _panels(mem, 32)[0]
        in_maps.append(m)
    res = run_bass_kernel_spmd(nc, in_maps, core_ids=list(range(8)))
    y_prompt = np.empty_like(x_prompt)
    y_sample = np.empty_like(x_sample)
    for c in range(8):
        b, hf = c // 2, c % 2
        yT = np.asarray(res.results[c]["yT"], np.float32)
        y = yT.transpose(0, 3, 2, 1).reshape(TOWN, D)
        y_prompt[b, hf * 2048:(hf + 1) * 2048] = y[0:2048]
        y_sample[b, hf * 1024:(hf + 1) * 1024] = y[2048:3072]
    return (y_prompt, y_sample)
```

```python
import numpy as np
import ml_dtypes
from contextlib import ExitStack
import concourse.bass as bass
import concourse.mybir as mybir
from concourse.bass_utils import run_bass_kernel_spmd

F32 = mybir.dt.float32
BF16 = mybir.dt.bfloat16
AF = mybir.ActivationFunctionType
ALU = mybir.AluOpType
AX = mybir.AxisListType
BF = ml_dtypes.bfloat16

D = 4096
EPS = 1e-6
NPO = 6
NPA = 12
TOWN = 3072
TALL = 6144
ATT_SCALE = 128 ** -0.5
CA_SCALE = 256 ** -0.5


class Obj:
    _n = 0

    def __init__(self, name=""):
        Obj._n += 1
        self.id = Obj._n
        self.name = name
        self.w = {}
        self.r = {}


class Buf:
    def __init__(self, nc, st, name, shape, dtype):
        self.t = st.enter_context(nc.sbuf_tensor(name, list(shape), dtype))
        self.o = Obj(name)


class Ring:
    def __init__(self, items):
        self.items = items
        self.i = 0

    def next(self):
        it = self.items[self.i % len(self.items)]
        self.i += 1
        return it


class Sched:
    ENG = ("pe", "act", "dve", "pool", "sp")
    BLK = {"pe": "tensor", "act": "scalar", "dve": "vector", "pool": "gpsimd", "sp": "sync"}

    def __init__(self, nc, semstack):
        self.nc = nc
        self.semstack = semstack
        self.sems = {}
        self.ops = {e: [] for e in self.ENG}
        self.cnt = {}
        self.known = {e: {} for e in self.ENG}
        self.unsig = {e: False for e in self.ENG}
        self.nops = 0
        self.bufphys = {}
        self.free_phys = []
        self.nphys = 0

    def _collect(self, reads, writes, mykey, dma):
        d = {}
        for o in reads:
            for k, v in o.w.items():
                if v > d.get(k, 0):
                    d[k] = v
        for o in writes:
            for k, v in o.w.items():
                if dma and k == mykey:
                    continue
                if v > d.get(k, 0):
                    d[k] = v
            for k, v in o.r.items():
                if v > d.get(k, 0):
                    d[k] = v
        return d

    def _emit_waits(self, eng, deps, skipkey=None):
        kn = self.known[eng]
        lst = self.ops[eng]
        for k, v in deps.items():
            if k == skipkey:
                continue
            if kn.get(k, 0) >= v:
                continue
            lst.append((0, k, v))
            kn[k] = v

    def _record(self, reads, writes, key, c, merge=False):
        for o in reads:
            if o.r.get(key, 0) < c:
                o.r[key] = c
        for o in writes:
            if merge and key in o.w:
                o.w[key] = c
            else:
                o.w = {key: c}
            o.r = {}

    def op(self, eng, name, kw, reads=(), writes=(), signal=True, nosame=False):
        key = ("e", eng)
        deps = self._collect(reads, writes, key, False)
        self._emit_waits(eng, deps, skipkey=key if (eng == "pe" or nosame) else None)
        c = self.cnt.get(key, 0) + 1
        if signal:
            self.cnt[key] = c
            self.ops[eng].append((1, name, kw, key))
            self.unsig[eng] = False
        else:
            self.ops[eng].append((1, name, kw, None))
            self.unsig[eng] = True
        self._record(reads, writes, key, c)
        self.nops += 1

    def dma(self, q, out_ap, in_ap, semobj, reads=(), writes=()):
        oid = getattr(semobj, "o", semobj).id
        if oid not in self.bufphys:
            if self.free_phys:
                self.bufphys[oid] = self.free_phys.pop()
            else:
                self.bufphys[oid] = self.nphys
                self.nphys += 1
        key = ("b", self.bufphys[oid])
        deps = self._collect(reads, writes, key, True)
        self._emit_waits(q, deps)
        c = self.cnt.get(key, 0) + 16
        self.cnt[key] = c
        self.ops[q].append((2, out_ap, in_ap, key))
        self._record(reads, writes, key, c, merge=True)
        self.nops += 1

    def barrier(self):
        for e in self.ENG:
            assert not self.unsig[e], e
            self._emit_waits(e, dict(self.cnt))

    def flush(self, st):
        nc = self.nc
        for k in self.cnt:
            if k not in self.sems:
                self.sems[k] = self.semstack.enter_context(nc.semaphore("s%d" % len(self.sems)))
        sems = self.sems
        block = st.enter_context(nc.Block())
        for e in self.ENG:
            lst = self.ops[e]
            if not lst:
                continue

            def body(engine, lst=lst):
                for it in lst:
                    if it[0] == 0:
                        engine.wait_ge(sems[it[1]], it[2])
                    elif it[0] == 1:
                        ins = getattr(engine, it[1])(**it[2])
                        if it[3] is not None:
                            ins.then_inc(sems[it[3]], 1)
                    else:
                        engine.dma_start(out=it[1], in_=it[2]).then_inc(sems[it[3]], 16)

            getattr(block, self.BLK[e])(body)
        self.ops = {e: [] for e in self.ENG}
        self.free_phys = list(range(self.nphys))
        self.bufphys = {}


def build(stop_after=99, debug_out=()):
    nc = bass.Bass("TRN2", target_bir_lowering=False)

    def inp(name, shape, dt=F32):
        return nc.dram_tensor(name, list(shape), dt, kind="ExternalInput").ap()

    def scr(name, shape, dt=BF16):
        kind = "ExternalOutput" if name in debug_out else "Internal"
        return nc.dram_tensor(name, list(shape), dt, kind=kind).ap()

    xT = inp("xT", [NPA, 128, 32, 512])
    memT = inp("memT", [128, 32, 512])
    w_in = inp("w_in", [10, 128, 32, 512])
    w_attn = inp("w_attn", [16, 128, 16, 256])
    w_four = inp("w_four", [16, 128, 16, 256])
    w_gate = inp("w_gate", [32, 128, 32, 256])
    w_out = inp("w_out", [8, 128, 32, 512])
    w_cq = inp("w_cq", [2, 128, 32, 512])
    w_ckv = inp("w_ckv", [4, 128, 32, 512])
    w_co = inp("w_co", [8, 128, 8, 512])
    w_pq = inp("w_pq", [4, 128, 32, 512])
    uT = inp("uT", [32, 128, 32, 512])
    ev = inp("ev", [8, 4, 128, 32, 512])
    g_mix = inp("g_mix", [128, 32])
    g_ca = inp("g_ca", [128, 32])
    g_mem = inp("g_mem", [128, 32])
    g_ffn = inp("g_ffn", [128, 32])
    g_fin = inp("g_fin", [128, 32])
    qn = inp("qn", [128, 1])
    kn = inp("kn", [128, 1])
    bg = inp("bg", [128, 64])
    subkT = inp("subkT", [128, 2, 128])
    cosT = inp("cosT", [128, TALL])
    sinT = inp("sinT", [128, TALL])
    rmat = inp("rmat", [128, 128], BF16)
    ones_d = inp("ones", [128, 128], BF16)
    ident = inp("ident", [128, 128], BF16)
    ccs = inp("ccs", [8, 128, 16, 512], BF16)
    csp = inp("csp", [2, 4, 128, 32, 512], BF16)
    css = inp("css", [2, 128, 32, 512], BF16)
    yT = nc.dram_tensor("yT", [NPO, 128, 32, 512], F32, kind="ExternalOutput").ap()

    HT = scr("HT", [NPA, 128, 32, 512])
    QT = scr("QT", [16, 128, TOWN])
    KT = scr("KT", [4, 128, TALL])
    VS = scr("VS", [TALL, 512])
    FT = scr("FT", [NPA, 128, 16, 512])
    PQ = scr("PQ", [4, 2, TALL, 512])
    ZT = scr("ZT", [NPO, 128, 16, 512])
    AT = scr("AT", [NPO, 128, 16, 512])
    MT = scr("MT", [NPO, 128, 32, 512])
    X1T = scr("X1T", [NPO, 128, 32, 512], F32)
    HCT = scr("HCT", [NPO, 128, 32, 512])
    MNT = scr("MNT", [1, 128, 32, 512])
    QCT = scr("QCT", [8, 128, TOWN])
    KCT = scr("KCT", [8, 128, 512])
    VC = scr("VC", [512, 1024])
    OCT = scr("OCT", [NPO, 128, 8, 512])
    X2T = scr("X2T", [NPO, 128, 32, 512], F32)
    HPT = scr("HPT", [NPO, 128, 32, 512])
    QPT = scr("QPT", [16, 128, TOWN])
    WTp = scr("WTp", [NPO, 4, 128, 32, 512])
    GAT = scr("GAT", [NPO, 4, 128, 32, 512])
    X3T = scr("X3T", [NPO, 128, 32, 512], F32)

    outer = ExitStack()
    S = Sched(nc, outer)
    cnt = [0]

    def mkbuf(st, shape, dt, name="b"):
        cnt[0] += 1
        return Buf(nc, st, "%s%d" % (name, cnt[0]), shape, dt)

    def mkps(st, n, dt=F32, cols=512):
        out = []
        for i in range(n):
            cnt[0] += 1
            t = st.enter_context(nc.psum_tensor("ps%d" % cnt[0], [128, cols], dt))
            out.append((t, Obj("ps")))
        return out

    def mm(ps, po, pairs, reads, start=True, stop=True):
        n = len(pairs)
        for i, (l, r) in enumerate(pairs):
            last = i == n - 1
            S.op("pe", "matmul", dict(out=ps, lhsT=l, rhs=r, start=(start and i == 0), stop=(stop and last)),
                 reads=reads, writes=[po], signal=last)

    def load_w(buf, wpanel, kc):
        step = 8 if kc >= 8 else kc
        for c0 in range(0, kc, step):
            S.dma("pool", buf.t[:, c0:c0 + step, :], wpanel[:, c0:c0 + step, :], buf, writes=[buf.o])

    def consts(st, need_ones=True):
        ON = mkbuf(st, [128, 128], BF16, "ones")
        S.dma("sp", ON.t[:], ones_d, ON, writes=[ON.o])
        EP = mkbuf(st, [128, 1], F32, "eps")
        S.op("dve", "memset", dict(ap=EP.t[:], constant=EPS), writes=[EP.o])
        return ON, EP

    phase_idx = [0]

    def run_phase(fn):
        phase_idx[0] += 1
        if phase_idx[0] > stop_after:
            return
        with ExitStack() as st:
            fn(st)
            S.barrier()
            S.flush(st)

    def norm_phase(srcs, gain_ap, dsts, out_dt):
        def fn(st):
            ON, EP = consts(st)
            Xr = Ring([mkbuf(st, [128, 32, 256], F32, "nx") for _ in range(2)])
            SQr = Ring([mkbuf(st, [128, 32, 256], BF16, "nsq") for _ in range(2)])
            Or = Ring([mkbuf(st, [128, 32, 256], out_dt, "no") for _ in range(2)])
            RSr = Ring([mkbuf(st, [128, 256], F32, "nrs") for _ in range(2)])
            G = mkbuf(st, [128, 32], F32, "ng")
            PSr = Ring(mkps(st, 2))
            S.dma("sp", G.t[:], gain_ap, G, writes=[G.o])
            for src, dst in zip(srcs, dsts):
                for hf in range(2):
                    cs = slice(hf * 256, (hf + 1) * 256)
                    X, SQ, O, RS = Xr.next(), SQr.next(), Or.next(), RSr.next()
                    ps, po = PSr.next()
                    S.dma("sp", X.t[:], src[:, :, cs], X, writes=[X.o])
                    for q2 in range(2):
                        S.op("act", "activation", dict(out=SQ.t[:, q2 * 16:(q2 + 1) * 16, :], in_=X.t[:, q2 * 16:(q2 + 1) * 16, :],
                                                       func=AF.Square), reads=[X.o], writes=[SQ.o])
                    mm(ps[:, 0:256], po, [(ON.t[:], SQ.t[:, c, :]) for c in range(32)], reads=[ON.o, SQ.o])
                    S.op("act", "activation", dict(out=RS.t[:], in_=ps[:, 0:256], func=AF.Sqrt, bias=EP.t[:, 0:1], scale=1.0 / D),
                         reads=[po, EP.o], writes=[RS.o])
                    S.op("dve", "reciprocal", dict(out=RS.t[:], in_=RS.t[:]), reads=[RS.o], writes=[RS.o])
                    for q4 in range(4):
                        c8 = slice(q4 * 8, (q4 + 1) * 8)
                        S.op("pool", "tensor_tensor",
                             dict(out=X.t[:, c8, :], in0=X.t[:, c8, :],
                                  in1=G.t[:, c8].unsqueeze(2).broadcast_to([128, 8, 256]), op=ALU.mult),
                             reads=[X.o, G.o], writes=[X.o], nosame=(q4 > 0))
                    for q4 in range(4):
                        c8 = slice(q4 * 8, (q4 + 1) * 8)
                        S.op("dve", "tensor_tensor",
                             dict(out=O.t[:, c8, :], in0=X.t[:, c8, :],
                                  in1=RS.t[:].unsqueeze(1).broadcast_to([128, 8, 256]), op=ALU.mult),
                             reads=[X.o, RS.o], writes=[O.o], nosame=(q4 > 0))
                    S.dma("act", dst[:, :, cs], O.t[:], O, reads=[O.o])
        return fn

    def p2(st):
        ON, EP = consts(st)
        A = [mkbuf(st, [128, 32, 512], BF16, "A") for _ in range(2)]
        HB = Ring([mkbuf(st, [128, 32, 512], BF16, "HB") for _ in range(2)])
        CS = Ring([mkbuf(st, [128, 2, 512], F32, "CS") for _ in range(2)])
        RM = mkbuf(st, [128, 128], BF16, "RM")
        QN = mkbuf(st, [128, 1], F32, "QN")
        KN = mkbuf(st, [128, 1], F32, "KN")
        S.dma("sp", RM.t[:], rmat, RM, writes=[RM.o])
        S.dma("sp", QN.t[:], qn, QN, writes=[QN.o])
        S.dma("sp", KN.t[:], kn, KN, writes=[KN.o])
        EV = Ring([mkbuf(st, [128, 512], BF16, "EV") for _ in range(3)])
        XQ = Ring([mkbuf(st, [128, 512], F32, "XQ") for _ in range(4)])
        SQ = Ring([mkbuf(st, [128, 512], BF16, "SQ") for _ in range(4)])
        RS = Ring([mkbuf(st, [128, 512], F32, "RS") for _ in range(2)])
        XN = Ring([mkbuf(st, [128, 512], BF16, "XN") for _ in range(4)])
        T1 = Ring([mkbuf(st, [128, 512], F32, "T1") for _ in range(2)])
        T2 = Ring([mkbuf(st, [128, 512], F32, "T2") for _ in range(2)])
        OB = Ring([mkbuf(st, [128, 512], BF16, "OB") for _ in range(3)])
        pss = mkps(st, 8)
        PSM = Ring(pss[0:4])
        PS2 = Ring(pss[4:6])
        PS3 = Ring(pss[6:8])

        def stage_a(job):
            ps, po = job["ps"], job["po"]
            xq, sq = XQ.next(), SQ.next()
            S.op("act", "activation", dict(out=xq.t[:], in_=ps[:], func=AF.Copy), reads=[po], writes=[xq.o])
            S.op("act", "activation", dict(out=sq.t[:], in_=ps[:], func=AF.Square), reads=[po], writes=[sq.o])
            job["xq"], job["sq"] = xq, sq

        def stage_b(job):
            xq, sq, gain = job["xq"], job["sq"], job["gain"]
            ps2, po2 = PS2.next()
            mm(ps2[:], po2, [(ON.t[:], sq.t[:])], reads=[ON.o, sq.o])
            rs, xn = RS.next(), XN.next()
            S.op("act", "activation", dict(out=rs.t[:], in_=ps2[:], func=AF.Sqrt, bias=EP.t[:, 0:1], scale=1.0 / 128),
                 reads=[po2, EP.o], writes=[rs.o])
            S.op("dve", "reciprocal", dict(out=rs.t[:], in_=rs.t[:]), reads=[rs.o], writes=[rs.o])
            S.op("dve", "scalar_tensor_tensor", dict(out=xn.t[:], in0=xq.t[:], scalar=gain.t[:, 0:1], in1=rs.t[:],
                                                     op0=ALU.mult, op1=ALU.mult),
                 reads=[xq.o, gain.o, rs.o], writes=[xn.o])
            job["xn"] = xn

        def stage_c(job):
            xn, CSb, dst = job["xn"], job["cs"], job["dst"]
            ps3, po3 = PS3.next()
            mm(ps3[:], po3, [(RM.t[:], xn.t[:])], reads=[RM.o, xn.o])
            t1, t2, ob = T1.next(), T2.next(), OB.next()
            S.op("dve", "tensor_tensor", dict(out=t1.t[:], in0=xn.t[:], in1=CSb.t[:, 0, :], op=ALU.mult),
                 reads=[xn.o, CSb.o], writes=[t1.o])
            S.op("dve", "tensor_tensor", dict(out=t2.t[:], in0=ps3[:], in1=CSb.t[:, 1, :], op=ALU.mult),
                 reads=[po3, CSb.o], writes=[t2.o])
            S.op("pool", "tensor_tensor", dict(out=ob.t[:], in0=t1.t[:], in1=t2.t[:], op=ALU.add),
                 reads=[t1.o, t2.o], writes=[ob.o])
            S.dma("pool", dst, ob.t[:], ob, reads=[ob.o])

        pend = []

        def advance(newjob):
            for job in list(pend):
                job["age"] += 1
                if job["age"] == 1:
                    stage_b(job)
                elif job["age"] == 2:
                    stage_c(job)
                    pend.remove(job)
            if newjob is not None:
                stage_a(newjob)
                newjob["age"] = 0
                pend.append(newjob)

        load_w(A[0], w_in[0], 32)
        for ap in range(10):
            kind = "q" if ap < 4 else "k" if ap == 4 else "v" if ap == 5 else "f"
            Ab = A[ap % 2]
            if ap + 1 < 10:
                load_w(A[(ap + 1) % 2], w_in[ap + 1], 32)
            nbs = range(NPO) if kind == "q" else range(NPA)
            for nb in nbs:
                Hb = HB.next()
                S.dma("sp", Hb.t[:], HT[nb], Hb, writes=[Hb.o])
                if kind in "qk":
                    CSb = CS.next()
                    S.dma("sp", CSb.t[:, 0, :], cosT[:, nb * 512:(nb + 1) * 512], CSb, writes=[CSb.o])
                    S.dma("sp", CSb.t[:, 1, :], sinT[:, nb * 512:(nb + 1) * 512], CSb, writes=[CSb.o])
                for m in range(4):
                    ps, po = PSM.next()
                    if kind == "v":
                        mm(ps[:], po, [(Hb.t[:, c, m * 128:(m + 1) * 128], Ab.t[:, c, :]) for c in range(32)],
                           reads=[Hb.o, Ab.o])
                        advance(None)
                        Eb = EV.next()
                        S.op("act", "activation", dict(out=Eb.t[:], in_=ps[:], func=AF.Copy), reads=[po], writes=[Eb.o])
                        S.dma("pool", VS[nb * 512 + m * 128:nb * 512 + (m + 1) * 128, :], Eb.t[:], Eb, reads=[Eb.o])
                        continue
                    mm(ps[:], po, [(Ab.t[:, c, m * 128:(m + 1) * 128], Hb.t[:, c, :]) for c in range(32)],
                       reads=[Hb.o, Ab.o])
                    if kind == "f":
                        advance(None)
                        Eb = EV.next()
                        S.op("act", "activation", dict(out=Eb.t[:], in_=ps[:], func=AF.Copy), reads=[po], writes=[Eb.o])
                        S.dma("pool", FT[nb, :, (ap - 6) * 4 + m, :], Eb.t[:], Eb, reads=[Eb.o])
                        continue
                    gain = QN if kind == "q" else KN
                    dst = QT[ap * 4 + m, :, nb * 512:(nb + 1) * 512] if kind == "q" else KT[m, :, nb * 512:(nb + 1) * 512]
                    advance(dict(ps=ps, po=po, gain=gain, dst=dst, cs=CSb))
        while pend:
            advance(None)

    def p3(st):
        Bp = [mkbuf(st, [128, 16, 512], BF16, "Bp") for _ in range(2)]
        Fb = Ring([mkbuf(st, [128, 16, 512], BF16, "Fb") for _ in range(2)])
        EV = Ring([mkbuf(st, [128, 512], BF16, "EV") for _ in range(4)])
        PSM = Ring(mkps(st, 4))
        k = 0
        for pi in range(8):
            pq, cb = pi // 4, pi % 4
            B_ = Bp[pi % 2]
            S.dma("sp", B_.t[:], ccs[pi], B_, writes=[B_.o])
            for nb in range(NPA):
                F_ = Fb.next()
                S.dma("sp", F_.t[:], FT[nb], F_, writes=[F_.o])
                for m in range(4):
                    ps, po = PSM.next()
                    mm(ps[:], po, [(F_.t[:, c, m * 128:(m + 1) * 128], B_.t[:, c, :]) for c in range(16)],
                       reads=[F_.o, B_.o])
                    Eb = EV.next()
                    k += 1
                    if k % 2:
                        S.op("act", "activation", dict(out=Eb.t[:], in_=ps[:], func=AF.Copy), reads=[po], writes=[Eb.o])
                    else:
                        S.op("dve", "tensor_copy", dict(out=Eb.t[:], in_=ps[:]), reads=[po], writes=[Eb.o])
                    r0 = nb * 512 + m * 128
                    S.dma("pool", PQ[cb, pq, r0:r0 + 128, :], Eb.t[:], Eb, reads=[Eb.o])

    def p4(st):
        Aa = [mkbuf(st, [128, 32, 512], BF16, "Aa") for _ in range(2)]
        Bb = Ring([mkbuf(st, [128, 32, 512], BF16, "Bb") for _ in range(3)])
        EV = Ring([mkbuf(st, [128, 512], BF16, "EV") for _ in range(4)])
        pss = mkps(st, 8)
        GR = Ring([pss[0:4], pss[4:8]])
        k = 0

        def rows(cb, pq, r0, n):
            return PQ[cb, pq, r0:r0 + n, :].rearrange("(c p) n -> p c n", p=128)

        def epi(grp, nbp, cb):
            nonlocal k
            for m in range(4):
                ps, po = grp[m]
                Eb = EV.next()
                k += 1
                if k % 2:
                    S.op("act", "activation", dict(out=Eb.t[:], in_=ps[:], func=AF.Copy), reads=[po], writes=[Eb.o])
                else:
                    S.op("dve", "tensor_copy", dict(out=Eb.t[:], in_=ps[:]), reads=[po], writes=[Eb.o])
                S.dma("pool", ZT[nbp, :, cb * 4 + m, :], Eb.t[:], Eb, reads=[Eb.o])

        for cb in range(4):
            for kp in range(2):
                S.dma("sp", Aa[kp].t[:, 0:16, :], rows(cb, kp, 0, 2048), Aa[kp], writes=[Aa[kp].o])
                S.dma("sp", Aa[kp].t[:, 16:32, :], rows(cb, kp, 3072, 2048), Aa[kp], writes=[Aa[kp].o])
            for nb in range(4):
                grp = GR.next()
                for kp in range(2):
                    Bk = Bb.next()
                    S.dma("sp", Bk.t[:], csp[kp, nb], Bk, writes=[Bk.o])
                    for m in range(4):
                        ps, po = grp[m]
                        mm(ps[:], po, [(Aa[kp].t[:, c, m * 128:(m + 1) * 128], Bk.t[:, c, :]) for c in range(32)],
                           reads=[Aa[kp].o, Bk.o], start=(kp == 0), stop=(kp == 1))
                epi(grp, nb, cb)
        for cb in range(4):
            A0 = Aa[cb % 2]
            S.dma("sp", A0.t[:, 0:8, :], rows(cb, 0, 2048, 1024), A0, writes=[A0.o])
            S.dma("sp", A0.t[:, 8:16, :], rows(cb, 0, 5120, 1024), A0, writes=[A0.o])
            S.dma("sp", A0.t[:, 16:24, :], rows(cb, 1, 2048, 1024), A0, writes=[A0.o])
            S.dma("sp", A0.t[:, 24:32, :], rows(cb, 1, 5120, 1024), A0, writes=[A0.o])
            for nb in range(2):
                grp = GR.next()
                Bk = Bb.next()
                S.dma("sp", Bk.t[:], css[nb], Bk, writes=[Bk.o])
                for m in range(4):
                    ps, po = grp[m]
                    mm(ps[:], po, [(A0.t[:, c, m * 128:(m + 1) * 128], Bk.t[:, c, :]) for c in range(32)],
                       reads=[A0.o, Bk.o])
                epi(grp, 4 + nb, cb)

    def p5(st):
        ON, EP = consts(st)
        KTg = mkbuf(st, [128, 4096], BF16, "KTg")
        Vg = mkbuf(st, [128, 32, 128], BF16, "Vg")
        QG = mkbuf(st, [128, 4, 2048], BF16, "QG")
        ER = Ring([mkbuf(st, [128, 512], BF16, "E") for _ in range(3)])
        RZ = Ring([mkbuf(st, [128, 512], F32, "RZ") for _ in range(2)])
        OB = Ring([mkbuf(st, [128, 4, 128], BF16, "OB") for _ in range(2)])
        pss = mkps(st, 6)
        PSS = Ring(pss[0:2])
        PSO = Ring(pss[2:4])
        PSZ = Ring(pss[4:6])
        seqs = [((0, 2048), (3072, 5120), 0, 2048, 0), ((2048, 3072), (5120, 6144), 2048, 1024, 4)]
        for (o0, o1), (t0, t1), q0, nq, pan0 in seqs:
            hk = o1 - o0
            nch = 2 * hk // 128
            for g in range(4):
                S.dma("sp", KTg.t[:, 0:hk], KT[g, :, o0:o1], KTg, writes=[KTg.o])
                S.dma("sp", KTg.t[:, hk:2 * hk], KT[g, :, t0:t1], KTg, writes=[KTg.o])
                S.dma("sp", Vg.t[:, 0:nch // 2, :], VS[o0:o1, g * 128:(g + 1) * 128].rearrange("(c p) d -> p c d", p=128),
                      Vg, writes=[Vg.o])
                S.dma("sp", Vg.t[:, nch // 2:nch, :], VS[t0:t1, g * 128:(g + 1) * 128].rearrange("(c p) d -> p c d", p=128),
                      Vg, writes=[Vg.o])
                S.dma("sp", QG.t[:, :, 0:nq], QT[g * 4:(g + 1) * 4, :, q0:q0 + nq].rearrange("h d q -> d h q"),
                      QG, writes=[QG.o])
                for qb in range(nq // 128):
                    rhsq = QG.t[:, :, qb * 128:(qb + 1) * 128]
                    pso, poo = PSO.next()
                    psz, poz = PSZ.next()

                    def issue_s(sc):
                        ps, po = PSS.next()
                        mm(ps[:], po, [(KTg.t[:, sc * 128:(sc + 1) * 128], rhsq)], reads=[KTg.o, QG.o])
                        Eb = ER.next()
                        S.op("act", "activation", dict(out=Eb.t[:], in_=ps[:], func=AF.Exp, scale=ATT_SCALE),
                             reads=[po], writes=[Eb.o])
                        return Eb

                    Ecur = issue_s(0)
                    for sc in range(nch):
                        Enext = issue_s(sc + 1) if sc + 1 < nch else None
                        mm(pso[:], poo, [(Vg.t[:, sc, :], Ecur.t[:])], reads=[Vg.o, Ecur.o],
                           start=(sc == 0), stop=(sc == nch - 1))
                        mm(psz[:], poz, [(ON.t[:], Ecur.t[:])], reads=[ON.o, Ecur.o],
                           start=(sc == 0), stop=(sc == nch - 1))
                        Ecur = Enext
                    rz, ob = RZ.next(), OB.next()
                    S.op("dve", "reciprocal", dict(out=rz.t[:], in_=psz[:]), reads=[poz], writes=[rz.o])
                    S.op("dve", "tensor_tensor", dict(out=ob.t[:].rearrange("p h q -> p (h q)"), in0=pso[:], in1=rz.t[:],
                                                      op=ALU.mult), reads=[poo, rz.o], writes=[ob.o])
                    tq = qb * 128
                    S.dma("pool", AT[pan0 + tq // 512, :, g * 4:(g + 1) * 4, tq % 512:tq % 512 + 128], ob.t[:], ob,
                          reads=[ob.o])

    def p6(st):
        Wa = mkbuf(st, [128, 16, 256], BF16, "Wa")
        Wf = mkbuf(st, [128, 16, 256], BF16, "Wf")
        Wg0 = mkbuf(st, [128, 32, 256], BF16, "Wg0")
        Wg1 = mkbuf(st, [128, 32, 256], BF16, "Wg1")
        ATb = Ring([mkbuf(st, [128, 16, 512], BF16, "ATb") for _ in range(2)])
        ZTb = Ring([mkbuf(st, [128, 16, 512], BF16, "ZTb") for _ in range(2)])
        HBb = Ring([mkbuf(st, [128, 32, 512], BF16, "HBb") for _ in range(2)])
        BG = mkbuf(st, [128, 64], F32, "BG")
        S.dma("sp", BG.t[:], bg, BG, writes=[BG.o])
        S0 = Ring([mkbuf(st, [128, 512], F32, "S0") for _ in range(2)])
        S1 = Ring([mkbuf(st, [128, 512], F32, "S1") for _ in range(2)])
        MO = Ring([mkbuf(st, [128, 512], BF16, "MO") for _ in range(2)])
        pss = mkps(st, 8)
        GR = Ring([pss[0:4], pss[4:8]])
        for mb2 in range(16):
            load_w(Wa, w_attn[mb2], 16)
            load_w(Wf, w_four[mb2], 16)
            load_w(Wg0, w_gate[mb2], 32)
            load_w(Wg1, w_gate[16 + mb2], 32)
            for nb in range(NPO):
                a_, z_, h_ = ATb.next(), ZTb.next(), HBb.next()
                S.dma("sp", a_.t[:], AT[nb], a_, writes=[a_.o])
                S.dma("sp", z_.t[:], ZT[nb], z_, writes=[z_.o])
                S.dma("sp", h_.t[:], HT[nb], h_, writes=[h_.o])
                for mi in range(2):
                    m = mb2 * 2 + mi
                    (pa, oa), (pf, of), (pg0, og0), (pg1, og1) = GR.next()
                    cs = slice(mi * 128, (mi + 1) * 128)
                    mm(pa[:], oa, [(Wa.t[:, c, cs], a_.t[:, c, :]) for c in range(16)], reads=[Wa.o, a_.o])
                    mm(pf[:], of, [(Wf.t[:, c, cs], z_.t[:, c, :]) for c in range(16)], reads=[Wf.o, z_.o])
                    mm(pg0[:], og0, [(Wg0.t[:, c, cs], h_.t[:, c, :]) for c in range(32)], reads=[Wg0.o, h_.o])
                    mm(pg1[:], og1, [(Wg1.t[:, c, cs], h_.t[:, c, :]) for c in range(32)], reads=[Wg1.o, h_.o])
                    s0, s1, mo = S0.next(), S1.next(), MO.next()
                    t0, t1 = s0, s1
                    S.op("act", "activation", dict(out=s0.t[:], in_=pg0[:], func=AF.Sigmoid, bias=BG.t[:, m:m + 1], scale=1.0),
                         reads=[og0, BG.o], writes=[s0.o])
                    S.op("act", "activation", dict(out=s1.t[:], in_=pg1[:], func=AF.Sigmoid, bias=BG.t[:, 32 + m:33 + m], scale=1.0),
                         reads=[og1, BG.o], writes=[s1.o])
                    S.op("dve", "tensor_tensor", dict(out=t0.t[:], in0=pa[:], in1=s0.t[:], op=ALU.mult),
                         reads=[oa, s0.o], writes=[t0.o])
                    S.op("dve", "tensor_tensor", dict(out=t1.t[:], in0=pf[:], in1=s1.t[:], op=ALU.mult),
                         reads=[of, s1.o], writes=[t1.o])
                    S.op("pool", "tensor_tensor", dict(out=mo.t[:], in0=t0.t[:], in1=t1.t[:], op=ALU.add),
                         reads=[t0.o, t1.o], writes=[mo.o])
                    S.dma("pool", MT[nb, :, m, :], mo.t[:], mo, reads=[mo.o])

    def resid_gemm(W, kcw, Bsrc, resid, dst):
        def fn(st):
            A = [mkbuf(st, [128, kcw, 512], BF16, "A") for _ in range(2)]
            Bb = Ring([mkbuf(st, [128, kcw, 512], BF16, "B") for _ in range(2)])
            XR = Ring([mkbuf(st, [128, 512], F32, "XR") for _ in range(3)])
            XO = Ring([mkbuf(st, [128, 512], F32, "XO") for _ in range(3)])
            PSM = Ring(mkps(st, 4))
            load_w(A[0], W[0], kcw)
            for ap in range(8):
                Ab = A[ap % 2]
                if ap + 1 < 8:
                    load_w(A[(ap + 1) % 2], W[ap + 1], kcw)
                for nb in range(NPO):
                    B_ = Bb.next()
                    S.dma("sp", B_.t[:], Bsrc[nb], B_, writes=[B_.o])
                    for mi in range(4):
                        m = ap * 4 + mi
                        ps, po = PSM.next()
                        mm(ps[:], po, [(Ab.t[:, c, mi * 128:(mi + 1) * 128], B_.t[:, c, :]) for c in range(kcw)],
                           reads=[Ab.o, B_.o])
                        xr, xo = XR.next(), XO.next()
                        S.dma("sp", xr.t[:], resid[nb, :, m, :], xr, writes=[xr.o])
                        S.op("dve", "tensor_tensor", dict(out=xo.t[:], in0=ps[:], in1=xr.t[:], op=ALU.add),
                             reads=[po, xr.o], writes=[xo.o])
                        S.dma("pool", dst[nb, :, m, :], xo.t[:], xo, reads=[xo.o])
        return fn

    def proj_fm(W, naps, Bsrc, nbs, dst, woff=0):
        def fn(st):
            A = [mkbuf(st, [128, 32, 512], BF16, "A") for _ in range(2)]
            Bb = Ring([mkbuf(st, [128, 32, 512], BF16, "B") for _ in range(2)])
            EV = Ring([mkbuf(st, [128, 512], BF16, "EV") for _ in range(4)])
            PSM = Ring(mkps(st, 4))
            k = 0
            load_w(A[0], W[woff], 32)
            for ap in range(naps):
                Ab = A[ap % 2]
                if ap + 1 < naps:
                    load_w(A[(ap + 1) % 2], W[woff + ap + 1], 32)
                for nb in range(nbs):
                    B_ = Bb.next()
                    S.dma("sp", B_.t[:], Bsrc[nb], B_, writes=[B_.o])
                    for mi in range(4):
                        ps, po = PSM.next()
                        mm(ps[:], po, [(Ab.t[:, c, mi * 128:(mi + 1) * 128], B_.t[:, c, :]) for c in range(32)],
                           reads=[Ab.o, B_.o])
                        Eb = EV.next()
                        k += 1
                        if k % 2:
                            S.op("act", "activation", dict(out=Eb.t[:], in_=ps[:], func=AF.Copy), reads=[po], writes=[Eb.o])
                        else:
                            S.op("dve", "tensor_copy", dict(out=Eb.t[:], in_=ps[:]), reads=[po], writes=[Eb.o])
                        S.dma("pool", dst[ap * 4 + mi, :, nb * 512:(nb + 1) * 512], Eb.t[:], Eb, reads=[Eb.o])
        return fn

    def p8v(st):
        A = [mkbuf(st, [128, 32, 512], BF16, "A") for _ in range(2)]
        B_ = mkbuf(st, [128, 32, 512], BF16, "B")
        EV = Ring([mkbuf(st, [128, 512], BF16, "EV") for _ in range(4)])
        PSM = Ring(mkps(st, 4))
        S.dma("sp", B_.t[:], MNT[0], B_, writes=[B_.o])
        for ap in range(2):
            load_w(A[ap], w_ckv[2 + ap], 32)
        for ap in range(2):
            for mi in range(4):
                ps, po = PSM.next()
                mm(ps[:], po, [(B_.t[:, c, mi * 128:(mi + 1) * 128], A[ap].t[:, c, :]) for c in range(32)],
                   reads=[A[ap].o, B_.o])
                Eb = EV.next()
                S.op("act", "activation", dict(out=Eb.t[:], in_=ps[:], func=AF.Copy), reads=[po], writes=[Eb.o])
                S.dma("pool", VC[mi * 128:(mi + 1) * 128, ap * 512:(ap + 1) * 512], Eb.t[:], Eb, reads=[Eb.o])

    def p8c(st):
        ON, EP = consts(st)
        KCb = mkbuf(st, [128, 8, 512], BF16, "KCb")
        QCb = mkbuf(st, [128, 8, TOWN], BF16, "QCb")
        VCb = mkbuf(st, [128, 4, 1024], BF16, "VCb")
        S.dma("sp", KCb.t[:], KCT.rearrange("b d m -> d b m"), KCb, writes=[KCb.o])
        S.dma("sp", QCb.t[:], QCT.rearrange("b d t -> d b t"), QCb, writes=[QCb.o])
        S.dma("sp", VCb.t[:], VC.rearrange("(c p) n -> p c n", p=128), VCb, writes=[VCb.o])
        ER = Ring([mkbuf(st, [128, 512], BF16, "E") for _ in range(4)])
        RZ = Ring([mkbuf(st, [128, 512], F32, "RZ") for _ in range(2)])
        OB = Ring([mkbuf(st, [128, 512], BF16, "OB") for _ in range(4)])
        pss = mkps(st, 8)
        PSS = Ring(pss[0:2])
        PSO = Ring(pss[2:6])
        PSZ = Ring(pss[6:8])
        for nb in range(NPO):
            mc0 = 0 if nb < 4 else 2
            for hc in range(4):
                Es = []
                for mc in range(2):
                    ps, po = PSS.next()
                    ms = slice((mc0 + mc) * 128, (mc0 + mc + 1) * 128)
                    mm(ps[:], po, [(KCb.t[:, hc * 2 + db, ms], QCb.t[:, hc * 2 + db, nb * 512:(nb + 1) * 512]) for db in range(2)],
                       reads=[KCb.o, QCb.o])
                    Eb = ER.next()
                    S.op("act", "activation", dict(out=Eb.t[:], in_=ps[:], func=AF.Exp, scale=CA_SCALE), reads=[po], writes=[Eb.o])
                    Es.append(Eb)
                psz, poz = PSZ.next()
                mm(psz[:], poz, [(ON.t[:], Es[mc].t[:]) for mc in range(2)], reads=[ON.o, Es[0].o, Es[1].o])
                rz = RZ.next()
                S.op("dve", "reciprocal", dict(out=rz.t[:], in_=psz[:]), reads=[poz], writes=[rz.o])
                for dvb in range(2):
                    pso, poo = PSO.next()
                    cs = slice(hc * 256 + dvb * 128, hc * 256 + (dvb + 1) * 128)
                    mm(pso[:], poo, [(VCb.t[:, mc0 + mc, cs], Es[mc].t[:]) for mc in range(2)],
                       reads=[VCb.o, Es[0].o, Es[1].o])
                    ob = OB.next()
                    S.op("dve", "tensor_tensor", dict(out=ob.t[:], in0=pso[:], in1=rz.t[:], op=ALU.mult),
                         reads=[poo, rz.o], writes=[ob.o])
                    S.dma("pool", OCT[nb, :, hc * 2 + dvb, :], ob.t[:], ob, reads=[ob.o])

    def p9cd(st):
        SUBK = mkbuf(st, [128, 2, 128], BF16, "SUBK")
        S.dma("pool", SUBK.t[:], subkT, SUBK, writes=[SUBK.o])
        IDB = mkbuf(st, [128, 128], BF16, "IDB")
        S.dma("sp", IDB.t[:], ident, IDB, writes=[IDB.o])
        Q = mkbuf(st, [128, 16, 128], BF16, "Q16")
        SK = mkbuf(st, [128, 16, 128], F32, "SK")
        TOP = mkbuf(st, [128, 16, 16], F32, "TOP")
        B16 = mkbuf(st, [128, 8, 16], F32, "B16")
        JUNK = mkbuf(st, [128, 16], F32, "JUNK")
        NEGM = mkbuf(st, [128, 8], F32, "NEGM")
        ZS = mkbuf(st, [128, 8], F32, "ZS")
        LNZ = mkbuf(st, [128, 8], F32, "LNZ")
        BIAS = mkbuf(st, [128, 8], F32, "BIAS")
        TAUP = mkbuf(st, [128, 8], F32, "TAUP")
        S1B = mkbuf(st, [128, 8, 128], F32, "S1B")
        Xr = Ring([mkbuf(st, [128, 8, 2, 128], F32, "X") for _ in range(3)])
        Er = Ring([mkbuf(st, [128, 8, 256], BF16, "E") for _ in range(6)])
        WTSr = Ring([mkbuf(st, [128, 16, 128], BF16, "WTS") for _ in range(2)])
        pssk = mkps(st, 2)
        pstr = Ring(mkps(st, 2, F32, 256))
        A = [mkbuf(st, [128, 32, 512], BF16, "A") for _ in range(2)]
        Bb = Ring([mkbuf(st, [128, 32, 512], BF16, "B") for _ in range(2)])
        GA = Ring([mkbuf(st, [128, 512], BF16, "GA") for _ in range(4)])
        PSM = Ring(mkps(st, 4))
        LAG = 3

        def gen_c():
            for tt in range(24):
                nb, tsub = tt // 4, tt % 4
                S.dma("sp", Q.t[:], QPT[:, :, tt * 128:(tt + 1) * 128].rearrange("b d t -> d b t"), Q, writes=[Q.o])
                for half in range(2):
                    for h8 in range(8):
                        hc = half * 8 + h8
                        ps, po = pssk[h8 // 4]
                        mm(ps[:, (h8 % 4) * 128:(h8 % 4 + 1) * 128], po, [(Q.t[:, hc, :], SUBK.t[:, hc % 2, :])],
                           reads=[Q.o, SUBK.o])
                    for bk in range(2):
                        ps, po = pssk[bk]
                        c0 = half * 8 + bk * 4
                        S.op("act", "activation", dict(out=SK.t[:, c0:c0 + 4, :].rearrange("p a b -> p (a b)"), in_=ps[:],
                                                       func=AF.Copy), reads=[po], writes=[SK.o])
                yield
                X0, X1 = Xr.items[0], Xr.items[1]
                SKRv = X0.t[:].rearrange("p h i j -> p (h i) j")
                CNv = X1.t[:].rearrange("p h i j -> p h (i j)")
                for hc in range(16):
                    S.op("dve", "max", dict(out=TOP.t[:, hc, 0:8], in_=SK.t[:, hc, :]), reads=[SK.o], writes=[TOP.o], nosame=True)
                for hc in range(16):
                    S.op("dve", "match_replace", dict(out=SKRv[:, hc, :], in_to_replace=TOP.t[:, hc, 0:8], in_values=SK.t[:, hc, :],
                                                      imm_value=-1e30), reads=[SK.o, TOP.o], writes=[X0.o], nosame=(hc > 0))
                yield
                for hc in range(16):
                    S.op("dve", "max", dict(out=TOP.t[:, hc, 8:16], in_=SKRv[:, hc, :]), reads=[X0.o], writes=[TOP.o], nosame=(hc > 0))
                yield
                for hh in range(2):
                    for h4 in range(4):
                        h = hh * 4 + h4
                        a_ = TOP.t[:, 2 * h, :].unsqueeze(2).broadcast_to([128, 16, 16])
                        b_ = TOP.t[:, 2 * h + 1, :].unsqueeze(1).broadcast_to([128, 16, 16])
                        S.op("dve", "tensor_tensor", dict(out=CNv[:, h4, :].rearrange("p (a b) -> p a b", a=16), in0=a_, in1=b_, op=ALU.add),
                             reads=[TOP.o], writes=[X1.o], nosame=(h4 > 0))
                    for h4 in range(4):
                        h = hh * 4 + h4
                        S.op("dve", "max", dict(out=B16.t[:, h, 0:8], in_=CNv[:, h4, :]), reads=[X1.o], writes=[B16.o], nosame=(h4 > 0))
                    for h4 in range(4):
                        h = hh * 4 + h4
                        S.op("dve", "match_replace", dict(out=CNv[:, 4 + h4, :], in_to_replace=B16.t[:, h, 0:8], in_values=CNv[:, h4, :],
                                                          imm_value=-1e30), reads=[X1.o, B16.o], writes=[X1.o], nosame=(h4 > 0))
                    for h4 in range(4):
                        h = hh * 4 + h4
                        S.op("dve", "max", dict(out=B16.t[:, h, 8:16], in_=CNv[:, 4 + h4, :]), reads=[X1.o], writes=[B16.o], nosame=(h4 > 0))
                    yield
                S.op("dve", "tensor_scalar", dict(out=NEGM.t[:], in0=B16.t[:, :, 0], scalar1=-1.0, scalar2=0.0,
                                                  op0=ALU.mult, op1=ALU.add), reads=[B16.o], writes=[NEGM.o])
                S.op("dve", "memset", dict(ap=ZS.t[:], constant=0.0), writes=[ZS.o])
                for h in range(8):
                    S.op("act", "activation", dict(out=JUNK.t[:], in_=B16.t[:, h, :], func=AF.Exp, bias=NEGM.t[:, h:h + 1],
                                                   scale=1.0, accum_out=ZS.t[:, h:h + 1]),
                         reads=[B16.o, NEGM.o], writes=[JUNK.o, ZS.o], nosame=(h > 0))
                S.op("act", "activation", dict(out=LNZ.t[:], in_=ZS.t[:], func=AF.Ln), reads=[ZS.o], writes=[LNZ.o])
                S.op("dve", "tensor_tensor", dict(out=BIAS.t[:], in0=NEGM.t[:], in1=LNZ.t[:], op=ALU.subtract),
                     reads=[NEGM.o, LNZ.o], writes=[BIAS.o])
                S.op("dve", "tensor_tensor", dict(out=TAUP.t[:], in0=B16.t[:, :, 15], in1=BIAS.t[:], op=ALU.add),
                     reads=[B16.o, BIAS.o], writes=[TAUP.o])
                S.op("dve", "tensor_scalar", dict(out=TAUP.t[:], in0=TAUP.t[:], scalar1=1.0, scalar2=-2e-5,
                                                  op0=ALU.mult, op1=ALU.add), reads=[TAUP.o], writes=[TAUP.o])
                skv = SK.t[:].rearrange("p (h c) n -> p h c n", c=2)
                S.op("dve", "tensor_tensor", dict(out=S1B.t[:], in0=skv[:, :, 0, :],
                                                  in1=BIAS.t[:].unsqueeze(2).broadcast_to([128, 8, 128]), op=ALU.add),
                     reads=[SK.o, BIAS.o], writes=[S1B.o])
                s2 = skv[:, :, 1, :]
                yield
                pend = []
                wts = [None]

                def stage2(eb, Eb):
                    if eb % 8 == 0:
                        wts[0] = WTSr.next()
                    WTS = wts[0]
                    pt, pto = pstr.next()
                    for k in range(2):
                        mm(pt[:, k * 128:(k + 1) * 128], pto,
                           [(Eb.t[:, h, k * 128:(k + 1) * 128], IDB.t[:]) for h in range(8)], reads=[Eb.o, IDB.o])
                    cl = (eb % 8) * 2
                    S.op("dve", "tensor_copy", dict(out=WTS.t[:, cl:cl + 2, :].rearrange("p a b -> p (a b)"), in_=pt[:]),
                         reads=[pto], writes=[WTS.o], nosame=True)
                    if eb % 8 == 7:
                        c16 = ((eb // 8) % 2) * 16
                        S.dma("sp", WTp[nb, eb // 16, :, c16:c16 + 16, tsub * 128:(tsub + 1) * 128], WTS.t[:], WTS, reads=[WTS.o])

                for eb in range(64):
                    i0 = eb * 2
                    Xb, Eb = Xr.next(), Er.next()
                    S.op("pool", "tensor_tensor",
                         dict(out=Xb.t[:], in0=S1B.t[:, :, i0:i0 + 2].unsqueeze(3).broadcast_to([128, 8, 2, 128]),
                              in1=s2.unsqueeze(2).broadcast_to([128, 8, 2, 128]), op=ALU.add),
                         reads=[S1B.o, SK.o], writes=[Xb.o])
                    xv = Xb.t[:].rearrange("p h i j -> p h (i j)")
                    S.op("act", "activation", dict(out=Eb.t[:].rearrange("p h e -> p (h e)"),
                                                   in_=Xb.t[:].rearrange("p h i j -> p (h i j)"), func=AF.Exp),
                         reads=[Xb.o], writes=[Eb.o])
                    for h in range(8):
                        S.op("dve", "scalar_tensor_tensor",
                             dict(out=Eb.t[:, h, :], in0=xv[:, h, :], scalar=TAUP.t[:, h:h + 1], in1=Eb.t[:, h, :],
                                  op0=ALU.is_ge, op1=ALU.mult), reads=[Xb.o, TAUP.o, Eb.o], writes=[Eb.o], nosame=(h > 0))
                    pend.append((eb, Eb))
                    if len(pend) > LAG:
                        stage2(*pend.pop(0))
                    if eb % 2 == 1:
                        yield
                while pend:
                    stage2(*pend.pop(0))
                yield

        def gen_d():
            tiles = [(mb, nb, mi) for mb in range(32) for nb in range(NPO) for mi in range(4)]
            info = {}
            load_w(A[0], uT[0], 32)
            B_ = None
            for j in range(len(tiles) + 4):
                if j < len(tiles):
                    mb, nb, mi = tiles[j]
                    Ab = A[mb % 2]
                    if nb == 0 and mi == 0 and mb + 1 < 32:
                        load_w(A[(mb + 1) % 2], uT[mb + 1], 32)
                    if mi == 0:
                        B_ = Bb.next()
                        S.dma("sp", B_.t[:], HPT[nb], B_, writes=[B_.o])
                    ps, po = PSM.next()
                    mm(ps[:], po, [(Ab.t[:, c, mi * 128:(mi + 1) * 128], B_.t[:, c, :]) for c in range(32)],
                       reads=[Ab.o, B_.o])
                    info[j] = (ps, po, nb, mb * 4 + mi)
                if 0 <= j - 3 < len(tiles):
                    ps, po, nb_, ec = info[j - 3]
                    ga = GA.next()
                    S.op("act", "activation", dict(out=ga.t[:], in_=ps[:], func=AF.Gelu), reads=[po], writes=[ga.o])
                    info[j - 3] = (ga, nb_, ec)
                if 0 <= j - 4 < len(tiles):
                    ga, nb_, ec = info.pop(j - 4)
                    S.dma("sp", GAT[nb_, ec // 32, :, ec % 32, :], ga.t[:], ga, reads=[ga.o])
                yield

        gc, gd = gen_c(), gen_d()
        alive = [True, True]
        while alive[0] or alive[1]:
            for i, g in enumerate((gc, gd)):
                for _ in range(4):
                    if alive[i]:
                        try:
                            next(g)
                        except StopIteration:
                            alive[i] = False

    def p9e(st):
        ACC = mkbuf(st, [128, 32, 512], F32, "ACC")
        A = Ring([mkbuf(st, [128, 32, 512], BF16, "A") for _ in range(2)])
        Bg = mkbuf(st, [128, 32, 512], BF16, "Bg")
        Bw = Ring([mkbuf(st, [128, 8, 512], BF16, "Bw") for _ in range(2)])
        PSM = Ring(mkps(st, 8))
        for nb in range(NPO):
            S.dma("sp", ACC.t[:], X2T[nb], ACC, writes=[ACC.o])
            for kp in range(4):
                S.dma("sp", Bg.t[:], GAT[nb, kp], Bg, writes=[Bg.o])
                for q in range(4):
                    bw = Bw.next()
                    c8 = slice(q * 8, (q + 1) * 8)
                    S.dma("sp", bw.t[:], WTp[nb, kp, :, c8, :], bw, writes=[bw.o])
                    S.op("dve", "tensor_tensor", dict(out=Bg.t[:, c8, :], in0=Bg.t[:, c8, :], in1=bw.t[:], op=ALU.mult),
                         reads=[Bg.o, bw.o], writes=[Bg.o])
                for ap in range(8):
                    Ab = A.next()
                    load_w(Ab, ev[ap, kp], 32)
                    for mi in range(4):
                        m = ap * 4 + mi
                        ps, po = PSM.next()
                        mm(ps[:], po, [(Ab.t[:, c, mi * 128:(mi + 1) * 128], Bg.t[:, c, :]) for c in range(32)],
                           reads=[Ab.o, Bg.o])
                        S.op("dve", "tensor_tensor", dict(out=ACC.t[:, m, :], in0=ps[:], in1=ACC.t[:, m, :], op=ALU.add),
                             reads=[po, ACC.o], writes=[ACC.o], nosame=True)
            S.dma("act", X3T[nb], ACC.t[:], ACC, reads=[ACC.o])

    run_phase(norm_phase([xT[j] for j in range(NPA)], g_mix, [HT[j] for j in range(NPA)], BF16))
    run_phase(p2)
    run_phase(p3)
    run_phase(p4)
    run_phase(p5)
    run_phase(p6)
    run_phase(resid_gemm(w_out, 32, MT, xT, X1T))
    run_phase(norm_phase([X1T[j] for j in range(NPO)], g_ca, [HCT[j] for j in range(NPO)], BF16))
    run_phase(norm_phase([memT], g_mem, [MNT[0]], BF16))
    run_phase(proj_fm(w_cq, 2, HCT, NPO, QCT))
    run_phase(proj_fm(w_ckv, 2, MNT, 1, KCT))
    run_phase(p8v)
    run_phase(p8c)
    run_phase(resid_gemm(w_co, 8, OCT, X1T, X2T))
    run_phase(norm_phase([X2T[j] for j in range(NPO)], g_ffn, [HPT[j] for j in range(NPO)], BF16))
    run_phase(proj_fm(w_pq, 4, HPT, NPO, QPT))
    run_phase(p9cd)
    run_phase(p9e)
    run_phase(norm_phase([X3T[j] for j in range(NPO)], g_fin, [yT[j] for j in range(NPO)], F32))
    outer.close()
    return nc, S.nops


def _panels(x2d, kc):
    n = x2d.shape[0] // 512
    return np.ascontiguousarray(x2d.reshape(n, 512, kc, 128).transpose(0, 3, 2, 1))


def _kpanel(m2d):
    k = m2d.shape[0] // 128
    return np.ascontiguousarray(m2d.reshape(k, 128, m2d.shape[1]).transpose(1, 0, 2))


def _wp(W, wc):
    W = np.asarray(W, np.float32)
    K, N = W.shape
    return np.ascontiguousarray(W.reshape(K // 128, 128, N // wc, wc).transpose(2, 1, 0, 3))


def _gain(g):
    return np.ascontiguousarray(np.asarray(g, np.float32).reshape(-1, 128).T)


def _consts(hf):
    own_p = hf * 2048 + np.arange(2048)
    oth_p = (1 - hf) * 2048 + np.arange(2048)
    own_s = hf * 1024 + np.arange(1024)
    oth_s = (1 - hf) * 1024 + np.arange(1024)
    pos_all = np.concatenate([own_p, own_s, oth_p, oth_s])
    d = np.arange(128)
    a, f = d // 64, d % 32
    inv = 10000.0 ** (-np.arange(0, 64, 2, dtype=np.float64) / 64)
    pa = np.where(a[:, None] == 0, pos_all[None, :] // 64, pos_all[None, :] % 64).astype(np.float64)
    ang = pa * inv[f][:, None]
    cosT = np.cos(ang).astype(np.float32)
    sinT = np.sin(ang).astype(np.float32)

    def dft(rows, cols, n, neg_sin):
        prod = (rows[:, None].astype(np.int64) * cols[None, :].astype(np.int64)) % n
        angm = 2.0 * np.pi * prod / n
        c = np.cos(angm) / np.sqrt(n)
        s = np.sin(angm) / np.sqrt(n)
        return c, (-s if neg_sin else s)

    kp_rows = np.concatenate([own_p, oth_p])
    c, s = dft(kp_rows, own_p, 4096, True)
    csp = np.stack([np.stack([_kpanel(mat[:, nb * 512:(nb + 1) * 512]) for nb in range(4)]) for mat in (c, s)]).astype(BF)
    ks_rows = np.concatenate([own_s, oth_s])
    c, s = dft(ks_rows, own_s, 2048, True)
    mat = np.concatenate([c, s], axis=0)
    css = np.stack([_kpanel(mat[:, nb * 512:(nb + 1) * 512]) for nb in range(2)]).astype(BF)
    return dict(cosT=cosT, sinT=sinT, csp=csp, css=css)


_CACHE = {}


def kernel(x_prompt, x_sample, mem_prompt, mem_sample, norm_mix, w_in, q_norm, k_norm, w_attn_br, w_four_br,
           w_gate, b_gate, w_out, norm_ca, mem_norm, w_cq, w_ckv, w_co, norm_ffn, w_pq, sub_keys, expert_u,
           expert_v, final_norm):
    f32 = lambda a: np.ascontiguousarray(np.asarray(a, np.float32))
    x_prompt, x_sample, mem_prompt, mem_sample = map(f32, (x_prompt, x_sample, mem_prompt, mem_sample))
    if "nc" not in _CACHE:
        _CACHE["nc"] = build()[0]
    nc = _CACHE["nc"]
    ch = np.arange(2048)
    prod = (ch[:, None] * ch[None, :]) % 2048
    angc = 2.0 * np.pi * prod / 2048
    cc = np.cos(angc) / np.sqrt(2048.0)
    sc = np.sin(angc) / np.sqrt(2048.0)
    ccs = np.stack([_kpanel(mat[:, cb * 512:(cb + 1) * 512]) for mat in (cc, sc) for cb in range(4)]).astype(BF)
    rm = np.zeros((128, 128), np.float32)
    for dout in range(128):
        if (dout % 64) < 32:
            rm[dout + 32, dout] = -1.0
        else:
            rm[dout - 32, dout] = 1.0
    shared = dict(
        w_in=_wp(w_in[0], 512), w_attn=_wp(w_attn_br[0], 256), w_four=_wp(w_four_br[0], 256), w_gate=_wp(w_gate[0], 256),
        w_out=_wp(w_out[0], 512), w_cq=_wp(w_cq[0], 512), w_ckv=_wp(w_ckv[0], 512), w_co=_wp(w_co[0], 512),
        w_pq=_wp(w_pq[0], 512),
        uT=np.ascontiguousarray(np.asarray(expert_u[0], np.float32).reshape(32, 512, 32, 128).transpose(0, 3, 2, 1)),
        ev=np.ascontiguousarray(np.asarray(expert_v[0], np.float32).reshape(4, 32, 128, 8, 512).transpose(3, 0, 2, 1, 4)),
        g_mix=_gain(norm_mix[0]), g_ca=_gain(norm_ca[0]), g_mem=_gain(mem_norm[0]), g_ffn=_gain(norm_ffn[0]),
        g_fin=_gain(final_norm), qn=f32(np.asarray(q_norm[0]).reshape(128, 1)), kn=f32(np.asarray(k_norm[0]).reshape(128, 1)),
        bg=_gain(b_gate[0]),
        subkT=np.ascontiguousarray(np.asarray(sub_keys[0], np.float32).transpose(2, 0, 1)),
        rmat=rm.astype(BF), ones=np.ones((128, 128), BF), ident=np.eye(128, dtype=np.float32).astype(BF), ccs=ccs,
    )
    pc = [_consts(0), _consts(1)]
    in_maps = []
    for c in range(8):
        b, hf = c // 2, c % 2
        op_ = slice(hf * 2048, (hf + 1) * 2048)
        tp_ = slice((1 - hf) * 2048, (2 - hf) * 2048)
        os_ = slice(hf * 1024, (hf + 1) * 1024)
        ts_ = slice((1 - hf) * 1024, (2 - hf) * 1024)
        xall = np.concatenate([x_prompt[b, op_], x_sample[b, os_], x_prompt[b, tp_], x_sample[b, ts_]], axis=0)
        mem = np.concatenate([mem_prompt[b], mem_sample[b]], axis=0)
        m = dict(shared)
        m.update(pc[hf])
        m["xT"] = _panels(xall, 32)
        m["memT"] = _panels(mem, 32)[0]
        in_maps.append(m)
    res = run_bass_kernel_spmd(nc, in_maps, core_ids=list(range(8)))
    y_prompt = np.empty_like(x_prompt)
    y_sample = np.empty_like(x_sample)
    for c in range(8):
        b, hf = c // 2, c % 2
        yT = np.asarray(res.results[c]["yT"], np.float32)
        y = yT.transpose(0, 3, 2, 1).reshape(TOWN, D)
        y_prompt[b, hf * 2048:(hf + 1) * 2048] = y[0:2048]
        y_sample[b, hf * 1024:(hf + 1) * 1024] = y[2048:3072]
    return (y_prompt, y_sample)
```

```python
import numpy as np
import ml_dtypes
from contextlib import ExitStack
import concourse.bass as bass
import concourse.mybir as mybir
from concourse.bass_utils import run_bass_kernel_spmd

F32 = mybir.dt.float32
BF16 = mybir.dt.bfloat16
AF = mybir.ActivationFunctionType
ALU = mybir.AluOpType
AX = mybir.AxisListType
BF = ml_dtypes.bfloat16

D = 4096
EPS = 1e-6
NPO = 6
NPA = 12
TOWN = 3072
TALL = 6144
ATT_SCALE = 128 ** -0.5
CA_SCALE = 256 ** -0.5


class Obj:
    _n = 0

    def __init__(self, name=""):
        Obj._n += 1
        self.id = Obj._n
        self.name = name
        self.w = {}
        self.r = {}


class Buf:
    def __init__(self, nc, st, name, shape, dtype):
        self.t = st.enter_context(nc.sbuf_tensor(name, list(shape), dtype))
        self.o = Obj(name)


class Ring:
    def __init__(self, items):
        self.items = items
        self.i = 0

    def next(self):
        it = self.items[self.i % len(self.items)]
        self.i += 1
        return it


class Sched:
    ENG = ("pe", "act", "dve", "pool", "sp")
    BLK = {"pe": "tensor", "act": "scalar", "dve": "vector", "pool": "gpsimd", "sp": "sync"}

    def __init__(self, nc, semstack):
        self.nc = nc
        self.semstack = semstack
        self.sems = {}
        self.ops = {e: [] for e in self.ENG}
        self.cnt = {}
        self.known = {e: {} for e in self.ENG}
        self.unsig = {e: False for e in self.ENG}
        self.nops = 0
        self.bufphys = {}
        self.free_phys = []
        self.nphys = 0

    def _collect(self, reads, writes, mykey, dma):
        d = {}
        for o in reads:
            for k, v in o.w.items():
                if v > d.get(k, 0):
                    d[k] = v
        for o in writes:
            for k, v in o.w.items():
                if dma and k == mykey:
                    continue
                if v > d.get(k, 0):
                    d[k] = v
            for k, v in o.r.items():
                if v > d.get(k, 0):
                    d[k] = v
        return d

    def _emit_waits(self, eng, deps, skipkey=None):
        kn = self.known[eng]
        lst = self.ops[eng]
        for k, v in deps.items():
            if k == skipkey:
                continue
            if kn.get(k, 0) >= v:
                continue
            lst.append((0, k, v))
            kn[k] = v

    def _record(self, reads, writes, key, c, merge=False):
        for o in reads:
            if o.r.get(key, 0) < c:
                o.r[key] = c
        for o in writes:
            if merge and key in o.w:
                o.w[key] = c
            else:
                o.w = {key: c}
            o.r = {}

    def op(self, eng, name, kw, reads=(), writes=(), signal=True, nosame=False):
        key = ("e", eng)
        deps = self._collect(reads, writes, key, False)
        self._emit_waits(eng, deps, skipkey=key if (eng == "pe" or nosame) else None)
        c = self.cnt.get(key, 0) + 1
        if signal:
            self.cnt[key] = c
            self.ops[eng].append((1, name, kw, key))
            self.unsig[eng] = False
        else:
            self.ops[eng].append((1, name, kw, None))
            self.unsig[eng] = True
        self._record(reads, writes, key, c)
        self.nops += 1

    def dma(self, q, out_ap, in_ap, semobj, reads=(), writes=()):
        oid = getattr(semobj, "o", semobj).id
        if oid not in self.bufphys:
            if self.free_phys:
                self.bufphys[oid] = self.free_phys.pop()
            else:
                self.bufphys[oid] = self.nphys
                self.nphys += 1
        key = ("b", self.bufphys[oid])
        deps = self._collect(reads, writes, key, True)
        self._emit_waits(q, deps)
        c = self.cnt.get(key, 0) + 16
        self.cnt[key] = c
        self.ops[q].append((2, out_ap, in_ap, key))
        self._record(reads, writes, key, c, merge=True)
        self.nops += 1

    def barrier(self):
        for e in self.ENG:
            assert not self.unsig[e], e
            self._emit_waits(e, dict(self.cnt))

    def flush(self, st):
        nc = self.nc
        for k in self.cnt:
            if k not in self.sems:
                self.sems[k] = self.semstack.enter_context(nc.semaphore("s%d" % len(self.sems)))
        sems = self.sems
        block = st.enter_context(nc.Block())
        for e in self.ENG:
            lst = self.ops[e]
            if not lst:
                continue

            def body(engine, lst=lst):
                for it in lst:
                    if it[0] == 0:
                        engine.wait_ge(sems[it[1]], it[2])
                    elif it[0] == 1:
                        ins = getattr(engine, it[1])(**it[2])
                        if it[3] is not None:
                            ins.then_inc(sems[it[3]], 1)
                    else:
                        engine.dma_start(out=it[1], in_=it[2]).then_inc(sems[it[3]], 16)

            getattr(block, self.BLK[e])(body)
        self.ops = {e: [] for e in self.ENG}
        self.free_phys = list(range(self.nphys))
        self.bufphys = {}


def build(stop_after=99, debug_out=()):
    nc = bass.Bass("TRN2", target_bir_lowering=False)

    def inp(name, shape, dt=F32):
        return nc.dram_tensor(name, list(shape), dt, kind="ExternalInput").ap()

    def scr(name, shape, dt=BF16):
        kind = "ExternalOutput" if name in debug_out else "Internal"
        return nc.dram_tensor(name, list(shape), dt, kind=kind).ap()

    xT = inp("xT", [NPA, 128, 32, 512])
    memT = inp("memT", [128, 32, 512])
    w_in = inp("w_in", [10, 128, 32, 512])
    w_attn = inp("w_attn", [16, 128, 16, 256])
    w_four = inp("w_four", [16, 128, 16, 256])
    w_gate = inp("w_gate", [32, 128, 32, 256])
    w_out = inp("w_out", [8, 128, 32, 512])
    w_cq = inp("w_cq", [2, 128, 32, 512])
    w_ckv = inp("w_ckv", [4, 128, 32, 512])
    w_co = inp("w_co", [8, 128, 8, 512])
    w_pq = inp("w_pq", [4, 128, 32, 512])
    uT = inp("uT", [32, 128, 32, 512])
    ev = inp("ev", [8, 4, 128, 32, 512])
    g_mix = inp("g_mix", [128, 32])
    g_ca = inp("g_ca", [128, 32])
    g_mem = inp("g_mem", [128, 32])
    g_ffn = inp("g_ffn", [128, 32])
    g_fin = inp("g_fin", [128, 32])
    qn = inp("qn", [128, 1])
    kn = inp("kn", [128, 1])
    bg = inp("bg", [128, 64])
    subkT = inp("subkT", [128, 2, 128])
    cosT = inp("cosT", [128, TALL])
    sinT = inp("sinT", [128, TALL])
    rmat = inp("rmat", [128, 128], BF16)
    ones_d = inp("ones", [128, 128], BF16)
    ident = inp("ident", [128, 128], BF16)
    ccs = inp("ccs", [8, 128, 16, 512], BF16)
    csp = inp("csp", [2, 4, 128, 32, 512], BF16)
    css = inp("css", [2, 128, 32, 512], BF16)
    yT = nc.dram_tensor("yT", [NPO, 128, 32, 512], F32, kind="ExternalOutput").ap()

    HT = scr("HT", [NPA, 128, 32, 512])
    QT = scr("QT", [16, 128, TOWN])
    KT = scr("KT", [4, 128, TALL])
    VS = scr("VS", [TALL, 512])
    FT = scr("FT", [NPA, 128, 16, 512])
    PQ = scr("PQ", [4, 2, TALL, 512])
    ZT = scr("ZT", [NPO, 128, 16, 512])
    AT = scr("AT", [NPO, 128, 16, 512])
    MT = scr("MT", [NPO, 128, 32, 512])
    X1T = scr("X1T", [NPO, 128, 32, 512], F32)
    HCT = scr("HCT", [NPO, 128, 32, 512])
    MNT = scr("MNT", [1, 128, 32, 512])
    QCT = scr("QCT", [8, 128, TOWN])
    KCT = scr("KCT", [8, 128, 512])
    VC = scr("VC", [512, 1024])
    OCT = scr("OCT", [NPO, 128, 8, 512])
    X2T = scr("X2T", [NPO, 128, 32, 512], F32)
    HPT = scr("HPT", [NPO, 128, 32, 512])
    QPT = scr("QPT", [16, 128, TOWN])
    WTp = scr("WTp", [NPO, 4, 128, 32, 512])
    GAT = scr("GAT", [NPO, 4, 128, 32, 512])
    X3T = scr("X3T", [NPO, 128, 32, 512], F32)

    outer = ExitStack()
    S = Sched(nc, outer)
    cnt = [0]

    def mkbuf(st, shape, dt, name="b"):
        cnt[0] += 1
        return Buf(nc, st, "%s%d" % (name, cnt[0]), shape, dt)

    def mkps(st, n, dt=F32, cols=512):
        out = []
        for i in range(n):
            cnt[0] += 1
            t = st.enter_context(nc.psum_tensor("ps%d" % cnt[0], [128, cols], dt))
            out.append((t, Obj("ps")))
        return out

    def mm(ps, po, pairs, reads, start=True, stop=True):
        n = len(pairs)
        for i, (l, r) in enumerate(pairs):
            last = i == n - 1
            S.op("pe", "matmul", dict(out=ps, lhsT=l, rhs=r, start=(start and i == 0), stop=(stop and last)),
                 reads=reads, writes=[po], signal=last)

    def load_w(buf, wpanel, kc):
        step = 8 if kc >= 8 else kc
        for c0 in range(0, kc, step):
            S.dma("pool", buf.t[:, c0:c0 + step, :], wpanel[:, c0:c0 + step, :], buf, writes=[buf.o])

    def consts(st, need_ones=True):
        ON = mkbuf(st, [128, 128], BF16, "ones")
        S.dma("sp", ON.t[:], ones_d, ON, writes=[ON.o])
        EP = mkbuf(st, [128, 1], F32, "eps")
        S.op("dve", "memset", dict(ap=EP.t[:], constant=EPS), writes=[EP.o])
        return ON, EP

    phase_idx = [0]

    def run_phase(fn):
        phase_idx[0] += 1
        if phase_idx[0] > stop_after:
            return
        with ExitStack() as st:
            fn(st)
            S.barrier()
            S.flush(st)

    def norm_phase(srcs, gain_ap, dsts, out_dt):
        def fn(st):
            ON, EP = consts(st)
            Xr = Ring([mkbuf(st, [128, 32, 256], F32, "nx") for _ in range(2)])
            SQr = Ring([mkbuf(st, [128, 32, 256], BF16, "nsq") for _ in range(2)])
            Or = Ring([mkbuf(st, [128, 32, 256], out_dt, "no") for _ in range(2)])
            RSr = Ring([mkbuf(st, [128, 256], F32, "nrs") for _ in range(2)])
            G = mkbuf(st, [128, 32], F32, "ng")
            PSr = Ring(mkps(st, 2))
            S.dma("sp", G.t[:], gain_ap, G, writes=[G.o])
            for src, dst in zip(srcs, dsts):
                for hf in range(2):
                    cs = slice(hf * 256, (hf + 1) * 256)
                    X, SQ, O, RS = Xr.next(), SQr.next(), Or.next(), RSr.next()
                    ps, po = PSr.next()
                    S.dma("sp", X.t[:], src[:, :, cs], X, writes=[X.o])
                    for q2 in range(2):
                        S.op("act", "activation", dict(out=SQ.t[:, q2 * 16:(q2 + 1) * 16, :], in_=X.t[:, q2 * 16:(q2 + 1) * 16, :],
                                                       func=AF.Square), reads=[X.o], writes=[SQ.o])
                    mm(ps[:, 0:256], po, [(ON.t[:], SQ.t[:, c, :]) for c in range(32)], reads=[ON.o, SQ.o])
                    S.op("act", "activation", dict(out=RS.t[:], in_=ps[:, 0:256], func=AF.Sqrt, bias=EP.t[:, 0:1], scale=1.0 / D),
                         reads=[po, EP.o], writes=[RS.o])
                    S.op("dve", "reciprocal", dict(out=RS.t[:], in_=RS.t[:]), reads=[RS.o], writes=[RS.o])
                    for q4 in range(4):
                        c8 = slice(q4 * 8, (q4 + 1) * 8)
                        S.op("pool", "tensor_tensor",
                             dict(out=X.t[:, c8, :], in0=X.t[:, c8, :],
                                  in1=G.t[:, c8].unsqueeze(2).broadcast_to([128, 8, 256]), op=ALU.mult),
                             reads=[X.o, G.o], writes=[X.o], nosame=(q4 > 0))
                    for q4 in range(4):
                        c8 = slice(q4 * 8, (q4 + 1) * 8)
                        S.op("dve", "tensor_tensor",
                             dict(out=O.t[:, c8, :], in0=X.t[:, c8, :],
                                  in1=RS.t[:].unsqueeze(1).broadcast_to([128, 8, 256]), op=ALU.mult),
                             reads=[X.o, RS.o], writes=[O.o], nosame=(q4 > 0))
                    S.dma("act", dst[:, :, cs], O.t[:], O, reads=[O.o])
        return fn

    def p2(st):
        ON, EP = consts(st)
        A = [mkbuf(st, [128, 32, 512], BF16, "A") for _ in range(2)]
        HB = Ring([mkbuf(st, [128, 32, 512], BF16, "HB") for _ in range(2)])
        CS = Ring([mkbuf(st, [128, 2, 512], F32, "CS") for _ in range(2)])
        RM = mkbuf(st, [128, 128], BF16, "RM")
        QN = mkbuf(st, [128, 1], F32, "QN")
        KN = mkbuf(st, [128, 1], F32, "KN")
        S.dma("sp", RM.t[:], rmat, RM, writes=[RM.o])
        S.dma("sp", QN.t[:], qn, QN, writes=[QN.o])
        S.dma("sp", KN.t[:], kn, KN, writes=[KN.o])
        EV = Ring([mkbuf(st, [128, 512], BF16, "EV") for _ in range(3)])
        XQ = Ring([mkbuf(st, [128, 512], F32, "XQ") for _ in range(4)])
        SQ = Ring([mkbuf(st, [128, 512], BF16, "SQ") for _ in range(4)])
        RS = Ring([mkbuf(st, [128, 512], F32, "RS") for _ in range(2)])
        XN = Ring([mkbuf(st, [128, 512], BF16, "XN") for _ in range(4)])
        T1 = Ring([mkbuf(st, [128, 512], F32, "T1") for _ in range(2)])
        T2 = Ring([mkbuf(st, [128, 512], F32, "T2") for _ in range(2)])
        OB = Ring([mkbuf(st, [128, 512], BF16, "OB") for _ in range(3)])
        pss = mkps(st, 8)
        PSM = Ring(pss[0:4])
        PS2 = Ring(pss[4:6])
        PS3 = Ring(pss[6:8])

        def stage_a(job):
            ps, po = job["ps"], job["po"]
            xq, sq = XQ.next(), SQ.next()
            S.op("act", "activation", dict(out=xq.t[:], in_=ps[:], func=AF.Copy), reads=[po], writes=[xq.o])
            S.op("act", "activation", dict(out=sq.t[:], in_=ps[:], func=AF.Square), reads=[po], writes=[sq.o])
            job["xq"], job["sq"] = xq, sq

        def stage_b(job):
            xq, sq, gain = job["xq"], job["sq"], job["gain"]
            ps2, po2 = PS2.next()
            mm(ps2[:], po2, [(ON.t[:], sq.t[:])], reads=[ON.o, sq.o])
            rs, xn = RS.next(), XN.next()
            S.op("act", "activation", dict(out=rs.t[:], in_=ps2[:], func=AF.Sqrt, bias=EP.t[:, 0:1], scale=1.0 / 128),
                 reads=[po2, EP.o], writes=[rs.o])
            S.op("dve", "reciprocal", dict(out=rs.t[:], in_=rs.t[:]), reads=[rs.o], writes=[rs.o])
            S.op("dve", "scalar_tensor_tensor", dict(out=xn.t[:], in0=xq.t[:], scalar=gain.t[:, 0:1], in1=rs.t[:],
                                                     op0=ALU.mult, op1=ALU.mult),
                 reads=[xq.o, gain.o, rs.o], writes=[xn.o])
            job["xn"] = xn

        def stage_c(job):
            xn, CSb, dst = job["xn"], job["cs"], job["dst"]
            ps3, po3 = PS3.next()
            mm(ps3[:], po3, [(RM.t[:], xn.t[:])], reads=[RM.o, xn.o])
            t1, t2, ob = T1.next(), T2.next(), OB.next()
            S.op("dve", "tensor_tensor", dict(out=t1.t[:], in0=xn.t[:], in1=CSb.t[:, 0, :], op=ALU.mult),
                 reads=[xn.o, CSb.o], writes=[t1.o])
            S.op("dve", "tensor_tensor", dict(out=t2.t[:], in0=ps3[:], in1=CSb.t[:, 1, :], op=ALU.mult),
                 reads=[po3, CSb.o], writes=[t2.o])
            S.op("pool", "tensor_tensor", dict(out=ob.t[:], in0=t1.t[:], in1=t2.t[:], op=ALU.add),
                 reads=[t1.o, t2.o], writes=[ob.o])
            S.dma("pool", dst, ob.t[:], ob, reads=[ob.o])

        pend = []

        def advance(newjob):
            for job in list(pend):
                job["age"] += 1
                if job["age"] == 1:
                    stage_b(job)
                elif job["age"] == 2:
                    stage_c(job)
                    pend.remove(job)
            if newjob is not None:
                stage_a(newjob)
                newjob["age"] = 0
                pend.append(newjob)

        load_w(A[0], w_in[0], 32)
        for ap in range(10):
            kind = "q" if ap < 4 else "k" if ap == 4 else "v" if ap == 5 else "f"
            Ab = A[ap % 2]
            if ap + 1 < 10:
                load_w(A[(ap + 1) % 2], w_in[ap + 1], 32)
            nbs = range(NPO) if kind == "q" else range(NPA)
            for nb in nbs:
                Hb = HB.next()
                S.dma("sp", Hb.t[:], HT[nb], Hb, writes=[Hb.o])
                if kind in "qk":
                    CSb = CS.next()
                    S.dma("sp", CSb.t[:, 0, :], cosT[:, nb * 512:(nb + 1) * 512], CSb, writes=[CSb.o])
                    S.dma("sp", CSb.t[:, 1, :], sinT[:, nb * 512:(nb + 1) * 512], CSb, writes=[CSb.o])
                for m in range(4):
                    ps, po = PSM.next()
                    if kind == "v":
                        mm(ps[:], po, [(Hb.t[:, c, m * 128:(m + 1) * 128], Ab.t[:, c, :]) for c in range(32)],
                           reads=[Hb.o, Ab.o])
                        advance(None)
                        Eb = EV.next()
                        S.op("act", "activation", dict(out=Eb.t[:], in_=ps[:], func=AF.Copy), reads=[po], writes=[Eb.o])
                        S.dma("pool", VS[nb * 512 + m * 128:nb * 512 + (m + 1) * 128, :], Eb.t[:], Eb, reads=[Eb.o])
                        continue
                    mm(ps[:], po, [(Ab.t[:, c, m * 128:(m + 1) * 128], Hb.t[:, c, :]) for c in range(32)],
                       reads=[Hb.o, Ab.o])
                    if kind == "f":
                        advance(None)
                        Eb = EV.next()
                        S.op("act", "activation", dict(out=Eb.t[:], in_=ps[:], func=AF.Copy), reads=[po], writes=[Eb.o])
                        S.dma("pool", FT[nb, :, (ap - 6) * 4 + m, :], Eb.t[:], Eb, reads=[Eb.o])
                        continue
                    gain = QN if kind == "q" else KN
                    dst = QT[ap * 4 + m, :, nb * 512:(nb + 1) * 512] if kind == "q" else KT[m, :, nb * 512:(nb + 1) * 512]
                    advance(dict(ps=ps, po=po, gain=gain, dst=dst, cs=CSb))
        while pend:
            advance(None)

    def p3(st):
        Bp = [mkbuf(st, [128, 16, 512], BF16, "Bp") for _ in range(2)]
        Fb = Ring([mkbuf(st, [128, 16, 512], BF16, "Fb") for _ in range(2)])
        EV = Ring([mkbuf(st, [128, 512], BF16, "EV") for _ in range(4)])
        PSM = Ring(mkps(st, 4))
        k = 0
        for pi in range(8):
            pq, cb = pi // 4, pi % 4
            B_ = Bp[pi % 2]
            S.dma("sp", B_.t[:], ccs[pi], B_, writes=[B_.o])
            for nb in range(NPA):
                F_ = Fb.next()
                S.dma("sp", F_.t[:], FT[nb], F_, writes=[F_.o])
                for m in range(4):
                    ps, po = PSM.next()
                    mm(ps[:], po, [(F_.t[:, c, m * 128:(m + 1) * 128], B_.t[:, c, :]) for c in range(16)],
                       reads=[F_.o, B_.o])
                    Eb = EV.next()
                    k += 1
                    if k % 2:
                        S.op("act", "activation", dict(out=Eb.t[:], in_=ps[:], func=AF.Copy), reads=[po], writes=[Eb.o])
                    else:
                        S.op("dve", "tensor_copy", dict(out=Eb.t[:], in_=ps[:]), reads=[po], writes=[Eb.o])
                    r0 = nb * 512 + m * 128
                    S.dma("pool", PQ[cb, pq, r0:r0 + 128, :], Eb.t[:], Eb, reads=[Eb.o])

    def p4(st):
        Aa = [mkbuf(st, [128, 32, 512], BF16, "Aa") for _ in range(2)]
        Bb = Ring([mkbuf(st, [128, 32, 512], BF16, "Bb") for _ in range(3)])
        EV = Ring([mkbuf(st, [128, 512], BF16, "EV") for _ in range(4)])
        pss = mkps(st, 8)
        GR = Ring([pss[0:4], pss[4:8]])
        k = 0

        def rows(cb, pq, r0, n):
            return PQ[cb, pq, r0:r0 + n, :].rearrange("(c p) n -> p c n", p=128)

        def epi(grp, nbp, cb):
            nonlocal k
            for m in range(4):
                ps, po = grp[m]
                Eb = EV.next()
                k += 1
                if k % 2:
                    S.op("act", "activation", dict(out=Eb.t[:], in_=ps[:], func=AF.Copy), reads=[po], writes=[Eb.o])
                else:
                    S.op("dve", "tensor_copy", dict(out=Eb.t[:], in_=ps[:]), reads=[po], writes=[Eb.o])
                S.dma("pool", ZT[nbp, :, cb * 4 + m, :], Eb.t[:], Eb, reads=[Eb.o])

        for cb in range(4):
            for kp in range(2):
                S.dma("sp", Aa[kp].t[:, 0:16, :], rows(cb, kp, 0, 2048), Aa[kp], writes=[Aa[kp].o])
                S.dma("sp", Aa[kp].t[:, 16:32, :], rows(cb, kp, 3072, 2048), Aa[kp], writes=[Aa[kp].o])
            for nb in range(4):
                grp = GR.next()
                for kp in range(2):
                    Bk = Bb.next()
                    S.dma("sp", Bk.t[:], csp[kp, nb], Bk, writes=[Bk.o])
                    for m in range(4):
                        ps, po = grp[m]
                        mm(ps[:], po, [(Aa[kp].t[:, c, m * 128:(m + 1) * 128], Bk.t[:, c, :]) for c in range(32)],
                           reads=[Aa[kp].o, Bk.o], start=(kp == 0), stop=(kp == 1))
                epi(grp, nb, cb)
        for cb in range(4):
            A0 = Aa[cb % 2]
            S.dma("sp", A0.t[:, 0:8, :], rows(cb, 0, 2048, 1024), A0, writes=[A0.o])
            S.dma("sp", A0.t[:, 8:16, :], rows(cb, 0, 5120, 1024), A0, writes=[A0.o])
            S.dma("sp", A0.t[:, 16:24, :], rows(cb, 1, 2048, 1024), A0, writes=[A0.o])
            S.dma("sp", A0.t[:, 24:32, :], rows(cb, 1, 5120, 1024), A0, writes=[A0.o])
            for nb in range(2):
                grp = GR.next()
                Bk = Bb.next()
                S.dma("sp", Bk.t[:], css[nb], Bk, writes=[Bk.o])
                for m in range(4):
                    ps, po = grp[m]
                    mm(ps[:], po, [(A0.t[:, c, m * 128:(m + 1) * 128], Bk.t[:, c, :]) for c in range(32)],
                       reads=[A0.o, Bk.o])
                epi(grp, 4 + nb, cb)

    def p5(st):
        ON, EP = consts(st)
        KTg = mkbuf(st, [128, 4096], BF16, "KTg")
        Vg = mkbuf(st, [128, 32, 128], BF16, "Vg")
        QG = mkbuf(st, [128, 4, 2048], BF16, "QG")
        ER = Ring([mkbuf(st, [128, 512], BF16, "E") for _ in range(3)])
        RZ = Ring([mkbuf(st, [128, 512], F32, "RZ") for _ in range(2)])
        OB = Ring([mkbuf(st, [128, 4, 128], BF16, "OB") for _ in range(2)])
        pss = mkps(st, 6)
        PSS = Ring(pss[0:2])
        PSO = Ring(pss[2:4])
        PSZ = Ring(pss[4:6])
        seqs = [((0, 2048), (3072, 5120), 0, 2048, 0), ((2048, 3072), (5120, 6144), 2048, 1024, 4)]
        for (o0, o1), (t0, t1), q0, nq, pan0 in seqs:
            hk = o1 - o0
            nch = 2 * hk // 128
            for g in range(4):
                S.dma("sp", KTg.t[:, 0:hk], KT[g, :, o0:o1], KTg, writes=[KTg.o])
                S.dma("sp", KTg.t[:, hk:2 * hk], KT[g, :, t0:t1], KTg, writes=[KTg.o])
                S.dma("sp", Vg.t[:, 0:nch // 2, :], VS[o0:o1, g * 128:(g + 1) * 128].rearrange("(c p) d -> p c d", p=128),
                      Vg, writes=[Vg.o])
                S.dma("sp", Vg.t[:, nch // 2:nch, :], VS[t0:t1, g * 128:(g + 1) * 128].rearrange("(c p) d -> p c d", p=128),
                      Vg, writes=[Vg.o])
                S.dma("sp", QG.t[:, :, 0:nq], QT[g * 4:(g + 1) * 4, :, q0:q0 + nq].rearrange("h d q -> d h q"),
                      QG, writes=[QG.o])
                for qb in range(nq // 128):
                    rhsq = QG.t[:, :, qb * 128:(qb + 1) * 128]
                    pso, poo = PSO.next()
                    psz, poz = PSZ.next()

                    def issue_s(sc):
                        ps, po = PSS.next()
                        mm(ps[:], po, [(KTg.t[:, sc * 128:(sc + 1) * 128], rhsq)], reads=[KTg.o, QG.o])
                        Eb = ER.next()
                        S.op("act", "activation", dict(out=Eb.t[:], in_=ps[:], func=AF.Exp, scale=ATT_SCALE),
                             reads=[po], writes=[Eb.o])
                        return Eb

                    Ecur = issue_s(0)
                    for sc in range(nch):
                        Enext = issue_s(sc + 1) if sc + 1 < nch else None
                        mm(pso[:], poo, [(Vg.t[:, sc, :], Ecur.t[:])], reads=[Vg.o, Ecur.o],
                           start=(sc == 0), stop=(sc == nch - 1))
                        mm(psz[:], poz, [(ON.t[:], Ecur.t[:])], reads=[ON.o, Ecur.o],
                           start=(sc == 0), stop=(sc == nch - 1))
                        Ecur = Enext
                    rz, ob = RZ.next(), OB.next()
                    S.op("dve", "reciprocal", dict(out=rz.t[:], in_=psz[:]), reads=[poz], writes=[rz.o])
                    S.op("dve", "tensor_tensor", dict(out=ob.t[:].rearrange("p h q -> p (h q)"), in0=pso[:], in1=rz.t[:],
                                                      op=ALU.mult), reads=[poo, rz.o], writes=[ob.o])
                    tq = qb * 128
                    S.dma("pool", AT[pan0 + tq // 512, :, g * 4:(g + 1) * 4, tq % 512:tq % 512 + 128], ob.t[:], ob,
                          reads=[ob.o])

    def p6(st):
        Wa = mkbuf(st, [128, 16, 256], BF16, "Wa")
        Wf = mkbuf(st, [128, 16, 256], BF16, "Wf")
        Wg0 = mkbuf(st, [128, 32, 256], BF16, "Wg0")
        Wg1 = mkbuf(st, [128, 32, 256], BF16, "Wg1")
        ATb = Ring([mkbuf(st, [128, 16, 512], BF16, "ATb") for _ in range(2)])
        ZTb = Ring([mkbuf(st, [128, 16, 512], BF16, "ZTb") for _ in range(2)])
        HBb = Ring([mkbuf(st, [128, 32, 512], BF16, "HBb") for _ in range(2)])
        BG = mkbuf(st, [128, 64], F32, "BG")
        S.dma("sp", BG.t[:], bg, BG, writes=[BG.o])
        S0 = Ring([mkbuf(st, [128, 512], F32, "S0") for _ in range(2)])
        S1 = Ring([mkbuf(st, [128, 512], F32, "S1") for _ in range(2)])
        MO = Ring([mkbuf(st, [128, 512], BF16, "MO") for _ in range(2)])
        pss = mkps(st, 8)
        GR = Ring([pss[0:4], pss[4:8]])
        for mb2 in range(16):
            load_w(Wa, w_attn[mb2], 16)
            load_w(Wf, w_four[mb2], 16)
            load_w(Wg0, w_gate[mb2], 32)
            load_w(Wg1, w_gate[16 + mb2], 32)
            for nb in range(NPO):
                a_, z_, h_ = ATb.next(), ZTb.next(), HBb.next()
                S.dma("sp", a_.t[:], AT[nb], a_, writes=[a_.o])
                S.dma("sp", z_.t[:], ZT[nb], z_, writes=[z_.o])
                S.dma("sp", h_.t[:], HT[nb], h_, writes=[h_.o])
                for mi in range(2):
                    m = mb2 * 2 + mi
                    (pa, oa), (pf, of), (pg0, og0), (pg1, og1) = GR.next()
                    cs = slice(mi * 128, (mi + 1) * 128)
                    mm(pa[:], oa, [(Wa.t[:, c, cs], a_.t[:, c, :]) for c in range(16)], reads=[Wa.o, a_.o])
                    mm(pf[:], of, [(Wf.t[:, c, cs], z_.t[:, c, :]) for c in range(16)], reads=[Wf.o, z_.o])
                    mm(pg0[:], og0, [(Wg0.t[:, c, cs], h_.t[:, c, :]) for c in range(32)], reads=[Wg0.o, h_.o])
                    mm(pg1[:], og1, [(Wg1.t[:, c, cs], h_.t[:, c, :]) for c in range(32)], reads=[Wg1.o, h_.o])
                    s0, s1, mo = S0.next(), S1.next(), MO.next()
                    t0, t1 = s0, s1
                    S.op("act", "activation", dict(out=s0.t[:], in_=pg0[:], func=AF.Sigmoid, bias=BG.t[:, m:m + 1], scale=1.0),
                         reads=[og0, BG.o], writes=[s0.o])
                    S.op("act", "activation", dict(out=s1.t[:], in_=pg1[:], func=AF.Sigmoid, bias=BG.t[:, 32 + m:33 + m], scale=1.0),
                         reads=[og1, BG.o], writes=[s1.o])
                    S.op("dve", "tensor_tensor", dict(out=t0.t[:], in0=pa[:], in1=s0.t[:], op=ALU.mult),
                         reads=[oa, s0.o], writes=[t0.o])
                    S.op("dve", "tensor_tensor", dict(out=t1.t[:], in0=pf[:], in1=s1.t[:], op=ALU.mult),
                         reads=[of, s1.o], writes=[t1.o])
                    S.op("pool", "tensor_tensor", dict(out=mo.t[:], in0=t0.t[:], in1=t1.t[:], op=ALU.add),
                         reads=[t0.o, t1.o], writes=[mo.o])
                    S.dma("pool", MT[nb, :, m, :], mo.t[:], mo, reads=[mo.o])

    def resid_gemm(W, kcw, Bsrc, resid, dst):
        def fn(st):
            A = [mkbuf(st, [128, kcw, 512], BF16, "A") for _ in range(2)]
            Bb = Ring([mkbuf(st, [128, kcw, 512], BF16, "B") for _ in range(2)])
            XR = Ring([mkbuf(st, [128, 512], F32, "XR") for _ in range(3)])
            XO = Ring([mkbuf(st, [128, 512], F32, "XO") for _ in range(3)])
            PSM = Ring(mkps(st, 4))
            load_w(A[0], W[0], kcw)
            for ap in range(8):
                Ab = A[ap % 2]
                if ap + 1 < 8:
                    load_w(A[(ap + 1) % 2], W[ap + 1], kcw)
                for nb in range(NPO):
                    B_ = Bb.next()
                    S.dma("sp", B_.t[:], Bsrc[nb], B_, writes=[B_.o])
                    for mi in range(4):
                        m = ap * 4 + mi
                        ps, po = PSM.next()
                        mm(ps[:], po, [(Ab.t[:, c, mi * 128:(mi + 1) * 128], B_.t[:, c, :]) for c in range(kcw)],
                           reads=[Ab.o, B_.o])
                        xr, xo = XR.next(), XO.next()
                        S.dma("sp", xr.t[:], resid[nb, :, m, :], xr, writes=[xr.o])
                        S.op("dve", "tensor_tensor", dict(out=xo.t[:], in0=ps[:], in1=xr.t[:], op=ALU.add),
                             reads=[po, xr.o], writes=[xo.o])
                        S.dma("pool", dst[nb, :, m, :], xo.t[:], xo, reads=[xo.o])
        return fn

    def proj_fm(W, naps, Bsrc, nbs, dst, woff=0):
        def fn(st):
            A = [mkbuf(st, [128, 32, 512], BF16, "A") for _ in range(2)]
            Bb = Ring([mkbuf(st, [128, 32, 512], BF16, "B") for _ in range(2)])
            EV = Ring([mkbuf(st, [128, 512], BF16, "EV") for _ in range(4)])
            PSM = Ring(mkps(st, 4))
            k = 0
            load_w(A[0], W[woff], 32)
            for ap in range(naps):
                Ab = A[ap % 2]
                if ap + 1 < naps:
                    load_w(A[(ap + 1) % 2], W[woff + ap + 1], 32)
                for nb in range(nbs):
                    B_ = Bb.next()
                    S.dma("sp", B_.t[:], Bsrc[nb], B_, writes=[B_.o])
                    for mi in range(4):
                        ps, po = PSM.next()
                        mm(ps[:], po, [(Ab.t[:, c, mi * 128:(mi + 1) * 128], B_.t[:, c, :]) for c in range(32)],
                           reads=[Ab.o, B_.o])
                        Eb = EV.next()
                        k += 1
                        if k % 2:
                            S.op("act", "activation", dict(out=Eb.t[:], in_=ps[:], func=AF.Copy), reads=[po], writes=[Eb.o])
                        else:
                            S.op("dve", "tensor_copy", dict(out=Eb.t[:], in_=ps[:]), reads=[po], writes=[Eb.o])
                        S.dma("pool", dst[ap * 4 + mi, :, nb * 512:(nb + 1) * 512], Eb.t[:], Eb, reads=[Eb.o])
        return fn

    def p8v(st):
        A = [mkbuf(st, [128, 32, 512], BF16, "A") for _ in range(2)]
        B_ = mkbuf(st, [128, 32, 512], BF16, "B")
        EV = Ring([mkbuf(st, [128, 512], BF16, "EV") for _ in range(4)])
        PSM = Ring(mkps(st, 4))
        S.dma("sp", B_.t[:], MNT[0], B_, writes=[B_.o])
        for ap in range(2):
            load_w(A[ap], w_ckv[2 + ap], 32)
        for ap in range(2):
            for mi in range(4):
                ps, po = PSM.next()
                mm(ps[:], po, [(B_.t[:, c, mi * 128:(mi + 1) * 128], A[ap].t[:, c, :]) for c in range(32)],
                   reads=[A[ap].o, B_.o])
                Eb = EV.next()
                S.op("act", "activation", dict(out=Eb.t[:], in_=ps[:], func=AF.Copy), reads=[po], writes=[Eb.o])
                S.dma("pool", VC[mi * 128:(mi + 1) * 128, ap * 512:(ap + 1) * 512], Eb.t[:], Eb, reads=[Eb.o])

    def p8c(st):
        ON, EP = consts(st)
        KCb = mkbuf(st, [128, 8, 512], BF16, "KCb")
        QCb = mkbuf(st, [128, 8, TOWN], BF16, "QCb")
        VCb = mkbuf(st, [128, 4, 1024], BF16, "VCb")
        S.dma("sp", KCb.t[:], KCT.rearrange("b d m -> d b m"), KCb, writes=[KCb.o])
        S.dma("sp", QCb.t[:], QCT.rearrange("b d t -> d b t"), QCb, writes=[QCb.o])
        S.dma("sp", VCb.t[:], VC.rearrange("(c p) n -> p c n", p=128), VCb, writes=[VCb.o])
        ER = Ring([mkbuf(st, [128, 512], BF16, "E") for _ in range(4)])
        RZ = Ring([mkbuf(st, [128, 512], F32, "RZ") for _ in range(2)])
        OB = Ring([mkbuf(st, [128, 512], BF16, "OB") for _ in range(4)])
        pss = mkps(st, 8)
        PSS = Ring(pss[0:2])
        PSO = Ring(pss[2:6])
        PSZ = Ring(pss[6:8])
        for nb in range(NPO):
            mc0 = 0 if nb < 4 else 2
            for hc in range(4):
                Es = []
                for mc in range(2):
                    ps, po = PSS.next()
                    ms = slice((mc0 + mc) * 128, (mc0 + mc + 1) * 128)
                    mm(ps[:], po, [(KCb.t[:, hc * 2 + db, ms], QCb.t[:, hc * 2 + db, nb * 512:(nb + 1) * 512]) for db in range(2)],
                       reads=[KCb.o, QCb.o])
                    Eb = ER.next()
                    S.op("act", "activation", dict(out=Eb.t[:], in_=ps[:], func=AF.Exp, scale=CA_SCALE), reads=[po], writes=[Eb.o])
                    Es.append(Eb)
                psz, poz = PSZ.next()
                mm(psz[:], poz, [(ON.t[:], Es[mc].t[:]) for mc in range(2)], reads=[ON.o, Es[0].o, Es[1].o])
                rz = RZ.next()
                S.op("dve", "reciprocal", dict(out=rz.t[:], in_=psz[:]), reads=[poz], writes=[rz.o])
                for dvb in range(2):
                    pso, poo = PSO.next()
                    cs = slice(hc * 256 + dvb * 128, hc * 256 + (dvb + 1) * 128)
                    mm(pso[:], poo, [(VCb.t[:, mc0 + mc, cs], Es[mc].t[:]) for mc in range(2)],
                       reads=[VCb.o, Es[0].o, Es[1].o])
                    ob = OB.next()
                    S.op("dve", "tensor_tensor", dict(out=ob.t[:], in0=pso[:], in1=rz.t[:], op=ALU.mult),
                         reads=[poo, rz.o], writes=[ob.o])
                    S.dma("pool", OCT[nb, :, hc * 2 + dvb, :], ob.t[:], ob, reads=[ob.o])

    def p9cd(st):
        SUBK = mkbuf(st, [128, 2, 128], BF16, "SUBK")
        S.dma("pool", SUBK.t[:], subkT, SUBK, writes=[SUBK.o])
        IDB = mkbuf(st, [128, 128], BF16, "IDB")
        S.dma("sp", IDB.t[:], ident, IDB, writes=[IDB.o])
        Q = mkbuf(st, [128, 16, 128], BF16, "Q16")
        SK = mkbuf(st, [128, 16, 128], F32, "SK")
        TOP = mkbuf(st, [128, 16, 16], F32, "TOP")
        B16 = mkbuf(st, [128, 8, 16], F32, "B16")
        JUNK = mkbuf(st, [128, 16], F32, "JUNK")
        NEGM = mkbuf(st, [128, 8], F32, "NEGM")
        ZS = mkbuf(st, [128, 8], F32, "ZS")
        LNZ = mkbuf(st, [128, 8], F32, "LNZ")
        BIAS = mkbuf(st, [128, 8], F32, "BIAS")
        TAUP = mkbuf(st, [128, 8], F32, "TAUP")
        S1B = mkbuf(st, [128, 8, 128], F32, "S1B")
        Xr = Ring([mkbuf(st, [128, 8, 2, 128], F32, "X") for _ in range(3)])
        Er = Ring([mkbuf(st, [128, 8, 256], BF16, "E") for _ in range(6)])
        WTSr = Ring([mkbuf(st, [128, 16, 128], BF16, "WTS") for _ in range(2)])
        pssk = mkps(st, 2)
        pstr = Ring(mkps(st, 2, F32, 256))
        A = [mkbuf(st, [128, 32, 512], BF16, "A") for _ in range(2)]
        Bb = Ring([mkbuf(st, [128, 32, 512], BF16, "B") for _ in range(2)])
        GA = Ring([mkbuf(st, [128, 512], BF16, "GA") for _ in range(4)])
        PSM = Ring(mkps(st, 4))
        LAG = 3

        def gen_c():
            for tt in range(24):
                nb, tsub = tt // 4, tt % 4
                S.dma("sp", Q.t[:], QPT[:, :, tt * 128:(tt + 1) * 128].rearrange("b d t -> d b t"), Q, writes=[Q.o])
                for half in range(2):
                    for h8 in range(8):
                        hc = half * 8 + h8
                        ps, po = pssk[h8 // 4]
                        mm(ps[:, (h8 % 4) * 128:(h8 % 4 + 1) * 128], po, [(Q.t[:, hc, :], SUBK.t[:, hc % 2, :])],
                           reads=[Q.o, SUBK.o])
                    for bk in range(2):
                        ps, po = pssk[bk]
                        c0 = half * 8 + bk * 4
                        S.op("act", "activation", dict(out=SK.t[:, c0:c0 + 4, :].rearrange("p a b -> p (a b)"), in_=ps[:],
                                                       func=AF.Copy), reads=[po], writes=[SK.o])
                yield
                X0, X1 = Xr.items[0], Xr.items[1]
                SKRv = X0.t[:].rearrange("p h i j -> p (h i) j")
                CNv = X1.t[:].rearrange("p h i j -> p h (i j)")
                for hc in range(16):
                    S.op("dve", "max", dict(out=TOP.t[:, hc, 0:8], in_=SK.t[:, hc, :]), reads=[SK.o], writes=[TOP.o], nosame=True)
                for hc in range(16):
                    S.op("dve", "match_replace", dict(out=SKRv[:, hc, :], in_to_replace=TOP.t[:, hc, 0:8], in_values=SK.t[:, hc, :],
                                                      imm_value=-1e30), reads=[SK.o, TOP.o], writes=[X0.o], nosame=(hc > 0))
                yield
                for hc in range(16):
                    S.op("dve", "max", dict(out=TOP.t[:, hc, 8:16], in_=SKRv[:, hc, :]), reads=[X0.o], writes=[TOP.o], nosame=(hc > 0))
                yield
                for hh in range(2):
                    for h4 in range(4):
                        h = hh * 4 + h4
                        a_ = TOP.t[:, 2 * h, :].unsqueeze(2).broadcast_to([128, 16, 16])
                        b_ = TOP.t[:, 2 * h + 1, :].unsqueeze(1).broadcast_to([128, 16, 16])
                        S.op("dve", "tensor_tensor", dict(out=CNv[:, h4, :].rearrange("p (a b) -> p a b", a=16), in0=a_, in1=b_, op=ALU.add),
                             reads=[TOP.o], writes=[X1.o], nosame=(h4 > 0))
                    for h4 in range(4):
                        h = hh * 4 + h4
                        S.op("dve", "max", dict(out=B16.t[:, h, 0:8], in_=CNv[:, h4, :]), reads=[X1.o], writes=[B16.o], nosame=(h4 > 0))
                    for h4 in range(4):
                        h = hh * 4 + h4
                        S.op("dve", "match_replace", dict(out=CNv[:, 4 + h4, :], in_to_replace=B16.t[:, h, 0:8], in_values=CNv[:, h4, :],
                                                          imm_value=-1e30), reads=[X1.o, B16.o], writes=[X1.o], nosame=(h4 > 0))
                    for h4 in range(4):
                        h = hh * 4 + h4
                        S.op("dve", "max", dict(out=B16.t[:, h, 8:16], in_=CNv[:, 4 + h4, :]), reads=[X1.o], writes=[B16.o], nosame=(h4 > 0))
                    yield
                S.op("dve", "tensor_scalar", dict(out=NEGM.t[:], in0=B16.t[:, :, 0], scalar1=-1.0, scalar2=0.0,
                                                  op0=ALU.mult, op1=ALU.add), reads=[B16.o], writes=[NEGM.o])
                S.op("dve", "memset", dict(ap=ZS.t[:], constant=0.0), writes=[ZS.o])
                for h in range(8):
                    S.op("act", "activation", dict(out=JUNK.t[:], in_=B16.t[:, h, :], func=AF.Exp, bias=NEGM.t[:, h:h + 1],
                                                   scale=1.0, accum_out=ZS.t[:, h:h + 1]),
                         reads=[B16.o, NEGM.o], writes=[JUNK.o, ZS.o], nosame=(h > 0))
                S.op("act", "activation", dict(out=LNZ.t[:], in_=ZS.t[:], func=AF.Ln), reads=[ZS.o], writes=[LNZ.o])
                S.op("dve", "tensor_tensor", dict(out=BIAS.t[:], in0=NEGM.t[:], in1=LNZ.t[:], op=ALU.subtract),
                     reads=[NEGM.o, LNZ.o], writes=[BIAS.o])
                S.op("dve", "tensor_tensor", dict(out=TAUP.t[:], in0=B16.t[:, :, 15], in1=BIAS.t[:], op=ALU.add),
                     reads=[B16.o, BIAS.o], writes=[TAUP.o])
                S.op("dve", "tensor_scalar", dict(out=TAUP.t[:], in0=TAUP.t[:], scalar1=1.0, scalar2=-2e-5,
                                                  op0=ALU.mult, op1=ALU.add), reads=[TAUP.o], writes=[TAUP.o])
                skv = SK.t[:].rearrange("p (h c) n -> p h c n", c=2)
                S.op("dve", "tensor_tensor", dict(out=S1B.t[:], in0=skv[:, :, 0, :],
                                                  in1=BIAS.t[:].unsqueeze(2).broadcast_to([128, 8, 128]), op=ALU.add),
                     reads=[SK.o, BIAS.o], writes=[S1B.o])
                s2 = skv[:, :, 1, :]
                yield
                pend = []
                wts = [None]

                def stage2(eb, Eb):
                    if eb % 8 == 0:
                        wts[0] = WTSr.next()
                    WTS = wts[0]
                    pt, pto = pstr.next()
                    for k in range(2):
                        mm(pt[:, k * 128:(k + 1) * 128], pto,
                           [(Eb.t[:, h, k * 128:(k + 1) * 128], IDB.t[:]) for h in range(8)], reads=[Eb.o, IDB.o])
                    cl = (eb % 8) * 2
                    S.op("dve", "tensor_copy", dict(out=WTS.t[:, cl:cl + 2, :].rearrange("p a b -> p (a b)"), in_=pt[:]),
                         reads=[pto], writes=[WTS.o], nosame=True)
                    if eb % 8 == 7:
                        c16 = ((eb // 8) % 2) * 16
                        S.dma("sp", WTp[nb, eb // 16, :, c16:c16 + 16, tsub * 128:(tsub + 1) * 128], WTS.t[:], WTS, reads=[WTS.o])

                for eb in range(64):
                    i0 = eb * 2
                    Xb, Eb = Xr.next(), Er.next()
                    S.op("dve", "tensor_tensor",
                         dict(out=Xb.t[:], in0=S1B.t[:, :, i0:i0 + 2].unsqueeze(3).broadcast_to([128, 8, 2, 128]),
                              in1=s2.unsqueeze(2).broadcast_to([128, 8, 2, 128]), op=ALU.add),
                         reads=[S1B.o, SK.o], writes=[Xb.o])
                    xv = Xb.t[:].rearrange("p h i j -> p h (i j)")
                    S.op("act", "activation", dict(out=Eb.t[:].rearrange("p h e -> p (h e)"),
                                                   in_=Xb.t[:].rearrange("p h i j -> p (h i j)"), func=AF.Exp),
                         reads=[Xb.o], writes=[Eb.o])
                    for h in range(8):
                        S.op("dve", "scalar_tensor_tensor",
                             dict(out=Eb.t[:, h, :], in0=xv[:, h, :], scalar=TAUP.t[:, h:h + 1], in1=Eb.t[:, h, :],
                                  op0=ALU.is_ge, op1=ALU.mult), reads=[Xb.o, TAUP.o, Eb.o], writes=[Eb.o], nosame=(h > 0))
                    pend.append((eb, Eb))
                    if len(pend) > LAG:
                        stage2(*pend.pop(0))
                    if eb % 2 == 1:
                        yield
                while pend:
                    stage2(*pend.pop(0))
                yield

        def gen_d():
            tiles = [(mb, nb, mi) for mb in range(32) for nb in range(NPO) for mi in range(4)]
            info = {}
            load_w(A[0], uT[0], 32)
            B_ = None
            for j in range(len(tiles) + 4):
                if j < len(tiles):
                    mb, nb, mi = tiles[j]
                    Ab = A[mb % 2]
                    if nb == 0 and mi == 0 and mb + 1 < 32:
                        load_w(A[(mb + 1) % 2], uT[mb + 1], 32)
                    if mi == 0:
                        B_ = Bb.next()
                        S.dma("sp", B_.t[:], HPT[nb], B_, writes=[B_.o])
                    ps, po = PSM.next()
                    mm(ps[:], po, [(Ab.t[:, c, mi * 128:(mi + 1) * 128], B_.t[:, c, :]) for c in range(32)],
                       reads=[Ab.o, B_.o])
                    info[j] = (ps, po, nb, mb * 4 + mi)
                if 0 <= j - 3 < len(tiles):
                    ps, po, nb_, ec = info[j - 3]
                    ga = GA.next()
                    S.op("act", "activation", dict(out=ga.t[:], in_=ps[:], func=AF.Copy), reads=[po], writes=[ga.o])
                    info[j - 3] = (ga, nb_, ec)
                if 0 <= j - 4 < len(tiles):
                    ga, nb_, ec = info.pop(j - 4)
                    S.dma("sp", GAT[nb_, ec // 32, :, ec % 32, :], ga.t[:], ga, reads=[ga.o])
                yield

        gc, gd = gen_c(), gen_d()
        alive = [True, True]
        while alive[0] or alive[1]:
            for i, g in enumerate((gc, gd)):
                for _ in range(1):
                    if alive[i]:
                        try:
                            next(g)
                        except StopIteration:
                            alive[i] = False

    def p9e(st):
        ACC = mkbuf(st, [128, 32, 512], F32, "ACC")
        A = Ring([mkbuf(st, [128, 32, 512], BF16, "A") for _ in range(2)])
        Bg = mkbuf(st, [128, 32, 512], BF16, "Bg")
        Bw = Ring([mkbuf(st, [128, 8, 512], BF16, "Bw") for _ in range(2)])
        PSM = Ring(mkps(st, 8))
        for nb in range(NPO):
            S.dma("sp", ACC.t[:], X2T[nb], ACC, writes=[ACC.o])
            for kp in range(4):
                S.dma("sp", Bg.t[:], GAT[nb, kp], Bg, writes=[Bg.o])
                for q in range(4):
                    bw = Bw.next()
                    c8 = slice(q * 8, (q + 1) * 8)
                    S.dma("sp", bw.t[:], WTp[nb, kp, :, c8, :], bw, writes=[bw.o])
                    S.op("act", "activation", dict(out=Bg.t[:, c8, :], in_=Bg.t[:, c8, :], func=AF.Gelu),
                         reads=[Bg.o], writes=[Bg.o], nosame=(q > 0))
                    S.op("dve", "tensor_tensor", dict(out=Bg.t[:, c8, :], in0=Bg.t[:, c8, :], in1=bw.t[:], op=ALU.mult),
                         reads=[Bg.o, bw.o], writes=[Bg.o], nosame=(q > 0))
                for ap in range(8):
                    Ab = A.next()
                    load_w(Ab, ev[ap, kp], 32)
                    for mi in range(4):
                        m = ap * 4 + mi
                        ps, po = PSM.next()
                        mm(ps[:], po, [(Ab.t[:, c, mi * 128:(mi + 1) * 128], Bg.t[:, c, :]) for c in range(32)],
                           reads=[Ab.o, Bg.o])
                        S.op("dve", "tensor_tensor", dict(out=ACC.t[:, m, :], in0=ps[:], in1=ACC.t[:, m, :], op=ALU.add),
                             reads=[po, ACC.o], writes=[ACC.o], nosame=True)
            S.dma("act", X3T[nb], ACC.t[:], ACC, reads=[ACC.o])

    run_phase(norm_phase([xT[j] for j in range(NPA)], g_mix, [HT[j] for j in range(NPA)], BF16))
    run_phase(p2)
    run_phase(p3)
    run_phase(p4)
    run_phase(p5)
    run_phase(p6)
    run_phase(resid_gemm(w_out, 32, MT, xT, X1T))
    run_phase(norm_phase([X1T[j] for j in range(NPO)], g_ca, [HCT[j] for j in range(NPO)], BF16))
    run_phase(norm_phase([memT], g_mem, [MNT[0]], BF16))
    run_phase(proj_fm(w_cq, 2, HCT, NPO, QCT))
    run_phase(proj_fm(w_ckv, 2, MNT, 1, KCT))
    run_phase(p8v)
    run_phase(p8c)
    run_phase(resid_gemm(w_co, 8, OCT, X1T, X2T))
    run_phase(norm_phase([X2T[j] for j in range(NPO)], g_ffn, [HPT[j] for j in range(NPO)], BF16))
    run_phase(proj_fm(w_pq, 4, HPT, NPO, QPT))
    run_phase(p9cd)
    run_phase(p9e)
    run_phase(norm_phase([X3T[j] for j in range(NPO)], g_fin, [yT[j] for j in range(NPO)], F32))
    outer.close()
    return nc, S.nops


def _panels(x2d, kc):
    n = x2d.shape[0] // 512
    return np.ascontiguousarray(x2d.reshape(n, 512, kc, 128).transpose(0, 3, 2, 1))


def _kpanel(m2d):
    k = m2d.shape[0] // 128
    return np.ascontiguousarray(m2d.reshape(k, 128, m2d.shape[1]).transpose(1, 0, 2))


def _wp(W, wc):
    W = np.asarray(W, np.float32)
    K, N = W.shape
    return np.ascontiguousarray(W.reshape(K // 128, 128, N // wc, wc).transpose(2, 1, 0, 3))


def _gain(g):
    return np.ascontiguousarray(np.asarray(g, np.float32).reshape(-1, 128).T)


def _consts(hf):
    own_p = hf * 2048 + np.arange(2048)
    oth_p = (1 - hf) * 2048 + np.arange(2048)
    own_s = hf * 1024 + np.arange(1024)
    oth_s = (1 - hf) * 1024 + np.arange(1024)
    pos_all = np.concatenate([own_p, own_s, oth_p, oth_s])
    d = np.arange(128)
    a, f = d // 64, d % 32
    inv = 10000.0 ** (-np.arange(0, 64, 2, dtype=np.float64) / 64)
    pa = np.where(a[:, None] == 0, pos_all[None, :] // 64, pos_all[None, :] % 64).astype(np.float64)
    ang = pa * inv[f][:, None]
    cosT = np.cos(ang).astype(np.float32)
    sinT = np.sin(ang).astype(np.float32)

    def dft(rows, cols, n, neg_sin):
        prod = (rows[:, None].astype(np.int64) * cols[None, :].astype(np.int64)) % n
        angm = 2.0 * np.pi * prod / n
        c = np.cos(angm) / np.sqrt(n)
        s = np.sin(angm) / np.sqrt(n)
        return c, (-s if neg_sin else s)

    kp_rows = np.concatenate([own_p, oth_p])
    c, s = dft(kp_rows, own_p, 4096, True)
    csp = np.stack([np.stack([_kpanel(mat[:, nb * 512:(nb + 1) * 512]) for nb in range(4)]) for mat in (c, s)]).astype(BF)
    ks_rows = np.concatenate([own_s, oth_s])
    c, s = dft(ks_rows, own_s, 2048, True)
    mat = np.concatenate([c, s], axis=0)
    css = np.stack([_kpanel(mat[:, nb * 512:(nb + 1) * 512]) for nb in range(2)]).astype(BF)
    return dict(cosT=cosT, sinT=sinT, csp=csp, css=css)


_CACHE = {}


def kernel(x_prompt, x_sample, mem_prompt, mem_sample, norm_mix, w_in, q_norm, k_norm, w_attn_br, w_four_br,
           w_gate, b_gate, w_out, norm_ca, mem_norm, w_cq, w_ckv, w_co, norm_ffn, w_pq, sub_keys, expert_u,
           expert_v, final_norm):
    f32 = lambda a: np.ascontiguousarray(np.asarray(a, np.float32))
    x_prompt, x_sample, mem_prompt, mem_sample = map(f32, (x_prompt, x_sample, mem_prompt, mem_sample))
    if "nc" not in _CACHE:
        _CACHE["nc"] = build()[0]
    nc = _CACHE["nc"]
    ch = np.arange(2048)
    prod = (ch[:, None] * ch[None, :]) % 2048
    angc = 2.0 * np.pi * prod / 2048
    cc = np.cos(angc) / np.sqrt(2048.0)
    sc = np.sin(angc) / np.sqrt(2048.0)
    ccs = np.stack([_kpanel(mat[:, cb * 512:(cb + 1) * 512]) for mat in (cc, sc) for cb in range(4)]).astype(BF)
    rm = np.zeros((128, 128), np.float32)
    for dout in range(128):
        if (dout % 64) < 32:
            rm[dout + 32, dout] = -1.0
        else:
            rm[dout - 32, dout] = 1.0
    shared = dict(
        w_in=_wp(w_in[0], 512), w_attn=_wp(w_attn_br[0], 256), w_four=_wp(w_four_br[0], 256), w_gate=_wp(w_gate[0], 256),
        w_out=_wp(w_out[0], 512), w_cq=_wp(w_cq[0], 512), w_ckv=_wp(w_ckv[0], 512), w_co=_wp(w_co[0], 512),
        w_pq=_wp(w_pq[0], 512),
        uT=np.ascontiguousarray(np.asarray(expert_u[0], np.float32).reshape(32, 512, 32, 128).transpose(0, 3, 2, 1)),
        ev=np.ascontiguousarray(np.asarray(expert_v[0], np.float32).reshape(4, 32, 128, 8, 512).transpose(3, 0, 2, 1, 4)),
        g_mix=_gain(norm_mix[0]), g_ca=_gain(norm_ca[0]), g_mem=_gain(mem_norm[0]), g_ffn=_gain(norm_ffn[0]),
        g_fin=_gain(final_norm), qn=f32(np.asarray(q_norm[0]).reshape(128, 1)), kn=f32(np.asarray(k_norm[0]).reshape(128, 1)),
        bg=_gain(b_gate[0]),
        subkT=np.ascontiguousarray(np.asarray(sub_keys[0], np.float32).transpose(2, 0, 1)),
        rmat=rm.astype(BF), ones=np.ones((128, 128), BF), ident=np.eye(128, dtype=np.float32).astype(BF), ccs=ccs,
    )
    pc = [_consts(0), _consts(1)]
    in_maps = []
    for c in range(8):
        b, hf = c // 2, c % 2
        op_ = slice(hf * 2048, (hf + 1) * 2048)
        tp_ = slice((1 - hf) * 2048, (2 - hf) * 2048)
        os_ = slice(hf * 1024, (hf + 1) * 1024)
        ts_ = slice((1 - hf) * 1024, (2 - hf) * 1024)
        xall = np.concatenate([x_prompt[b, op_], x_sample[b, os_], x_prompt[b, tp_], x_sample[b, ts_]], axis=0)
        mem = np.concatenate([mem_prompt[b], mem_sample[b]], axis=0)
        m = dict(shared)
        m.update(pc[hf])
        m["xT"] = _panels(xall, 32)
        m["memT"] = _panels(mem, 32)[0]
        in_maps.append(m)
    res = run_bass_kernel_spmd(nc, in_maps, core_ids=list(range(8)))
    y_prompt = np.empty_like(x_prompt)
    y_sample = np.empty_like(x_sample)
    for c in range(8):
        b, hf = c // 2, c % 2
        yT = np.asarray(res.results[c]["yT"], np.float32)
        y = yT.transpose(0, 3, 2, 1).reshape(TOWN, D)
        y_prompt[b, hf * 2048:(hf + 1) * 2048] = y[0:2048]
        y_sample[b, hf * 1024:(hf + 1) * 1024] = y[2048:3072]
    return (y_prompt, y_sample)
```

```python
import numpy as np
import ml_dtypes
from contextlib import ExitStack
import concourse.bass as bass
import concourse.mybir as mybir
from concourse.bass_utils import run_bass_kernel_spmd

F32 = mybir.dt.float32
BF16 = mybir.dt.bfloat16
AF = mybir.ActivationFunctionType
ALU = mybir.AluOpType
AX = mybir.AxisListType
BF = ml_dtypes.bfloat16

D = 4096
EPS = 1e-6
NPO = 6
NPA = 12
TOWN = 3072
TALL = 6144
ATT_SCALE = 128 ** -0.5
CA_SCALE = 256 ** -0.5


class Obj:
    _n = 0

    def __init__(self, name=""):
        Obj._n += 1
        self.id = Obj._n
        self.name = name
        self.w = {}
        self.r = {}


class Buf:
    def __init__(self, nc, st, name, shape, dtype):
        self.t = st.enter_context(nc.sbuf_tensor(name, list(shape), dtype))
        self.o = Obj(name)


class Ring:
    def __init__(self, items):
        self.items = items
        self.i = 0

    def next(self):
        it = self.items[self.i % len(self.items)]
        self.i += 1
        return it


class Sched:
    ENG = ("pe", "act", "dve", "pool", "sp")
    BLK = {"pe": "tensor", "act": "scalar", "dve": "vector", "pool": "gpsimd", "sp": "sync"}

    def __init__(self, nc, semstack):
        self.nc = nc
        self.semstack = semstack
        self.sems = {}
        self.ops = {e: [] for e in self.ENG}
        self.cnt = {}
        self.known = {e: {} for e in self.ENG}
        self.unsig = {e: False for e in self.ENG}
        self.nops = 0
        self.bufphys = {}
        self.free_phys = []
        self.nphys = 0

    def _collect(self, reads, writes, mykey, dma):
        d = {}
        for o in reads:
            for k, v in o.w.items():
                if v > d.get(k, 0):
                    d[k] = v
        for o in writes:
            for k, v in o.w.items():
                if dma and k == mykey:
                    continue
                if v > d.get(k, 0):
                    d[k] = v
            for k, v in o.r.items():
                if v > d.get(k, 0):
                    d[k] = v
        return d

    def _emit_waits(self, eng, deps, skipkey=None):
        kn = self.known[eng]
        lst = self.ops[eng]
        for k, v in deps.items():
            if k == skipkey:
                continue
            if kn.get(k, 0) >= v:
                continue
            lst.append((0, k, v))
            kn[k] = v

    def _record(self, reads, writes, key, c, merge=False):
        for o in reads:
            if o.r.get(key, 0) < c:
                o.r[key] = c
        for o in writes:
            if merge and key in o.w:
                o.w[key] = c
            else:
                o.w = {key: c}
            o.r = {}

    def op(self, eng, name, kw, reads=(), writes=(), signal=True, nosame=False):
        key = ("e", eng)
        deps = self._collect(reads, writes, key, False)
        self._emit_waits(eng, deps, skipkey=key if (eng == "pe" or nosame) else None)
        c = self.cnt.get(key, 0) + 1
        if signal:
            self.cnt[key] = c
            self.ops[eng].append((1, name, kw, key))
            self.unsig[eng] = False
        else:
            self.ops[eng].append((1, name, kw, None))
            self.unsig[eng] = True
        self._record(reads, writes, key, c)
        self.nops += 1

    def dma(self, q, out_ap, in_ap, semobj, reads=(), writes=()):
        oid = getattr(semobj, "o", semobj).id
        if oid not in self.bufphys:
            if self.free_phys:
                self.bufphys[oid] = self.free_phys.pop()
            else:
                self.bufphys[oid] = self.nphys
                self.nphys += 1
        key = ("b", self.bufphys[oid])
        deps = self._collect(reads, writes, key, True)
        self._emit_waits(q, deps)
        c = self.cnt.get(key, 0) + 16
        self.cnt[key] = c
        self.ops[q].append((2, out_ap, in_ap, key))
        self._record(reads, writes, key, c, merge=True)
        self.nops += 1

    def barrier(self):
        for e in self.ENG:
            assert not self.unsig[e], e
            self._emit_waits(e, dict(self.cnt))

    def flush(self, st):
        nc = self.nc
        for k in self.cnt:
            if k not in self.sems:
                self.sems[k] = self.semstack.enter_context(nc.semaphore("s%d" % len(self.sems)))
        sems = self.sems
        block = st.enter_context(nc.Block())
        for e in self.ENG:
            lst = self.ops[e]
            if not lst:
                continue

            def body(engine, lst=lst):
                for it in lst:
                    if it[0] == 0:
                        engine.wait_ge(sems[it[1]], it[2])
                    elif it[0] == 1:
                        ins = getattr(engine, it[1])(**it[2])
                        if it[3] is not None:
                            ins.then_inc(sems[it[3]], 1)
                    else:
                        engine.dma_start(out=it[1], in_=it[2]).then_inc(sems[it[3]], 16)

            getattr(block, self.BLK[e])(body)
        self.ops = {e: [] for e in self.ENG}
        self.free_phys = list(range(self.nphys))
        self.bufphys = {}


def build(stop_after=99, debug_out=()):
    nc = bass.Bass("TRN2", target_bir_lowering=False)

    def inp(name, shape, dt=F32):
        return nc.dram_tensor(name, list(shape), dt, kind="ExternalInput").ap()

    def scr(name, shape, dt=BF16):
        kind = "ExternalOutput" if name in debug_out else "Internal"
        return nc.dram_tensor(name, list(shape), dt, kind=kind).ap()

    xT = inp("xT", [NPA, 128, 32, 512])
    memT = inp("memT", [128, 32, 512])
    w_in = inp("w_in", [10, 128, 32, 512])
    w_attn = inp("w_attn", [16, 128, 16, 256])
    w_four = inp("w_four", [16, 128, 16, 256])
    w_gate = inp("w_gate", [32, 128, 32, 256])
    w_out = inp("w_out", [8, 128, 32, 512])
    w_cq = inp("w_cq", [2, 128, 32, 512])
    w_ckv = inp("w_ckv", [4, 128, 32, 512])
    w_co = inp("w_co", [8, 128, 8, 512])
    w_pq = inp("w_pq", [4, 128, 32, 512])
    uT = inp("uT", [32, 128, 32, 512])
    ev = inp("ev", [8, 4, 128, 32, 512])
    g_mix = inp("g_mix", [128, 32])
    g_ca = inp("g_ca", [128, 32])
    g_mem = inp("g_mem", [128, 32])
    g_ffn = inp("g_ffn", [128, 32])
    g_fin = inp("g_fin", [128, 32])
    qn = inp("qn", [128, 1])
    kn = inp("kn", [128, 1])
    bg = inp("bg", [128, 64])
    subkT = inp("subkT", [128, 2, 128])
    cosT = inp("cosT", [128, TALL])
    sinT = inp("sinT", [128, TALL])
    rmat = inp("rmat", [128, 128], BF16)
    ones_d = inp("ones", [128, 128], BF16)
    ident = inp("ident", [128, 128], BF16)
    ccs = inp("ccs", [8, 128, 16, 512], BF16)
    csp = inp("csp", [2, 4, 128, 32, 512], BF16)
    css = inp("css", [2, 128, 32, 512], BF16)
    yT = nc.dram_tensor("yT", [NPO, 128, 32, 512], F32, kind="ExternalOutput").ap()

    HT = scr("HT", [NPA, 128, 32, 512])
    QT = scr("QT", [16, 128, TOWN])
    KT = scr("KT", [4, 128, TALL])
    VS = scr("VS", [TALL, 512])
    FT = scr("FT", [NPA, 128, 16, 512])
    PQ = scr("PQ", [4, 2, TALL, 512])
    ZT = scr("ZT", [NPO, 128, 16, 512])
    AT = scr("AT", [NPO, 128, 16, 512])
    MT = scr("MT", [NPO, 128, 32, 512])
    X1T = scr("X1T", [NPO, 128, 32, 512], F32)
    HCT = scr("HCT", [NPO, 128, 32, 512])
    MNT = scr("MNT", [1, 128, 32, 512])
    QCT = scr("QCT", [8, 128, TOWN])
    KCT = scr("KCT", [8, 128, 512])
    VC = scr("VC", [512, 1024])
    OCT = scr("OCT", [NPO, 128, 8, 512])
    X2T = scr("X2T", [NPO, 128, 32, 512], F32)
    HPT = scr("HPT", [NPO, 128, 32, 512])
    QPT = scr("QPT", [16, 128, TOWN])
    WTp = scr("WTp", [NPO, 4, 128, 32, 512])
    GAT = scr("GAT", [NPO, 4, 128, 32, 512])
    X3T = scr("X3T", [NPO, 128, 32, 512], F32)

    outer = ExitStack()
    S = Sched(nc, outer)
    cnt = [0]

    def mkbuf(st, shape, dt, name="b"):
        cnt[0] += 1
        return Buf(nc, st, "%s%d" % (name, cnt[0]), shape, dt)

    def mkps(st, n, dt=F32, cols=512):
        out = []
        for i in range(n):
            cnt[0] += 1
            t = st.enter_context(nc.psum_tensor("ps%d" % cnt[0], [128, cols], dt))
            out.append((t, Obj("ps")))
        return out

    def mm(ps, po, pairs, reads, start=True, stop=True):
        n = len(pairs)
        for i, (l, r) in enumerate(pairs):
            last = i == n - 1
            S.op("pe", "matmul", dict(out=ps, lhsT=l, rhs=r, start=(start and i == 0), stop=(stop and last)),
                 reads=reads, writes=[po], signal=last)

    def load_w(buf, wpanel, kc):
        step = 8 if kc >= 8 else kc
        for c0 in range(0, kc, step):
            S.dma("pool", buf.t[:, c0:c0 + step, :], wpanel[:, c0:c0 + step, :], buf, writes=[buf.o])

    def consts(st, need_ones=True):
        ON = mkbuf(st, [128, 128], BF16, "ones")
        S.dma("sp", ON.t[:], ones_d, ON, writes=[ON.o])
        EP = mkbuf(st, [128, 1], F32, "eps")
        S.op("dve", "memset", dict(ap=EP.t[:], constant=EPS), writes=[EP.o])
        return ON, EP

    phase_idx = [0]

    def run_phase(fn):
        phase_idx[0] += 1
        if phase_idx[0] > stop_after:
            return
        with ExitStack() as st:
            fn(st)
            S.barrier()
            S.flush(st)

    def norm_phase(srcs, gain_ap, dsts, out_dt):
        def fn(st):
            ON, EP = consts(st)
            Xr = Ring([mkbuf(st, [128, 32, 256], F32, "nx") for _ in range(2)])
            SQr = Ring([mkbuf(st, [128, 32, 256], BF16, "nsq") for _ in range(2)])
            Or = Ring([mkbuf(st, [128, 32, 256], out_dt, "no") for _ in range(2)])
            RSr = Ring([mkbuf(st, [128, 256], F32, "nrs") for _ in range(2)])
            G = mkbuf(st, [128, 32], F32, "ng")
            PSr = Ring(mkps(st, 2))
            S.dma("sp", G.t[:], gain_ap, G, writes=[G.o])
            for src, dst in zip(srcs, dsts):
                for hf in range(2):
                    cs = slice(hf * 256, (hf + 1) * 256)
                    X, SQ, O, RS = Xr.next(), SQr.next(), Or.next(), RSr.next()
                    ps, po = PSr.next()
                    S.dma("sp", X.t[:], src[:, :, cs], X, writes=[X.o])
                    for q2 in range(2):
                        S.op("act", "activation", dict(out=SQ.t[:, q2 * 16:(q2 + 1) * 16, :], in_=X.t[:, q2 * 16:(q2 + 1) * 16, :],
                                                       func=AF.Square), reads=[X.o], writes=[SQ.o])
                    mm(ps[:, 0:256], po, [(ON.t[:], SQ.t[:, c, :]) for c in range(32)], reads=[ON.o, SQ.o])
                    S.op("act", "activation", dict(out=RS.t[:], in_=ps[:, 0:256], func=AF.Sqrt, bias=EP.t[:, 0:1], scale=1.0 / D),
                         reads=[po, EP.o], writes=[RS.o])
                    S.op("dve", "reciprocal", dict(out=RS.t[:], in_=RS.t[:]), reads=[RS.o], writes=[RS.o])
                    for q4 in range(4):
                        c8 = slice(q4 * 8, (q4 + 1) * 8)
                        S.op("pool", "tensor_tensor",
                             dict(out=X.t[:, c8, :], in0=X.t[:, c8, :],
                                  in1=G.t[:, c8].unsqueeze(2).broadcast_to([128, 8, 256]), op=ALU.mult),
                             reads=[X.o, G.o], writes=[X.o], nosame=(q4 > 0))
                    for q4 in range(4):
                        c8 = slice(q4 * 8, (q4 + 1) * 8)
                        S.op("dve", "tensor_tensor",
                             dict(out=O.t[:, c8, :], in0=X.t[:, c8, :],
                                  in1=RS.t[:].unsqueeze(1).broadcast_to([128, 8, 256]), op=ALU.mult),
                             reads=[X.o, RS.o], writes=[O.o], nosame=(q4 > 0))
                    S.dma("act", dst[:, :, cs], O.t[:], O, reads=[O.o])
        return fn

    def p2(st):
        ON, EP = consts(st)
        A = [mkbuf(st, [128, 32, 512], BF16, "A") for _ in range(2)]
        HB = Ring([mkbuf(st, [128, 32, 512], BF16, "HB") for _ in range(2)])
        CS = Ring([mkbuf(st, [128, 2, 512], F32, "CS") for _ in range(2)])
        RM = mkbuf(st, [128, 128], BF16, "RM")
        QN = mkbuf(st, [128, 1], F32, "QN")
        KN = mkbuf(st, [128, 1], F32, "KN")
        S.dma("sp", RM.t[:], rmat, RM, writes=[RM.o])
        S.dma("sp", QN.t[:], qn, QN, writes=[QN.o])
        S.dma("sp", KN.t[:], kn, KN, writes=[KN.o])
        EV = Ring([mkbuf(st, [128, 512], BF16, "EV") for _ in range(3)])
        XQ = Ring([mkbuf(st, [128, 512], F32, "XQ") for _ in range(4)])
        SQ = Ring([mkbuf(st, [128, 512], BF16, "SQ") for _ in range(4)])
        RS = Ring([mkbuf(st, [128, 512], F32, "RS") for _ in range(2)])
        XN = Ring([mkbuf(st, [128, 512], BF16, "XN") for _ in range(4)])
        T1 = Ring([mkbuf(st, [128, 512], F32, "T1") for _ in range(2)])
        T2 = Ring([mkbuf(st, [128, 512], F32, "T2") for _ in range(2)])
        OB = Ring([mkbuf(st, [128, 512], BF16, "OB") for _ in range(3)])
        pss = mkps(st, 8)
        PSM = Ring(pss[0:4])
        PS2 = Ring(pss[4:6])
        PS3 = Ring(pss[6:8])

        def stage_a(job):
            ps, po = job["ps"], job["po"]
            xq, sq = XQ.next(), SQ.next()
            S.op("act", "activation", dict(out=xq.t[:], in_=ps[:], func=AF.Copy), reads=[po], writes=[xq.o])
            S.op("act", "activation", dict(out=sq.t[:], in_=ps[:], func=AF.Square), reads=[po], writes=[sq.o])
            job["xq"], job["sq"] = xq, sq

        def stage_b(job):
            xq, sq, gain = job["xq"], job["sq"], job["gain"]
            ps2, po2 = PS2.next()
            mm(ps2[:], po2, [(ON.t[:], sq.t[:])], reads=[ON.o, sq.o])
            rs, xn = RS.next(), XN.next()
            S.op("act", "activation", dict(out=rs.t[:], in_=ps2[:], func=AF.Sqrt, bias=EP.t[:, 0:1], scale=1.0 / 128),
                 reads=[po2, EP.o], writes=[rs.o])
            S.op("dve", "reciprocal", dict(out=rs.t[:], in_=rs.t[:]), reads=[rs.o], writes=[rs.o])
            S.op("dve", "scalar_tensor_tensor", dict(out=xn.t[:], in0=xq.t[:], scalar=gain.t[:, 0:1], in1=rs.t[:],
                                                     op0=ALU.mult, op1=ALU.mult),
                 reads=[xq.o, gain.o, rs.o], writes=[xn.o])
            job["xn"] = xn

        def stage_c(job):
            xn, CSb, dst = job["xn"], job["cs"], job["dst"]
            ps3, po3 = PS3.next()
            mm(ps3[:], po3, [(RM.t[:], xn.t[:])], reads=[RM.o, xn.o])
            t1, t2, ob = T1.next(), T2.next(), OB.next()
            S.op("dve", "tensor_tensor", dict(out=t1.t[:], in0=xn.t[:], in1=CSb.t[:, 0, :], op=ALU.mult),
                 reads=[xn.o, CSb.o], writes=[t1.o])
            S.op("dve", "tensor_tensor", dict(out=t2.t[:], in0=ps3[:], in1=CSb.t[:, 1, :], op=ALU.mult),
                 reads=[po3, CSb.o], writes=[t2.o])
            S.op("pool", "tensor_tensor", dict(out=ob.t[:], in0=t1.t[:], in1=t2.t[:], op=ALU.add),
                 reads=[t1.o, t2.o], writes=[ob.o])
            S.dma("pool", dst, ob.t[:], ob, reads=[ob.o])

        pend = []

        def advance(newjob):
            for job in list(pend):
                job["age"] += 1
                if job["age"] == 1:
                    stage_b(job)
                elif job["age"] == 2:
                    stage_c(job)
                    pend.remove(job)
            if newjob is not None:
                stage_a(newjob)
                newjob["age"] = 0
                pend.append(newjob)

        load_w(A[0], w_in[0], 32)
        for ap in range(10):
            kind = "q" if ap < 4 else "k" if ap == 4 else "v" if ap == 5 else "f"
            Ab = A[ap % 2]
            if ap + 1 < 10:
                load_w(A[(ap + 1) % 2], w_in[ap + 1], 32)
            nbs = range(NPO) if kind == "q" else range(NPA)
            for nb in nbs:
                Hb = HB.next()
                S.dma("sp", Hb.t[:], HT[nb], Hb, writes=[Hb.o])
                if kind in "qk":
                    CSb = CS.next()
                    S.dma("sp", CSb.t[:, 0, :], cosT[:, nb * 512:(nb + 1) * 512], CSb, writes=[CSb.o])
                    S.dma("sp", CSb.t[:, 1, :], sinT[:, nb * 512:(nb + 1) * 512], CSb, writes=[CSb.o])
                for m in range(4):
                    ps, po = PSM.next()
                    if kind == "v":
                        mm(ps[:], po, [(Hb.t[:, c, m * 128:(m + 1) * 128], Ab.t[:, c, :]) for c in range(32)],
                           reads=[Hb.o, Ab.o])
                        advance(None)
                        Eb = EV.next()
                        S.op("act", "activation", dict(out=Eb.t[:], in_=ps[:], func=AF.Copy), reads=[po], writes=[Eb.o])
                        S.dma("pool", VS[nb * 512 + m * 128:nb * 512 + (m + 1) * 128, :], Eb.t[:], Eb, reads=[Eb.o])
                        continue
                    mm(ps[:], po, [(Ab.t[:, c, m * 128:(m + 1) * 128], Hb.t[:, c, :]) for c in range(32)],
                       reads=[Hb.o, Ab.o])
                    if kind == "f":
                        advance(None)
                        Eb = EV.next()
                        S.op("act", "activation", dict(out=Eb.t[:], in_=ps[:], func=AF.Copy), reads=[po], writes=[Eb.o])
                        S.dma("pool", FT[nb, :, (ap - 6) * 4 + m, :], Eb.t[:], Eb, reads=[Eb.o])
                        continue
                    gain = QN if kind == "q" else KN
                    dst = QT[ap * 4 + m, :, nb * 512:(nb + 1) * 512] if kind == "q" else KT[m, :, nb * 512:(nb + 1) * 512]
                    advance(dict(ps=ps, po=po, gain=gain, dst=dst, cs=CSb))
        while pend:
            advance(None)

    def p3(st):
        Bp = [mkbuf(st, [128, 16, 512], BF16, "Bp") for _ in range(2)]
        Fb = Ring([mkbuf(st, [128, 16, 512], BF16, "Fb") for _ in range(2)])
        EV = Ring([mkbuf(st, [128, 512], BF16, "EV") for _ in range(4)])
        PSM = Ring(mkps(st, 4))
        k = 0
        for pi in range(8):
            pq, cb = pi // 4, pi % 4
            B_ = Bp[pi % 2]
            S.dma("sp", B_.t[:], ccs[pi], B_, writes=[B_.o])
            for nb in range(NPA):
                F_ = Fb.next()
                S.dma("sp", F_.t[:], FT[nb], F_, writes=[F_.o])
                for m in range(4):
                    ps, po = PSM.next()
                    mm(ps[:], po, [(F_.t[:, c, m * 128:(m + 1) * 128], B_.t[:, c, :]) for c in range(16)],
                       reads=[F_.o, B_.o])
                    Eb = EV.next()
                    k += 1
                    if k % 2:
                        S.op("act", "activation", dict(out=Eb.t[:], in_=ps[:], func=AF.Copy), reads=[po], writes=[Eb.o])
                    else:
                        S.op("dve", "tensor_copy", dict(out=Eb.t[:], in_=ps[:]), reads=[po], writes=[Eb.o])
                    r0 = nb * 512 + m * 128
                    S.dma("pool", PQ[cb, pq, r0:r0 + 128, :], Eb.t[:], Eb, reads=[Eb.o])

    def p4(st):
        Aa = [mkbuf(st, [128, 32, 512], BF16, "Aa") for _ in range(2)]
        Bb = Ring([mkbuf(st, [128, 32, 512], BF16, "Bb") for _ in range(3)])
        EV = Ring([mkbuf(st, [128, 512], BF16, "EV") for _ in range(4)])
        pss = mkps(st, 8)
        GR = Ring([pss[0:4], pss[4:8]])
        k = 0

        def rows(cb, pq, r0, n):
            return PQ[cb, pq, r0:r0 + n, :].rearrange("(c p) n -> p c n", p=128)

        def epi(grp, nbp, cb):
            nonlocal k
            for m in range(4):
                ps, po = grp[m]
                Eb = EV.next()
                k += 1
                if k % 2:
                    S.op("act", "activation", dict(out=Eb.t[:], in_=ps[:], func=AF.Copy), reads=[po], writes=[Eb.o])
                else:
                    S.op("dve", "tensor_copy", dict(out=Eb.t[:], in_=ps[:]), reads=[po], writes=[Eb.o])
                S.dma("pool", ZT[nbp, :, cb * 4 + m, :], Eb.t[:], Eb, reads=[Eb.o])

        for cb in range(4):
            for kp in range(2):
                S.dma("sp", Aa[kp].t[:, 0:16, :], rows(cb, kp, 0, 2048), Aa[kp], writes=[Aa[kp].o])
                S.dma("sp", Aa[kp].t[:, 16:32, :], rows(cb, kp, 3072, 2048), Aa[kp], writes=[Aa[kp].o])
            for nb in range(4):
                grp = GR.next()
                for kp in range(2):
                    Bk = Bb.next()
                    S.dma("sp", Bk.t[:], csp[kp, nb], Bk, writes=[Bk.o])
                    for m in range(4):
                        ps, po = grp[m]
                        mm(ps[:], po, [(Aa[kp].t[:, c, m * 128:(m + 1) * 128], Bk.t[:, c, :]) for c in range(32)],
                           reads=[Aa[kp].o, Bk.o], start=(kp == 0), stop=(kp == 1))
                epi(grp, nb, cb)
        for cb in range(4):
            A0 = Aa[cb % 2]
            S.dma("sp", A0.t[:, 0:8, :], rows(cb, 0, 2048, 1024), A0, writes=[A0.o])
            S.dma("sp", A0.t[:, 8:16, :], rows(cb, 0, 5120, 1024), A0, writes=[A0.o])
            S.dma("sp", A0.t[:, 16:24, :], rows(cb, 1, 2048, 1024), A0, writes=[A0.o])
            S.dma("sp", A0.t[:, 24:32, :], rows(cb, 1, 5120, 1024), A0, writes=[A0.o])
            for nb in range(2):
                grp = GR.next()
                Bk = Bb.next()
                S.dma("sp", Bk.t[:], css[nb], Bk, writes=[Bk.o])
                for m in range(4):
                    ps, po = grp[m]
                    mm(ps[:], po, [(A0.t[:, c, m * 128:(m + 1) * 128], Bk.t[:, c, :]) for c in range(32)],
                       reads=[A0.o, Bk.o])
                epi(grp, 4 + nb, cb)

    def p5(st):
        ON, EP = consts(st)
        KTg = mkbuf(st, [128, 4096], BF16, "KTg")
        Vg = mkbuf(st, [128, 32, 128], BF16, "Vg")
        QG = mkbuf(st, [128, 4, 2048], BF16, "QG")
        ER = Ring([mkbuf(st, [128, 512], BF16, "E") for _ in range(3)])
        RZ = Ring([mkbuf(st, [128, 512], F32, "RZ") for _ in range(2)])
        OB = Ring([mkbuf(st, [128, 4, 128], BF16, "OB") for _ in range(2)])
        pss = mkps(st, 6)
        PSS = Ring(pss[0:2])
        PSO = Ring(pss[2:4])
        PSZ = Ring(pss[4:6])
        seqs = [((0, 2048), (3072, 5120), 0, 2048, 0), ((2048, 3072), (5120, 6144), 2048, 1024, 4)]
        for (o0, o1), (t0, t1), q0, nq, pan0 in seqs:
            hk = o1 - o0
            nch = 2 * hk // 128
            for g in range(4):
                S.dma("sp", KTg.t[:, 0:hk], KT[g, :, o0:o1], KTg, writes=[KTg.o])
                S.dma("sp", KTg.t[:, hk:2 * hk], KT[g, :, t0:t1], KTg, writes=[KTg.o])
                S.dma("sp", Vg.t[:, 0:nch // 2, :], VS[o0:o1, g * 128:(g + 1) * 128].rearrange("(c p) d -> p c d", p=128),
                      Vg, writes=[Vg.o])
                S.dma("sp", Vg.t[:, nch // 2:nch, :], VS[t0:t1, g * 128:(g + 1) * 128].rearrange("(c p) d -> p c d", p=128),
                      Vg, writes=[Vg.o])
                S.dma("sp", QG.t[:, :, 0:nq], QT[g * 4:(g + 1) * 4, :, q0:q0 + nq].rearrange("h d q -> d h q"),
                      QG, writes=[QG.o])
                for qb in range(nq // 128):
                    rhsq = QG.t[:, :, qb * 128:(qb + 1) * 128]
                    pso, poo = PSO.next()
                    psz, poz = PSZ.next()

                    def issue_s(sc):
                        ps, po = PSS.next()
                        mm(ps[:], po, [(KTg.t[:, sc * 128:(sc + 1) * 128], rhsq)], reads=[KTg.o, QG.o])
                        Eb = ER.next()
                        S.op("act", "activation", dict(out=Eb.t[:], in_=ps[:], func=AF.Exp, scale=ATT_SCALE),
                             reads=[po], writes=[Eb.o])
                        return Eb

                    Ecur = issue_s(0)
                    for sc in range(nch):
                        Enext = issue_s(sc + 1) if sc + 1 < nch else None
                        mm(pso[:], poo, [(Vg.t[:, sc, :], Ecur.t[:])], reads=[Vg.o, Ecur.o],
                           start=(sc == 0), stop=(sc == nch - 1))
                        mm(psz[:], poz, [(ON.t[:], Ecur.t[:])], reads=[ON.o, Ecur.o],
                           start=(sc == 0), stop=(sc == nch - 1))
                        Ecur = Enext
                    rz, ob = RZ.next(), OB.next()
                    S.op("dve", "reciprocal", dict(out=rz.t[:], in_=psz[:]), reads=[poz], writes=[rz.o])
                    S.op("dve", "tensor_tensor", dict(out=ob.t[:].rearrange("p h q -> p (h q)"), in0=pso[:], in1=rz.t[:],
                                                      op=ALU.mult), reads=[poo, rz.o], writes=[ob.o])
                    tq = qb * 128
                    S.dma("pool", AT[pan0 + tq // 512, :, g * 4:(g + 1) * 4, tq % 512:tq % 512 + 128], ob.t[:], ob,
                          reads=[ob.o])

    def p6(st):
        Wa = mkbuf(st, [128, 16, 256], BF16, "Wa")
        Wf = mkbuf(st, [128, 16, 256], BF16, "Wf")
        Wg0 = mkbuf(st, [128, 32, 256], BF16, "Wg0")
        Wg1 = mkbuf(st, [128, 32, 256], BF16, "Wg1")
        ATb = Ring([mkbuf(st, [128, 16, 512], BF16, "ATb") for _ in range(2)])
        ZTb = Ring([mkbuf(st, [128, 16, 512], BF16, "ZTb") for _ in range(2)])
        HBb = Ring([mkbuf(st, [128, 32, 512], BF16, "HBb") for _ in range(2)])
        BG = mkbuf(st, [128, 64], F32, "BG")
        S.dma("sp", BG.t[:], bg, BG, writes=[BG.o])
        S0 = Ring([mkbuf(st, [128, 512], F32, "S0") for _ in range(2)])
        S1 = Ring([mkbuf(st, [128, 512], F32, "S1") for _ in range(2)])
        MO = Ring([mkbuf(st, [128, 512], BF16, "MO") for _ in range(2)])
        pss = mkps(st, 8)
        GR = Ring([pss[0:4], pss[4:8]])
        for mb2 in range(16):
            load_w(Wa, w_attn[mb2], 16)
            load_w(Wf, w_four[mb2], 16)
            load_w(Wg0, w_gate[mb2], 32)
            load_w(Wg1, w_gate[16 + mb2], 32)
            for nb in range(NPO):
                a_, z_, h_ = ATb.next(), ZTb.next(), HBb.next()
                S.dma("sp", a_.t[:], AT[nb], a_, writes=[a_.o])
                S.dma("sp", z_.t[:], ZT[nb], z_, writes=[z_.o])
                S.dma("sp", h_.t[:], HT[nb], h_, writes=[h_.o])
                for mi in range(2):
                    m = mb2 * 2 + mi
                    (pa, oa), (pf, of), (pg0, og0), (pg1, og1) = GR.next()
                    cs = slice(mi * 128, (mi + 1) * 128)
                    mm(pa[:], oa, [(Wa.t[:, c, cs], a_.t[:, c, :]) for c in range(16)], reads=[Wa.o, a_.o])
                    mm(pf[:], of, [(Wf.t[:, c, cs], z_.t[:, c, :]) for c in range(16)], reads=[Wf.o, z_.o])
                    mm(pg0[:], og0, [(Wg0.t[:, c, cs], h_.t[:, c, :]) for c in range(32)], reads=[Wg0.o, h_.o])
                    mm(pg1[:], og1, [(Wg1.t[:, c, cs], h_.t[:, c, :]) for c in range(32)], reads=[Wg1.o, h_.o])
                    s0, s1, mo = S0.next(), S1.next(), MO.next()
                    t0, t1 = s0, s1
                    S.op("act", "activation", dict(out=s0.t[:], in_=pg0[:], func=AF.Sigmoid, bias=BG.t[:, m:m + 1], scale=1.0),
                         reads=[og0, BG.o], writes=[s0.o])
                    S.op("act", "activation", dict(out=s1.t[:], in_=pg1[:], func=AF.Sigmoid, bias=BG.t[:, 32 + m:33 + m], scale=1.0),
                         reads=[og1, BG.o], writes=[s1.o])
                    S.op("dve", "tensor_tensor", dict(out=t0.t[:], in0=pa[:], in1=s0.t[:], op=ALU.mult),
                         reads=[oa, s0.o], writes=[t0.o])
                    S.op("dve", "tensor_tensor", dict(out=t1.t[:], in0=pf[:], in1=s1.t[:], op=ALU.mult),
                         reads=[of, s1.o], writes=[t1.o])
                    S.op("pool", "tensor_tensor", dict(out=mo.t[:], in0=t0.t[:], in1=t1.t[:], op=ALU.add),
                         reads=[t0.o, t1.o], writes=[mo.o])
                    S.dma("pool", MT[nb, :, m, :], mo.t[:], mo, reads=[mo.o])

    def resid_gemm(W, kcw, Bsrc, resid, dst):
        def fn(st):
            A = [mkbuf(st, [128, kcw, 512], BF16, "A") for _ in range(2)]
            Bb = Ring([mkbuf(st, [128, kcw, 512], BF16, "B") for _ in range(2)])
            XR = Ring([mkbuf(st, [128, 512], F32, "XR") for _ in range(3)])
            XO = Ring([mkbuf(st, [128, 512], F32, "XO") for _ in range(3)])
            PSM = Ring(mkps(st, 4))
            load_w(A[0], W[0], kcw)
            for ap in range(8):
                Ab = A[ap % 2]
                if ap + 1 < 8:
                    load_w(A[(ap + 1) % 2], W[ap + 1], kcw)
                for nb in range(NPO):
                    B_ = Bb.next()
                    S.dma("sp", B_.t[:], Bsrc[nb], B_, writes=[B_.o])
                    for mi in range(4):
                        m = ap * 4 + mi
                        ps, po = PSM.next()
                        mm(ps[:], po, [(Ab.t[:, c, mi * 128:(mi + 1) * 128], B_.t[:, c, :]) for c in range(kcw)],
                           reads=[Ab.o, B_.o])
                        xr, xo = XR.next(), XO.next()
                        S.dma("sp", xr.t[:], resid[nb, :, m, :], xr, writes=[xr.o])
                        S.op("dve", "tensor_tensor", dict(out=xo.t[:], in0=ps[:], in1=xr.t[:], op=ALU.add),
                             reads=[po, xr.o], writes=[xo.o])
                        S.dma("pool", dst[nb, :, m, :], xo.t[:], xo, reads=[xo.o])
        return fn

    def proj_fm(W, naps, Bsrc, nbs, dst, woff=0):
        def fn(st):
            A = [mkbuf(st, [128, 32, 512], BF16, "A") for _ in range(2)]
            Bb = Ring([mkbuf(st, [128, 32, 512], BF16, "B") for _ in range(2)])
            EV = Ring([mkbuf(st, [128, 512], BF16, "EV") for _ in range(4)])
            PSM = Ring(mkps(st, 4))
            k = 0
            load_w(A[0], W[woff], 32)
            for ap in range(naps):
                Ab = A[ap % 2]
                if ap + 1 < naps:
                    load_w(A[(ap + 1) % 2], W[woff + ap + 1], 32)
                for nb in range(nbs):
                    B_ = Bb.next()
                    S.dma("sp", B_.t[:], Bsrc[nb], B_, writes=[B_.o])
                    for mi in range(4):
                        ps, po = PSM.next()
                        mm(ps[:], po, [(Ab.t[:, c, mi * 128:(mi + 1) * 128], B_.t[:, c, :]) for c in range(32)],
                           reads=[Ab.o, B_.o])
                        Eb = EV.next()
                        k += 1
                        if k % 2:
                            S.op("act", "activation", dict(out=Eb.t[:], in_=ps[:], func=AF.Copy), reads=[po], writes=[Eb.o])
                        else:
                            S.op("dve", "tensor_copy", dict(out=Eb.t[:], in_=ps[:]), reads=[po], writes=[Eb.o])
                        S.dma("pool", dst[ap * 4 + mi, :, nb * 512:(nb + 1) * 512], Eb.t[:], Eb, reads=[Eb.o])
        return fn

    def p8v(st):
        A = [mkbuf(st, [128, 32, 512], BF16, "A") for _ in range(2)]
        B_ = mkbuf(st, [128, 32, 512], BF16, "B")
        EV = Ring([mkbuf(st, [128, 512], BF16, "EV") for _ in range(4)])
        PSM = Ring(mkps(st, 4))
        S.dma("sp", B_.t[:], MNT[0], B_, writes=[B_.o])
        for ap in range(2):
            load_w(A[ap], w_ckv[2 + ap], 32)
        for ap in range(2):
            for mi in range(4):
                ps, po = PSM.next()
                mm(ps[:], po, [(B_.t[:, c, mi * 128:(mi + 1) * 128], A[ap].t[:, c, :]) for c in range(32)],
                   reads=[A[ap].o, B_.o])
                Eb = EV.next()
                S.op("act", "activation", dict(out=Eb.t[:], in_=ps[:], func=AF.Copy), reads=[po], writes=[Eb.o])
                S.dma("pool", VC[mi * 128:(mi + 1) * 128, ap * 512:(ap + 1) * 512], Eb.t[:], Eb, reads=[Eb.o])

    def p8c(st):
        ON, EP = consts(st)
        KCb = mkbuf(st, [128, 8, 512], BF16, "KCb")
        QCb = mkbuf(st, [128, 8, TOWN], BF16, "QCb")
        VCb = mkbuf(st, [128, 4, 1024], BF16, "VCb")
        S.dma("sp", KCb.t[:], KCT.rearrange("b d m -> d b m"), KCb, writes=[KCb.o])
        S.dma("sp", QCb.t[:], QCT.rearrange("b d t -> d b t"), QCb, writes=[QCb.o])
        S.dma("sp", VCb.t[:], VC.rearrange("(c p) n -> p c n", p=128), VCb, writes=[VCb.o])
        ER = Ring([mkbuf(st, [128, 512], BF16, "E") for _ in range(4)])
        RZ = Ring([mkbuf(st, [128, 512], F32, "RZ") for _ in range(2)])
        OB = Ring([mkbuf(st, [128, 512], BF16, "OB") for _ in range(4)])
        pss = mkps(st, 8)
        PSS = Ring(pss[0:2])
        PSO = Ring(pss[2:6])
        PSZ = Ring(pss[6:8])
        for nb in range(NPO):
            mc0 = 0 if nb < 4 else 2
            for hc in range(4):
                Es = []
                for mc in range(2):
                    ps, po = PSS.next()
                    ms = slice((mc0 + mc) * 128, (mc0 + mc + 1) * 128)
                    mm(ps[:], po, [(KCb.t[:, hc * 2 + db, ms], QCb.t[:, hc * 2 + db, nb * 512:(nb + 1) * 512]) for db in range(2)],
                       reads=[KCb.o, QCb.o])
                    Eb = ER.next()
                    S.op("act", "activation", dict(out=Eb.t[:], in_=ps[:], func=AF.Exp, scale=CA_SCALE), reads=[po], writes=[Eb.o])
                    Es.append(Eb)
                psz, poz = PSZ.next()
                mm(psz[:], poz, [(ON.t[:], Es[mc].t[:]) for mc in range(2)], reads=[ON.o, Es[0].o, Es[1].o])
                rz = RZ.next()
                S.op("dve", "reciprocal", dict(out=rz.t[:], in_=psz[:]), reads=[poz], writes=[rz.o])
                for dvb in range(2):
                    pso, poo = PSO.next()
                    cs = slice(hc * 256 + dvb * 128, hc * 256 + (dvb + 1) * 128)
                    mm(pso[:], poo, [(VCb.t[:, mc0 + mc, cs], Es[mc].t[:]) for mc in range(2)],
                       reads=[VCb.o, Es[0].o, Es[1].o])
                    ob = OB.next()
                    S.op("dve", "tensor_tensor", dict(out=ob.t[:], in0=pso[:], in1=rz.t[:], op=ALU.mult),
                         reads=[poo, rz.o], writes=[ob.o])
                    S.dma("pool", OCT[nb, :, hc * 2 + dvb, :], ob.t[:], ob, reads=[ob.o])

    def p9cd(st):
        SUBK = mkbuf(st, [128, 2, 128], BF16, "SUBK")
        S.dma("pool", SUBK.t[:], subkT, SUBK, writes=[SUBK.o])
        IDB = mkbuf(st, [128, 128], BF16, "IDB")
        S.dma("sp", IDB.t[:], ident, IDB, writes=[IDB.o])
        Q = mkbuf(st, [128, 16, 128], BF16, "Q16")
        SK = mkbuf(st, [128, 16, 128], F32, "SK")
        TOP = mkbuf(st, [128, 16, 16], F32, "TOP")
        B16 = mkbuf(st, [128, 8, 16], F32, "B16")
        JUNK = mkbuf(st, [128, 16], F32, "JUNK")
        NEGM = mkbuf(st, [128, 8], F32, "NEGM")
        ZS = mkbuf(st, [128, 8], F32, "ZS")
        LNZ = mkbuf(st, [128, 8], F32, "LNZ")
        BIAS = mkbuf(st, [128, 8], F32, "BIAS")
        TAUP = mkbuf(st, [128, 8], F32, "TAUP")
        S1B = mkbuf(st, [128, 8, 128], F32, "S1B")
        Xr = Ring([mkbuf(st, [128, 8, 2, 128], F32, "X") for _ in range(3)])
        Er = Ring([mkbuf(st, [128, 8, 256], BF16, "E") for _ in range(6)])
        WTSr = Ring([mkbuf(st, [128, 16, 128], BF16, "WTS") for _ in range(2)])
        pssk = mkps(st, 2)
        pstr = Ring(mkps(st, 2, F32, 256))
        A = [mkbuf(st, [128, 32, 512], BF16, "A") for _ in range(2)]
        Bb = Ring([mkbuf(st, [128, 32, 512], BF16, "B") for _ in range(2)])
        GA = Ring([mkbuf(st, [128, 512], BF16, "GA") for _ in range(4)])
        PSM = Ring(mkps(st, 4))
        LAG = 3

        def gen_c():
            for tt in range(24):
                nb, tsub = tt // 4, tt % 4
                S.dma("sp", Q.t[:], QPT[:, :, tt * 128:(tt + 1) * 128].rearrange("b d t -> d b t"), Q, writes=[Q.o])
                for half in range(2):
                    for h8 in range(8):
                        hc = half * 8 + h8
                        ps, po = pssk[h8 // 4]
                        mm(ps[:, (h8 % 4) * 128:(h8 % 4 + 1) * 128], po, [(Q.t[:, hc, :], SUBK.t[:, hc % 2, :])],
                           reads=[Q.o, SUBK.o])
                    for bk in range(2):
                        ps, po = pssk[bk]
                        c0 = half * 8 + bk * 4
                        S.op("act", "activation", dict(out=SK.t[:, c0:c0 + 4, :].rearrange("p a b -> p (a b)"), in_=ps[:],
                                                       func=AF.Copy), reads=[po], writes=[SK.o])
                yield
                X0, X1 = Xr.items[0], Xr.items[1]
                SKRv = X0.t[:].rearrange("p h i j -> p (h i) j")
                CNv = X1.t[:].rearrange("p h i j -> p h (i j)")
                for hc in range(16):
                    S.op("dve", "max", dict(out=TOP.t[:, hc, 0:8], in_=SK.t[:, hc, :]), reads=[SK.o], writes=[TOP.o], nosame=True)
                for hc in range(16):
                    S.op("dve", "match_replace", dict(out=SKRv[:, hc, :], in_to_replace=TOP.t[:, hc, 0:8], in_values=SK.t[:, hc, :],
                                                      imm_value=-1e30), reads=[SK.o, TOP.o], writes=[X0.o], nosame=(hc > 0))
                yield
                for hc in range(16):
                    S.op("dve", "max", dict(out=TOP.t[:, hc, 8:16], in_=SKRv[:, hc, :]), reads=[X0.o], writes=[TOP.o], nosame=(hc > 0))
                yield
                for hh in range(2):
                    for h4 in range(4):
                        h = hh * 4 + h4
                        a_ = TOP.t[:, 2 * h, :].unsqueeze(2).broadcast_to([128, 16, 16])
                        b_ = TOP.t[:, 2 * h + 1, :].unsqueeze(1).broadcast_to([128, 16, 16])
                        S.op("dve", "tensor_tensor", dict(out=CNv[:, h4, :].rearrange("p (a b) -> p a b", a=16), in0=a_, in1=b_, op=ALU.add),
                             reads=[TOP.o], writes=[X1.o], nosame=(h4 > 0))
                    for h4 in range(4):
                        h = hh * 4 + h4
                        S.op("dve", "max", dict(out=B16.t[:, h, 0:8], in_=CNv[:, h4, :]), reads=[X1.o], writes=[B16.o], nosame=(h4 > 0))
                    for h4 in range(4):
                        h = hh * 4 + h4
                        S.op("dve", "match_replace", dict(out=CNv[:, 4 + h4, :], in_to_replace=B16.t[:, h, 0:8], in_values=CNv[:, h4, :],
                                                          imm_value=-1e30), reads=[X1.o, B16.o], writes=[X1.o], nosame=(h4 > 0))
                    for h4 in range(4):
                        h = hh * 4 + h4
                        S.op("dve", "max", dict(out=B16.t[:, h, 8:16], in_=CNv[:, 4 + h4, :]), reads=[X1.o], writes=[B16.o], nosame=(h4 > 0))
                    yield
                S.op("dve", "tensor_scalar", dict(out=NEGM.t[:], in0=B16.t[:, :, 0], scalar1=-1.0, scalar2=0.0,
                                                  op0=ALU.mult, op1=ALU.add), reads=[B16.o], writes=[NEGM.o])
                S.op("dve", "memset", dict(ap=ZS.t[:], constant=0.0), writes=[ZS.o])
                for h in range(8):
                    S.op("act", "activation", dict(out=JUNK.t[:], in_=B16.t[:, h, :], func=AF.Exp, bias=NEGM.t[:, h:h + 1],
                                                   scale=1.0, accum_out=ZS.t[:, h:h + 1]),
                         reads=[B16.o, NEGM.o], writes=[JUNK.o, ZS.o], nosame=(h > 0))
                S.op("act", "activation", dict(out=LNZ.t[:], in_=ZS.t[:], func=AF.Ln), reads=[ZS.o], writes=[LNZ.o])
                S.op("dve", "tensor_tensor", dict(out=BIAS.t[:], in0=NEGM.t[:], in1=LNZ.t[:], op=ALU.subtract),
                     reads=[NEGM.o, LNZ.o], writes=[BIAS.o])
                S.op("dve", "tensor_tensor", dict(out=TAUP.t[:], in0=B16.t[:, :, 15], in1=BIAS.t[:], op=ALU.add),
                     reads=[B16.o, BIAS.o], writes=[TAUP.o])
                S.op("dve", "tensor_scalar", dict(out=TAUP.t[:], in0=TAUP.t[:], scalar1=1.0, scalar2=-2e-5,
                                                  op0=ALU.mult, op1=ALU.add), reads=[TAUP.o], writes=[TAUP.o])
                skv = SK.t[:].rearrange("p (h c) n -> p h c n", c=2)
                S.op("dve", "tensor_tensor", dict(out=S1B.t[:], in0=skv[:, :, 0, :],
                                                  in1=BIAS.t[:].unsqueeze(2).broadcast_to([128, 8, 128]), op=ALU.add),
                     reads=[SK.o, BIAS.o], writes=[S1B.o])
                s2 = skv[:, :, 1, :]
                yield
                pend = []
                wts = [None]

                def stage2(eb, Eb):
                    if eb % 8 == 0:
                        wts[0] = WTSr.next()
                    WTS = wts[0]
                    pt, pto = pstr.next()
                    for k in range(2):
                        mm(pt[:, k * 128:(k + 1) * 128], pto,
                           [(Eb.t[:, h, k * 128:(k + 1) * 128], IDB.t[:]) for h in range(8)], reads=[Eb.o, IDB.o])
                    cl = (eb % 8) * 2
                    S.op("dve", "tensor_copy", dict(out=WTS.t[:, cl:cl + 2, :].rearrange("p a b -> p (a b)"), in_=pt[:]),
                         reads=[pto], writes=[WTS.o], nosame=True)
                    if eb % 8 == 7:
                        c16 = ((eb // 8) % 2) * 16
                        S.dma("sp", WTp[nb, eb // 16, :, c16:c16 + 16, tsub * 128:(tsub + 1) * 128], WTS.t[:], WTS, reads=[WTS.o])

                def stage0(eb):
                    i0 = eb * 2
                    Xb, Eb = Xr.next(), Er.next()
                    S.op("dve", "tensor_tensor",
                         dict(out=Xb.t[:], in0=S1B.t[:, :, i0:i0 + 2].unsqueeze(3).broadcast_to([128, 8, 2, 128]),
                              in1=s2.unsqueeze(2).broadcast_to([128, 8, 2, 128]), op=ALU.add),
                         reads=[S1B.o, SK.o], writes=[Xb.o])
                    S.op("act", "activation", dict(out=Eb.t[:].rearrange("p h e -> p (h e)"),
                                                   in_=Xb.t[:].rearrange("p h i j -> p (h i j)"), func=AF.Exp),
                         reads=[Xb.o], writes=[Eb.o])
                    return Xb, Eb

                nxt = stage0(0)
                for eb in range(64):
                    Xb, Eb = nxt
                    if eb + 1 < 64:
                        nxt = stage0(eb + 1)
                    xv = Xb.t[:].rearrange("p h i j -> p h (i j)")
                    for h in range(8):
                        S.op("dve", "scalar_tensor_tensor",
                             dict(out=Eb.t[:, h, :], in0=xv[:, h, :], scalar=TAUP.t[:, h:h + 1], in1=Eb.t[:, h, :],
                                  op0=ALU.is_ge, op1=ALU.mult), reads=[Xb.o, TAUP.o, Eb.o], writes=[Eb.o], nosame=(h > 0))
                    pend.append((eb, Eb))
                    if len(pend) > LAG:
                        stage2(*pend.pop(0))
                    if eb % 2 == 1:
                        yield
                while pend:
                    stage2(*pend.pop(0))
                yield

        def gen_d():
            tiles = [(mb, nb, mi) for mb in range(32) for nb in range(NPO) for mi in range(4)]
            info = {}
            load_w(A[0], uT[0], 32)
            B_ = None
            for j in range(len(tiles) + 4):
                if j < len(tiles):
                    mb, nb, mi = tiles[j]
                    Ab = A[mb % 2]
                    if nb == 0 and mi == 0 and mb + 1 < 32:
                        load_w(A[(mb + 1) % 2], uT[mb + 1], 32)
                    if mi == 0:
                        B_ = Bb.next()
                        S.dma("sp", B_.t[:], HPT[nb], B_, writes=[B_.o])
                    ps, po = PSM.next()
                    mm(ps[:], po, [(Ab.t[:, c, mi * 128:(mi + 1) * 128], B_.t[:, c, :]) for c in range(32)],
                       reads=[Ab.o, B_.o])
                    info[j] = (ps, po, nb, mb * 4 + mi)
                if 0 <= j - 3 < len(tiles):
                    ps, po, nb_, ec = info[j - 3]
                    ga = GA.next()
                    S.op("act", "activation", dict(out=ga.t[:], in_=ps[:], func=AF.Copy), reads=[po], writes=[ga.o])
                    info[j - 3] = (ga, nb_, ec)
                if 0 <= j - 4 < len(tiles):
                    ga, nb_, ec = info.pop(j - 4)
                    S.dma("sp", GAT[nb_, ec // 32, :, ec % 32, :], ga.t[:], ga, reads=[ga.o])
                yield

        gc, gd = gen_c(), gen_d()
        alive = [True, True]
        while alive[0] or alive[1]:
            for i, g in enumerate((gc, gd)):
                for _ in range(1):
                    if alive[i]:
                        try:
                            next(g)
                        except StopIteration:
                            alive[i] = False

    def p9e(st):
        ACC = mkbuf(st, [128, 32, 512], F32, "ACC")
        A = Ring([mkbuf(st, [128, 32, 512], BF16, "A") for _ in range(2)])
        Bg = mkbuf(st, [128, 32, 512], BF16, "Bg")
        Bw = Ring([mkbuf(st, [128, 8, 512], BF16, "Bw") for _ in range(2)])
        BgO = [Obj("bg%d" % q) for q in range(4)]
        PSM = Ring(mkps(st, 8))
        for nb in range(NPO):
            S.dma("sp", ACC.t[:], X2T[nb], ACC, writes=[ACC.o])
            for kp in range(4):
                for q in range(4):
                    c8 = slice(q * 8, (q + 1) * 8)
                    S.dma("sp", Bg.t[:, c8, :], GAT[nb, kp, :, c8, :], BgO[q], writes=[BgO[q]])
                for q in range(4):
                    bw = Bw.next()
                    c8 = slice(q * 8, (q + 1) * 8)
                    S.dma("sp", bw.t[:], WTp[nb, kp, :, c8, :], bw, writes=[bw.o])
                    S.op("act", "activation", dict(out=Bg.t[:, c8, :], in_=Bg.t[:, c8, :], func=AF.Gelu),
                         reads=[BgO[q]], writes=[BgO[q]])
                    S.op("dve", "tensor_tensor", dict(out=Bg.t[:, c8, :], in0=Bg.t[:, c8, :], in1=bw.t[:], op=ALU.mult),
                         reads=[BgO[q], bw.o], writes=[BgO[q]])
                for ap in range(8):
                    Ab = A.next()
                    load_w(Ab, ev[ap, kp], 32)
                    for mi in range(4):
                        m = ap * 4 + mi
                        ps, po = PSM.next()
                        mm(ps[:], po, [(Ab.t[:, c, mi * 128:(mi + 1) * 128], Bg.t[:, c, :]) for c in range(32)],
                           reads=[Ab.o] + BgO)
                        S.op("dve", "tensor_tensor", dict(out=ACC.t[:, m, :], in0=ps[:], in1=ACC.t[:, m, :], op=ALU.add),
                             reads=[po, ACC.o], writes=[ACC.o], nosame=True)
            S.dma("act", X3T[nb], ACC.t[:], ACC, reads=[ACC.o])

    run_phase(norm_phase([xT[j] for j in range(NPA)], g_mix, [HT[j] for j in range(NPA)], BF16))
    run_phase(p2)
    run_phase(p3)
    run_phase(p4)
    run_phase(p5)
    run_phase(p6)
    run_phase(resid_gemm(w_out, 32, MT, xT, X1T))
    run_phase(norm_phase([X1T[j] for j in range(NPO)], g_ca, [HCT[j] for j in range(NPO)], BF16))
    run_phase(norm_phase([memT], g_mem, [MNT[0]], BF16))
    run_phase(proj_fm(w_cq, 2, HCT, NPO, QCT))
    run_phase(proj_fm(w_ckv, 2, MNT, 1, KCT))
    run_phase(p8v)
    run_phase(p8c)
    run_phase(resid_gemm(w_co, 8, OCT, X1T, X2T))
    run_phase(norm_phase([X2T[j] for j in range(NPO)], g_ffn, [HPT[j] for j in range(NPO)], BF16))
    run_phase(proj_fm(w_pq, 4, HPT, NPO, QPT))
    run_phase(p9cd)
    run_phase(p9e)
    run_phase(norm_phase([X3T[j] for j in range(NPO)], g_fin, [yT[j] for j in range(NPO)], F32))
    outer.close()
    return nc, S.nops


def _panels(x2d, kc):
    n = x2d.shape[0] // 512
    return np.ascontiguousarray(x2d.reshape(n, 512, kc, 128).transpose(0, 3, 2, 1))


def _kpanel(m2d):
    k = m2d.shape[0] // 128
    return np.ascontiguousarray(m2d.reshape(k, 128, m2d.shape[1]).transpose(1, 0, 2))


def _wp(W, wc):
    W = np.asarray(W, np.float32)
    K, N = W.shape
    return np.ascontiguousarray(W.reshape(K // 128, 128, N // wc, wc).transpose(2, 1, 0, 3))


def _gain(g):
    return np.ascontiguousarray(np.asarray(g, np.float32).reshape(-1, 128).T)


def _consts(hf):
    own_p = hf * 2048 + np.arange(2048)
    oth_p = (1 - hf) * 2048 + np.arange(2048)
    own_s = hf * 1024 + np.arange(1024)
    oth_s = (1 - hf) * 1024 + np.arange(1024)
    pos_all = np.concatenate([own_p, own_s, oth_p, oth_s])
    d = np.arange(128)
    a, f = d // 64, d % 32
    inv = 10000.0 ** (-np.arange(0, 64, 2, dtype=np.float64) / 64)
    pa = np.where(a[:, None] == 0, pos_all[None, :] // 64, pos_all[None, :] % 64).astype(np.float64)
    ang = pa * inv[f][:, None]
    cosT = np.cos(ang).astype(np.float32)
    sinT = np.sin(ang).astype(np.float32)

    def dft(rows, cols, n, neg_sin):
        prod = (rows[:, None].astype(np.int64) * cols[None, :].astype(np.int64)) % n
        angm = 2.0 * np.pi * prod / n
        c = np.cos(angm) / np.sqrt(n)
        s = np.sin(angm) / np.sqrt(n)
        return c, (-s if neg_sin else s)

    kp_rows = np.concatenate([own_p, oth_p])
    c, s = dft(kp_rows, own_p, 4096, True)
    csp = np.stack([np.stack([_kpanel(mat[:, nb * 512:(nb + 1) * 512]) for nb in range(4)]) for mat in (c, s)]).astype(BF)
    ks_rows = np.concatenate([own_s, oth_s])
    c, s = dft(ks_rows, own_s, 2048, True)
    mat = np.concatenate([c, s], axis=0)
    css = np.stack([_kpanel(mat[:, nb * 512:(nb + 1) * 512]) for nb in range(2)]).astype(BF)
    return dict(cosT=cosT, sinT=sinT, csp=csp, css=css)


_CACHE = {}


def kernel(x_prompt, x_sample, mem_prompt, mem_sample, norm_mix, w_in, q_norm, k_norm, w_attn_br, w_four_br,
           w_gate, b_gate, w_out, norm_ca, mem_norm, w_cq, w_ckv, w_co, norm_ffn, w_pq, sub_keys, expert_u,
           expert_v, final_norm):
    f32 = lambda a: np.ascontiguousarray(np.asarray(a, np.float32))
    x_prompt, x_sample, mem_prompt, mem_sample = map(f32, (x_prompt, x_sample, mem_prompt, mem_sample))
    if "nc" not in _CACHE:
        _CACHE["nc"] = build()[0]
    nc = _CACHE["nc"]
    ch = np.arange(2048)
    prod = (ch[:, None] * ch[None, :]) % 2048
    angc = 2.0 * np.pi * prod / 2048
    cc = np.cos(angc) / np.sqrt(2048.0)
    sc = np.sin(angc) / np.sqrt(2048.0)
    ccs = np.stack([_kpanel(mat[:, cb * 512:(cb + 1) * 512]) for mat in (cc, sc) for cb in range(4)]).astype(BF)
    rm = np.zeros((128, 128), np.float32)
    for dout in range(128):
        if (dout % 64) < 32:
            rm[dout + 32, dout] = -1.0
        else:
            rm[dout - 32, dout] = 1.0
    shared = dict(
        w_in=_wp(w_in[0], 512), w_attn=_wp(w_attn_br[0], 256), w_four=_wp(w_four_br[0], 256), w_gate=_wp(w_gate[0], 256),
        w_out=_wp(w_out[0], 512), w_cq=_wp(w_cq[0], 512), w_ckv=_wp(w_ckv[0], 512), w_co=_wp(w_co[0], 512),
        w_pq=_wp(w_pq[0], 512),
        uT=np.ascontiguousarray(np.asarray(expert_u[0], np.float32).reshape(32, 512, 32, 128).transpose(0, 3, 2, 1)),
        ev=np.ascontiguousarray(np.asarray(expert_v[0], np.float32).reshape(4, 32, 128, 8, 512).transpose(3, 0, 2, 1, 4)),
        g_mix=_gain(norm_mix[0]), g_ca=_gain(norm_ca[0]), g_mem=_gain(mem_norm[0]), g_ffn=_gain(norm_ffn[0]),
        g_fin=_gain(final_norm), qn=f32(np.asarray(q_norm[0]).reshape(128, 1)), kn=f32(np.asarray(k_norm[0]).reshape(128, 1)),
        bg=_gain(b_gate[0]),
        subkT=np.ascontiguousarray(np.asarray(sub_keys[0], np.float32).transpose(2, 0, 1)),
        rmat=rm.astype(BF), ones=np.ones((128, 128), BF), ident=np.eye(128, dtype=np.float32).astype(BF), ccs=ccs,
    )
    pc = [_consts(0), _consts(1)]
    in_maps = []
    for c in range(8):
        b, hf = c // 2, c % 2
        op_ = slice(hf * 2048, (hf + 1) * 2048)
        tp_ = slice((1 - hf) * 2048, (2 - hf) * 2048)
        os_ = slice(hf * 1024, (hf + 1) * 1024)
        ts_ = slice((1 - hf) * 1024, (2 - hf) * 1024)
        xall = np.concatenate([x_prompt[b, op_], x_sample[b, os_], x_prompt[b, tp_], x_sample[b, ts_]], axis=0)
        mem = np.concatenate([mem_prompt[b], mem_sample[b]], axis=0)
        m = dict(shared)
        m.update(pc[hf])
        m["xT"] = _panels(xall, 32)
        m["memT"] = _panels(mem, 32)[0]
        in_maps.append(m)
    res = run_bass_kernel_spmd(nc, in_maps, core_ids=list(range(8)))
    y_prompt = np.empty_like(x_prompt)
    y_sample = np.empty_like(x_sample)
    for c in range(8):
        b, hf = c // 2, c % 2
        yT = np.asarray(res.results[c]["yT"], np.float32)
        y = yT.transpose(0, 3, 2, 1).reshape(TOWN, D)
        y_prompt[b, hf * 2048:(hf + 1) * 2048] = y[0:2048]
        y_sample[b, hf * 1024:(hf + 1) * 1024] = y[2048:3072]
    return (y_prompt, y_sample)
```

```python
import numpy as np
import ml_dtypes
from contextlib import ExitStack
import concourse.bass as bass
import concourse.mybir as mybir
from concourse.bass_utils import run_bass_kernel_spmd

F32 = mybir.dt.float32
BF16 = mybir.dt.bfloat16
AF = mybir.ActivationFunctionType
ALU = mybir.AluOpType
AX = mybir.AxisListType
BF = ml_dtypes.bfloat16

D = 4096
EPS = 1e-6
NPO = 6
NPA = 12
TOWN = 3072
TALL = 6144
ATT_SCALE = 128 ** -0.5
CA_SCALE = 256 ** -0.5


class Obj:
    _n = 0

    def __init__(self, name=""):
        Obj._n += 1
        self.id = Obj._n
        self.name = name
        self.w = {}
        self.r = {}


class Buf:
    def __init__(self, nc, st, name, shape, dtype):
        self.t = st.enter_context(nc.sbuf_tensor(name, list(shape), dtype))
        self.o = Obj(name)


class Ring:
    def __init__(self, items):
        self.items = items
        self.i = 0

    def next(self):
        it = self.items[self.i % len(self.items)]
        self.i += 1
        return it


class Sched:
    ENG = ("pe", "act", "dve", "pool", "sp")
    BLK = {"pe": "tensor", "act": "scalar", "dve": "vector", "pool": "gpsimd", "sp": "sync"}

    def __init__(self, nc, semstack):
        self.nc = nc
        self.semstack = semstack
        self.sems = {}
        self.ops = {e: [] for e in self.ENG}
        self.cnt = {}
        self.known = {e: {} for e in self.ENG}
        self.unsig = {e: False for e in self.ENG}
        self.nops = 0
        self.bufphys = {}
        self.free_phys = []
        self.nphys = 0

    def _collect(self, reads, writes, mykey, dma):
        d = {}
        for o in reads:
            for k, v in o.w.items():
                if v > d.get(k, 0):
                    d[k] = v
        for o in writes:
            for k, v in o.w.items():
                if dma and k == mykey:
                    continue
                if v > d.get(k, 0):
                    d[k] = v
            for k, v in o.r.items():
                if v > d.get(k, 0):
                    d[k] = v
        return d

    def _emit_waits(self, eng, deps, skipkey=None):
        kn = self.known[eng]
        lst = self.ops[eng]
        for k, v in deps.items():
            if k == skipkey:
                continue
            if kn.get(k, 0) >= v:
                continue
            lst.append((0, k, v))
            kn[k] = v

    def _record(self, reads, writes, key, c, merge=False):
        for o in reads:
            if o.r.get(key, 0) < c:
                o.r[key] = c
        for o in writes:
            if merge and key in o.w:
                o.w[key] = c
            else:
                o.w = {key: c}
            o.r = {}

    def op(self, eng, name, kw, reads=(), writes=(), signal=True, nosame=False):
        key = ("e", eng)
        deps = self._collect(reads, writes, key, False)
        self._emit_waits(eng, deps, skipkey=key if (eng == "pe" or nosame) else None)
        c = self.cnt.get(key, 0) + 1
        if signal:
            self.cnt[key] = c
            self.ops[eng].append((1, name, kw, key))
            self.unsig[eng] = False
        else:
            self.ops[eng].append((1, name, kw, None))
            self.unsig[eng] = True
        self._record(reads, writes, key, c)
        self.nops += 1

    def dma(self, q, out_ap, in_ap, semobj, reads=(), writes=()):
        oid = getattr(semobj, "o", semobj).id
        if oid not in self.bufphys:
            if self.free_phys:
                self.bufphys[oid] = self.free_phys.pop()
            else:
                self.bufphys[oid] = self.nphys
                self.nphys += 1
        key = ("b", self.bufphys[oid])
        deps = self._collect(reads, writes, key, True)
        self._emit_waits(q, deps)
        c = self.cnt.get(key, 0) + 16
        self.cnt[key] = c
        self.ops[q].append((2, out_ap, in_ap, key))
        self._record(reads, writes, key, c, merge=True)
        self.nops += 1

    def barrier(self):
        for e in self.ENG:
            assert not self.unsig[e], e
            self._emit_waits(e, dict(self.cnt))

    def flush(self, st):
        nc = self.nc
        for k in self.cnt:
            if k not in self.sems:
                self.sems[k] = self.semstack.enter_context(nc.semaphore("s%d" % len(self.sems)))
        sems = self.sems
        block = st.enter_context(nc.Block())
        for e in self.ENG:
            lst = self.ops[e]
            if not lst:
                continue

            def body(engine, lst=lst):
                for it in lst:
                    if it[0] == 0:
                        engine.wait_ge(sems[it[1]], it[2])
                    elif it[0] == 1:
                        ins = getattr(engine, it[1])(**it[2])
                        if it[3] is not None:
                            ins.then_inc(sems[it[3]], 1)
                    else:
                        engine.dma_start(out=it[1], in_=it[2]).then_inc(sems[it[3]], 16)

            getattr(block, self.BLK[e])(body)
        self.ops = {e: [] for e in self.ENG}
        self.free_phys = list(range(self.nphys))
        self.bufphys = {}


def build(stop_after=99, debug_out=()):
    nc = bass.Bass("TRN2", target_bir_lowering=False)

    def inp(name, shape, dt=F32):
        return nc.dram_tensor(name, list(shape), dt, kind="ExternalInput").ap()

    def scr(name, shape, dt=BF16):
        kind = "ExternalOutput" if name in debug_out else "Internal"
        return nc.dram_tensor(name, list(shape), dt, kind=kind).ap()

    xT = inp("xT", [NPA, 128, 32, 512])
    memT = inp("memT", [128, 32, 512])
    w_in = inp("w_in", [10, 128, 32, 512])
    w_attn = inp("w_attn", [16, 128, 16, 256])
    w_four = inp("w_four", [16, 128, 16, 256])
    w_gate = inp("w_gate", [32, 128, 32, 256])
    w_out = inp("w_out", [8, 128, 32, 512])
    w_cq = inp("w_cq", [2, 128, 32, 512])
    w_ckv = inp("w_ckv", [4, 128, 32, 512])
    w_co = inp("w_co", [8, 128, 8, 512])
    w_pq = inp("w_pq", [4, 128, 32, 512])
    uT = inp("uT", [32, 128, 32, 512])
    ev = inp("ev", [8, 4, 128, 32, 512])
    g_mix = inp("g_mix", [128, 32])
    g_ca = inp("g_ca", [128, 32])
    g_mem = inp("g_mem", [128, 32])
    g_ffn = inp("g_ffn", [128, 32])
    g_fin = inp("g_fin", [128, 32])
    qn = inp("qn", [128, 1])
    kn = inp("kn", [128, 1])
    bg = inp("bg", [128, 64])
    subkT = inp("subkT", [128, 2, 128])
    cosT = inp("cosT", [128, TALL])
    sinT = inp("sinT", [128, TALL])
    rmat = inp("rmat", [128, 128], BF16)
    ones_d = inp("ones", [128, 128], BF16)
    ident = inp("ident", [128, 128], BF16)
    ccs = inp("ccs", [8, 128, 16, 512], BF16)
    csp = inp("csp", [2, 4, 128, 32, 512], BF16)
    css = inp("css", [2, 128, 32, 512], BF16)
    yT = nc.dram_tensor("yT", [NPO, 128, 32, 512], F32, kind="ExternalOutput").ap()

    HT = scr("HT", [NPA, 128, 32, 512])
    QT = scr("QT", [16, 128, TOWN])
    KT = scr("KT", [4, 128, TALL])
    VS = scr("VS", [TALL, 512])
    FT = scr("FT", [NPA, 128, 16, 512])
    PQ = scr("PQ", [4, 2, TALL, 512])
    ZT = scr("ZT", [NPO, 128, 16, 512])
    AT = scr("AT", [NPO, 128, 16, 512])
    MT = scr("MT", [NPO, 128, 32, 512])
    X1T = scr("X1T", [NPO, 128, 32, 512], F32)
    HCT = scr("HCT", [NPO, 128, 32, 512])
    MNT = scr("MNT", [1, 128, 32, 512])
    QCT = scr("QCT", [8, 128, TOWN])
    KCT = scr("KCT", [8, 128, 512])
    VC = scr("VC", [512, 1024])
    OCT = scr("OCT", [NPO, 128, 8, 512])
    X2T = scr("X2T", [NPO, 128, 32, 512], F32)
    HPT = scr("HPT", [NPO, 128, 32, 512])
    QPT = scr("QPT", [16, 128, TOWN])
    WTp = scr("WTp", [NPO, 4, 128, 32, 512])
    GAT = scr("GAT", [NPO, 4, 128, 32, 512])
    X3T = scr("X3T", [NPO, 128, 32, 512], F32)

    outer = ExitStack()
    S = Sched(nc, outer)
    cnt = [0]

    def mkbuf(st, shape, dt, name="b"):
        cnt[0] += 1
        return Buf(nc, st, "%s%d" % (name, cnt[0]), shape, dt)

    def mkps(st, n, dt=F32, cols=512):
        out = []
        for i in range(n):
            cnt[0] += 1
            t = st.enter_context(nc.psum_tensor("ps%d" % cnt[0], [128, cols], dt))
            out.append((t, Obj("ps")))
        return out

    def mm(ps, po, pairs, reads, start=True, stop=True):
        n = len(pairs)
        for i, (l, r) in enumerate(pairs):
            last = i == n - 1
            S.op("pe", "matmul", dict(out=ps, lhsT=l, rhs=r, start=(start and i == 0), stop=(stop and last)),
                 reads=reads, writes=[po], signal=last)

    def load_w(buf, wpanel, kc):
        step = 8 if kc >= 8 else kc
        for c0 in range(0, kc, step):
            S.dma("pool", buf.t[:, c0:c0 + step, :], wpanel[:, c0:c0 + step, :], buf, writes=[buf.o])

    def consts(st, need_ones=True):
        ON = mkbuf(st, [128, 128], BF16, "ones")
        S.dma("sp", ON.t[:], ones_d, ON, writes=[ON.o])
        EP = mkbuf(st, [128, 1], F32, "eps")
        S.op("dve", "memset", dict(ap=EP.t[:], constant=EPS), writes=[EP.o])
        return ON, EP

    phase_idx = [0]

    def run_phase(fn):
        phase_idx[0] += 1
        if phase_idx[0] > stop_after:
            return
        with ExitStack() as st:
            fn(st)
            S.barrier()
            S.flush(st)

    def norm_phase(srcs, gain_ap, dsts, out_dt):
        def fn(st):
            ON, EP = consts(st)
            Xr = Ring([mkbuf(st, [128, 32, 256], F32, "nx") for _ in range(2)])
            SQr = Ring([mkbuf(st, [128, 32, 256], BF16, "nsq") for _ in range(2)])
            Or = Ring([mkbuf(st, [128, 32, 256], out_dt, "no") for _ in range(2)])
            RSr = Ring([mkbuf(st, [128, 256], F32, "nrs") for _ in range(2)])
            G = mkbuf(st, [128, 32], F32, "ng")
            PSr = Ring(mkps(st, 2))
            S.dma("sp", G.t[:], gain_ap, G, writes=[G.o])
            for src, dst in zip(srcs, dsts):
                for hf in range(2):
                    cs = slice(hf * 256, (hf + 1) * 256)
                    X, SQ, O, RS = Xr.next(), SQr.next(), Or.next(), RSr.next()
                    ps, po = PSr.next()
                    S.dma("sp", X.t[:], src[:, :, cs], X, writes=[X.o])
                    for q2 in range(2):
                        S.op("act", "activation", dict(out=SQ.t[:, q2 * 16:(q2 + 1) * 16, :], in_=X.t[:, q2 * 16:(q2 + 1) * 16, :],
                                                       func=AF.Square), reads=[X.o], writes=[SQ.o])
                    mm(ps[:, 0:256], po, [(ON.t[:], SQ.t[:, c, :]) for c in range(32)], reads=[ON.o, SQ.o])
                    S.op("act", "activation", dict(out=RS.t[:], in_=ps[:, 0:256], func=AF.Sqrt, bias=EP.t[:, 0:1], scale=1.0 / D),
                         reads=[po, EP.o], writes=[RS.o])
                    S.op("dve", "reciprocal", dict(out=RS.t[:], in_=RS.t[:]), reads=[RS.o], writes=[RS.o])
                    for q4 in range(4):
                        c8 = slice(q4 * 8, (q4 + 1) * 8)
                        S.op("pool", "tensor_tensor",
                             dict(out=X.t[:, c8, :], in0=X.t[:, c8, :],
                                  in1=G.t[:, c8].unsqueeze(2).broadcast_to([128, 8, 256]), op=ALU.mult),
                             reads=[X.o, G.o], writes=[X.o], nosame=(q4 > 0))
                    for q4 in range(4):
                        c8 = slice(q4 * 8, (q4 + 1) * 8)
                        S.op("dve", "tensor_tensor",
                             dict(out=O.t[:, c8, :], in0=X.t[:, c8, :],
                                  in1=RS.t[:].unsqueeze(1).broadcast_to([128, 8, 256]), op=ALU.mult),
                             reads=[X.o, RS.o], writes=[O.o], nosame=(q4 > 0))
                    S.dma("act", dst[:, :, cs], O.t[:], O, reads=[O.o])
        return fn

    def p2(st):
        ON, EP = consts(st)
        A = [mkbuf(st, [128, 32, 512], BF16, "A") for _ in range(2)]
        HB = Ring([mkbuf(st, [128, 32, 512], BF16, "HB") for _ in range(2)])
        CS = Ring([mkbuf(st, [128, 2, 512], F32, "CS") for _ in range(2)])
        RM = mkbuf(st, [128, 128], BF16, "RM")
        QN = mkbuf(st, [128, 1], F32, "QN")
        KN = mkbuf(st, [128, 1], F32, "KN")
        S.dma("sp", RM.t[:], rmat, RM, writes=[RM.o])
        S.dma("sp", QN.t[:], qn, QN, writes=[QN.o])
        S.dma("sp", KN.t[:], kn, KN, writes=[KN.o])
        EV = Ring([mkbuf(st, [128, 512], BF16, "EV") for _ in range(3)])
        XQ = Ring([mkbuf(st, [128, 512], F32, "XQ") for _ in range(4)])
        SQ = Ring([mkbuf(st, [128, 512], BF16, "SQ") for _ in range(4)])
        RS = Ring([mkbuf(st, [128, 512], F32, "RS") for _ in range(2)])
        XN = Ring([mkbuf(st, [128, 512], BF16, "XN") for _ in range(4)])
        T1 = Ring([mkbuf(st, [128, 512], F32, "T1") for _ in range(2)])
        T2 = Ring([mkbuf(st, [128, 512], F32, "T2") for _ in range(2)])
        OB = Ring([mkbuf(st, [128, 512], BF16, "OB") for _ in range(3)])
        pss = mkps(st, 8)
        PSM = Ring(pss[0:4])
        PS2 = Ring(pss[4:6])
        PS3 = Ring(pss[6:8])

        def stage_a(job):
            ps, po = job["ps"], job["po"]
            xq, sq = XQ.next(), SQ.next()
            S.op("act", "activation", dict(out=xq.t[:], in_=ps[:], func=AF.Copy), reads=[po], writes=[xq.o])
            S.op("act", "activation", dict(out=sq.t[:], in_=ps[:], func=AF.Square), reads=[po], writes=[sq.o])
            job["xq"], job["sq"] = xq, sq

        def stage_b(job):
            xq, sq, gain = job["xq"], job["sq"], job["gain"]
            ps2, po2 = PS2.next()
            mm(ps2[:], po2, [(ON.t[:], sq.t[:])], reads=[ON.o, sq.o])
            rs, xn = RS.next(), XN.next()
            S.op("act", "activation", dict(out=rs.t[:], in_=ps2[:], func=AF.Sqrt, bias=EP.t[:, 0:1], scale=1.0 / 128),
                 reads=[po2, EP.o], writes=[rs.o])
            S.op("dve", "reciprocal", dict(out=rs.t[:], in_=rs.t[:]), reads=[rs.o], writes=[rs.o])
            S.op("dve", "scalar_tensor_tensor", dict(out=xn.t[:], in0=xq.t[:], scalar=gain.t[:, 0:1], in1=rs.t[:],
                                                     op0=ALU.mult, op1=ALU.mult),
                 reads=[xq.o, gain.o, rs.o], writes=[xn.o])
            job["xn"] = xn

        def stage_c(job):
            xn, CSb, dst = job["xn"], job["cs"], job["dst"]
            ps3, po3 = PS3.next()
            mm(ps3[:], po3, [(RM.t[:], xn.t[:])], reads=[RM.o, xn.o])
            t1, t2, ob = T1.next(), T2.next(), OB.next()
            S.op("dve", "tensor_tensor", dict(out=t1.t[:], in0=xn.t[:], in1=CSb.t[:, 0, :], op=ALU.mult),
                 reads=[xn.o, CSb.o], writes=[t1.o])
            S.op("dve", "tensor_tensor", dict(out=t2.t[:], in0=ps3[:], in1=CSb.t[:, 1, :], op=ALU.mult),
                 reads=[po3, CSb.o], writes=[t2.o])
            S.op("pool", "tensor_tensor", dict(out=ob.t[:], in0=t1.t[:], in1=t2.t[:], op=ALU.add),
                 reads=[t1.o, t2.o], writes=[ob.o])
            S.dma("pool", dst, ob.t[:], ob, reads=[ob.o])

        pend = []

        def advance(newjob):
            for job in list(pend):
                job["age"] += 1
                if job["age"] == 1:
                    stage_b(job)
                elif job["age"] == 2:
                    stage_c(job)
                    pend.remove(job)
            if newjob is not None:
                stage_a(newjob)
                newjob["age"] = 0
                pend.append(newjob)

        load_w(A[0], w_in[0], 32)
        for ap in range(10):
            kind = "q" if ap < 4 else "k" if ap == 4 else "v" if ap == 5 else "f"
            Ab = A[ap % 2]
            if ap + 1 < 10:
                load_w(A[(ap + 1) % 2], w_in[ap + 1], 32)
            nbs = range(NPO) if kind == "q" else range(NPA)
            for nb in nbs:
                Hb = HB.next()
                S.dma("sp", Hb.t[:], HT[nb], Hb, writes=[Hb.o])
                if kind in "qk":
                    CSb = CS.next()
                    S.dma("sp", CSb.t[:, 0, :], cosT[:, nb * 512:(nb + 1) * 512], CSb, writes=[CSb.o])
                    S.dma("sp", CSb.t[:, 1, :], sinT[:, nb * 512:(nb + 1) * 512], CSb, writes=[CSb.o])
                for m in range(4):
                    ps, po = PSM.next()
                    if kind == "v":
                        mm(ps[:], po, [(Hb.t[:, c, m * 128:(m + 1) * 128], Ab.t[:, c, :]) for c in range(32)],
                           reads=[Hb.o, Ab.o])
                        advance(None)
                        Eb = EV.next()
                        S.op("act", "activation", dict(out=Eb.t[:], in_=ps[:], func=AF.Copy), reads=[po], writes=[Eb.o])
                        S.dma("pool", VS[nb * 512 + m * 128:nb * 512 + (m + 1) * 128, :], Eb.t[:], Eb, reads=[Eb.o])
                        continue
                    mm(ps[:], po, [(Ab.t[:, c, m * 128:(m + 1) * 128], Hb.t[:, c, :]) for c in range(32)],
                       reads=[Hb.o, Ab.o])
                    if kind == "f":
                        advance(None)
                        Eb = EV.next()
                        S.op("act", "activation", dict(out=Eb.t[:], in_=ps[:], func=AF.Copy), reads=[po], writes=[Eb.o])
                        S.dma("pool", FT[nb, :, (ap - 6) * 4 + m, :], Eb.t[:], Eb, reads=[Eb.o])
                        continue
                    gain = QN if kind == "q" else KN
                    dst = QT[ap * 4 + m, :, nb * 512:(nb + 1) * 512] if kind == "q" else KT[m, :, nb * 512:(nb + 1) * 512]
                    advance(dict(ps=ps, po=po, gain=gain, dst=dst, cs=CSb))
        while pend:
            advance(None)

    def p3(st):
        Bp = [mkbuf(st, [128, 16, 512], BF16, "Bp") for _ in range(2)]
        Fb = Ring([mkbuf(st, [128, 16, 512], BF16, "Fb") for _ in range(2)])
        EV = Ring([mkbuf(st, [128, 512], BF16, "EV") for _ in range(4)])
        PSM = Ring(mkps(st, 4))
        k = 0
        for pi in range(8):
            pq, cb = pi // 4, pi % 4
            B_ = Bp[pi % 2]
            S.dma("sp", B_.t[:], ccs[pi], B_, writes=[B_.o])
            for nb in range(NPA):
                F_ = Fb.next()
                S.dma("sp", F_.t[:], FT[nb], F_, writes=[F_.o])
                for m in range(4):
                    ps, po = PSM.next()
                    mm(ps[:], po, [(F_.t[:, c, m * 128:(m + 1) * 128], B_.t[:, c, :]) for c in range(16)],
                       reads=[F_.o, B_.o])
                    Eb = EV.next()
                    k += 1
                    if k % 2:
                        S.op("act", "activation", dict(out=Eb.t[:], in_=ps[:], func=AF.Copy), reads=[po], writes=[Eb.o])
                    else:
                        S.op("dve", "tensor_copy", dict(out=Eb.t[:], in_=ps[:]), reads=[po], writes=[Eb.o])
                    r0 = nb * 512 + m * 128
                    S.dma("pool", PQ[cb, pq, r0:r0 + 128, :], Eb.t[:], Eb, reads=[Eb.o])

    def p4(st):
        Aa = [mkbuf(st, [128, 32, 512], BF16, "Aa") for _ in range(2)]
        Bb = Ring([mkbuf(st, [128, 32, 512], BF16, "Bb") for _ in range(3)])
        EV = Ring([mkbuf(st, [128, 512], BF16, "EV") for _ in range(4)])
        pss = mkps(st, 8)
        GR = Ring([pss[0:4], pss[4:8]])
        k = 0

        def rows(cb, pq, r0, n):
            return PQ[cb, pq, r0:r0 + n, :].rearrange("(c p) n -> p c n", p=128)

        def epi(grp, nbp, cb):
            nonlocal k
            for m in range(4):
                ps, po = grp[m]
                Eb = EV.next()
                k += 1
                if k % 2:
                    S.op("act", "activation", dict(out=Eb.t[:], in_=ps[:], func=AF.Copy), reads=[po], writes=[Eb.o])
                else:
                    S.op("dve", "tensor_copy", dict(out=Eb.t[:], in_=ps[:]), reads=[po], writes=[Eb.o])
                S.dma("pool", ZT[nbp, :, cb * 4 + m, :], Eb.t[:], Eb, reads=[Eb.o])

        for cb in range(4):
            for kp in range(2):
                S.dma("sp", Aa[kp].t[:, 0:16, :], rows(cb, kp, 0, 2048), Aa[kp], writes=[Aa[kp].o])
                S.dma("sp", Aa[kp].t[:, 16:32, :], rows(cb, kp, 3072, 2048), Aa[kp], writes=[Aa[kp].o])
            for nb in range(4):
                grp = GR.next()
                for kp in range(2):
                    Bk = Bb.next()
                    S.dma("sp", Bk.t[:], csp[kp, nb], Bk, writes=[Bk.o])
                    for m in range(4):
                        ps, po = grp[m]
                        mm(ps[:], po, [(Aa[kp].t[:, c, m * 128:(m + 1) * 128], Bk.t[:, c, :]) for c in range(32)],
                           reads=[Aa[kp].o, Bk.o], start=(kp == 0), stop=(kp == 1))
                epi(grp, nb, cb)
        for cb in range(4):
            A0 = Aa[cb % 2]
            S.dma("sp", A0.t[:, 0:8, :], rows(cb, 0, 2048, 1024), A0, writes=[A0.o])
            S.dma("sp", A0.t[:, 8:16, :], rows(cb, 0, 5120, 1024), A0, writes=[A0.o])
            S.dma("sp", A0.t[:, 16:24, :], rows(cb, 1, 2048, 1024), A0, writes=[A0.o])
            S.dma("sp", A0.t[:, 24:32, :], rows(cb, 1, 5120, 1024), A0, writes=[A0.o])
            for nb in range(2):
                grp = GR.next()
                Bk = Bb.next()
                S.dma("sp", Bk.t[:], css[nb], Bk, writes=[Bk.o])
                for m in range(4):
                    ps, po = grp[m]
                    mm(ps[:], po, [(A0.t[:, c, m * 128:(m + 1) * 128], Bk.t[:, c, :]) for c in range(32)],
                       reads=[A0.o, Bk.o])
                epi(grp, 4 + nb, cb)

    def p5(st):
        ON, EP = consts(st)
        KTg = mkbuf(st, [128, 4096], BF16, "KTg")
        Vg = mkbuf(st, [128, 32, 128], BF16, "Vg")
        QG = mkbuf(st, [128, 4, 2048], BF16, "QG")
        ER = Ring([mkbuf(st, [128, 512], BF16, "E") for _ in range(3)])
        RZ = Ring([mkbuf(st, [128, 512], F32, "RZ") for _ in range(2)])
        OB = Ring([mkbuf(st, [128, 4, 128], BF16, "OB") for _ in range(2)])
        pss = mkps(st, 6)
        PSS = Ring(pss[0:2])
        PSO = Ring(pss[2:4])
        PSZ = Ring(pss[4:6])
        seqs = [((0, 2048), (3072, 5120), 0, 2048, 0), ((2048, 3072), (5120, 6144), 2048, 1024, 4)]
        for (o0, o1), (t0, t1), q0, nq, pan0 in seqs:
            hk = o1 - o0
            nch = 2 * hk // 128
            for g in range(4):
                S.dma("sp", KTg.t[:, 0:hk], KT[g, :, o0:o1], KTg, writes=[KTg.o])
                S.dma("sp", KTg.t[:, hk:2 * hk], KT[g, :, t0:t1], KTg, writes=[KTg.o])
                S.dma("sp", Vg.t[:, 0:nch // 2, :], VS[o0:o1, g * 128:(g + 1) * 128].rearrange("(c p) d -> p c d", p=128),
                      Vg, writes=[Vg.o])
                S.dma("sp", Vg.t[:, nch // 2:nch, :], VS[t0:t1, g * 128:(g + 1) * 128].rearrange("(c p) d -> p c d", p=128),
                      Vg, writes=[Vg.o])
                S.dma("sp", QG.t[:, :, 0:nq], QT[g * 4:(g + 1) * 4, :, q0:q0 + nq].rearrange("h d q -> d h q"),
                      QG, writes=[QG.o])
                for qb in range(nq // 128):
                    rhsq = QG.t[:, :, qb * 128:(qb + 1) * 128]
                    pso, poo = PSO.next()
                    psz, poz = PSZ.next()

                    def issue_s(sc):
                        ps, po = PSS.next()
                        mm(ps[:], po, [(KTg.t[:, sc * 128:(sc + 1) * 128], rhsq)], reads=[KTg.o, QG.o])
                        Eb = ER.next()
                        S.op("act", "activation", dict(out=Eb.t[:], in_=ps[:], func=AF.Exp, scale=ATT_SCALE),
                             reads=[po], writes=[Eb.o])
                        return Eb

                    Ecur = issue_s(0)
                    for sc in range(nch):
                        Enext = issue_s(sc + 1) if sc + 1 < nch else None
                        mm(pso[:], poo, [(Vg.t[:, sc, :], Ecur.t[:])], reads=[Vg.o, Ecur.o],
                           start=(sc == 0), stop=(sc == nch - 1))
                        mm(psz[:], poz, [(ON.t[:], Ecur.t[:])], reads=[ON.o, Ecur.o],
                           start=(sc == 0), stop=(sc == nch - 1))
                        Ecur = Enext
                    rz, ob = RZ.next(), OB.next()
                    S.op("dve", "reciprocal", dict(out=rz.t[:], in_=psz[:]), reads=[poz], writes=[rz.o])
                    S.op("dve", "tensor_tensor", dict(out=ob.t[:].rearrange("p h q -> p (h q)"), in0=pso[:], in1=rz.t[:],
                                                      op=ALU.mult), reads=[poo, rz.o], writes=[ob.o])
                    tq = qb * 128
                    S.dma("pool", AT[pan0 + tq // 512, :, g * 4:(g + 1) * 4, tq % 512:tq % 512 + 128], ob.t[:], ob,
                          reads=[ob.o])

    def p6(st):
        Wa = mkbuf(st, [128, 16, 256], BF16, "Wa")
        Wf = mkbuf(st, [128, 16, 256], BF16, "Wf")
        Wg0 = mkbuf(st, [128, 32, 256], BF16, "Wg0")
        Wg1 = mkbuf(st, [128, 32, 256], BF16, "Wg1")
        ATb = Ring([mkbuf(st, [128, 16, 512], BF16, "ATb") for _ in range(2)])
        ZTb = Ring([mkbuf(st, [128, 16, 512], BF16, "ZTb") for _ in range(2)])
        HBb = Ring([mkbuf(st, [128, 32, 512], BF16, "HBb") for _ in range(2)])
        BG = mkbuf(st, [128, 64], F32, "BG")
        S.dma("sp", BG.t[:], bg, BG, writes=[BG.o])
        S0 = Ring([mkbuf(st, [128, 512], F32, "S0") for _ in range(2)])
        S1 = Ring([mkbuf(st, [128, 512], F32, "S1") for _ in range(2)])
        MO = Ring([mkbuf(st, [128, 512], BF16, "MO") for _ in range(2)])
        pss = mkps(st, 8)
        GR = Ring([pss[0:4], pss[4:8]])
        for mb2 in range(16):
            load_w(Wa, w_attn[mb2], 16)
            load_w(Wf, w_four[mb2], 16)
            load_w(Wg0, w_gate[mb2], 32)
            load_w(Wg1, w_gate[16 + mb2], 32)
            for nb in range(NPO):
                a_, z_, h_ = ATb.next(), ZTb.next(), HBb.next()
                S.dma("sp", a_.t[:], AT[nb], a_, writes=[a_.o])
                S.dma("sp", z_.t[:], ZT[nb], z_, writes=[z_.o])
                S.dma("sp", h_.t[:], HT[nb], h_, writes=[h_.o])
                for mi in range(2):
                    m = mb2 * 2 + mi
                    (pa, oa), (pf, of), (pg0, og0), (pg1, og1) = GR.next()
                    cs = slice(mi * 128, (mi + 1) * 128)
                    mm(pa[:], oa, [(Wa.t[:, c, cs], a_.t[:, c, :]) for c in range(16)], reads=[Wa.o, a_.o])
                    mm(pf[:], of, [(Wf.t[:, c, cs], z_.t[:, c, :]) for c in range(16)], reads=[Wf.o, z_.o])
                    mm(pg0[:], og0, [(Wg0.t[:, c, cs], h_.t[:, c, :]) for c in range(32)], reads=[Wg0.o, h_.o])
                    mm(pg1[:], og1, [(Wg1.t[:, c, cs], h_.t[:, c, :]) for c in range(32)], reads=[Wg1.o, h_.o])
                    s0, s1, mo = S0.next(), S1.next(), MO.next()
                    t0, t1 = s0, s1
                    S.op("act", "activation", dict(out=s0.t[:], in_=pg0[:], func=AF.Sigmoid, bias=BG.t[:, m:m + 1], scale=1.0),
                         reads=[og0, BG.o], writes=[s0.o])
                    S.op("act", "activation", dict(out=s1.t[:], in_=pg1[:], func=AF.Sigmoid, bias=BG.t[:, 32 + m:33 + m], scale=1.0),
                         reads=[og1, BG.o], writes=[s1.o])
                    S.op("dve", "tensor_tensor", dict(out=t0.t[:], in0=pa[:], in1=s0.t[:], op=ALU.mult),
                         reads=[oa, s0.o], writes=[t0.o])
                    S.op("dve", "tensor_tensor", dict(out=t1.t[:], in0=pf[:], in1=s1.t[:], op=ALU.mult),
                         reads=[of, s1.o], writes=[t1.o])
                    S.op("pool", "tensor_tensor", dict(out=mo.t[:], in0=t0.t[:], in1=t1.t[:], op=ALU.add),
                         reads=[t0.o, t1.o], writes=[mo.o])
                    S.dma("pool", MT[nb, :, m, :], mo.t[:], mo, reads=[mo.o])

    def resid_gemm(W, kcw, Bsrc, resid, dst):
        def fn(st):
            A = [mkbuf(st, [128, kcw, 512], BF16, "A") for _ in range(2)]
            Bb = Ring([mkbuf(st, [128, kcw, 512], BF16, "B") for _ in range(2)])
            XR = Ring([mkbuf(st, [128, 512], F32, "XR") for _ in range(3)])
            XO = Ring([mkbuf(st, [128, 512], F32, "XO") for _ in range(3)])
            PSM = Ring(mkps(st, 4))
            load_w(A[0], W[0], kcw)
            for ap in range(8):
                Ab = A[ap % 2]
                if ap + 1 < 8:
                    load_w(A[(ap + 1) % 2], W[ap + 1], kcw)
                for nb in range(NPO):
                    B_ = Bb.next()
                    S.dma("sp", B_.t[:], Bsrc[nb], B_, writes=[B_.o])
                    for mi in range(4):
                        m = ap * 4 + mi
                        ps, po = PSM.next()
                        mm(ps[:], po, [(Ab.t[:, c, mi * 128:(mi + 1) * 128], B_.t[:, c, :]) for c in range(kcw)],
                           reads=[Ab.o, B_.o])
                        xr, xo = XR.next(), XO.next()
                        S.dma("sp", xr.t[:], resid[nb, :, m, :], xr, writes=[xr.o])
                        S.op("dve", "tensor_tensor", dict(out=xo.t[:], in0=ps[:], in1=xr.t[:], op=ALU.add),
                             reads=[po, xr.o], writes=[xo.o])
                        S.dma("pool", dst[nb, :, m, :], xo.t[:], xo, reads=[xo.o])
        return fn

    def proj_fm(W, naps, Bsrc, nbs, dst, woff=0):
        def fn(st):
            A = [mkbuf(st, [128, 32, 512], BF16, "A") for _ in range(2)]
            Bb = Ring([mkbuf(st, [128, 32, 512], BF16, "B") for _ in range(2)])
            EV = Ring([mkbuf(st, [128, 512], BF16, "EV") for _ in range(4)])
            PSM = Ring(mkps(st, 4))
            k = 0
            load_w(A[0], W[woff], 32)
            for ap in range(naps):
                Ab = A[ap % 2]
                if ap + 1 < naps:
                    load_w(A[(ap + 1) % 2], W[woff + ap + 1], 32)
                for nb in range(nbs):
                    B_ = Bb.next()
                    S.dma("sp", B_.t[:], Bsrc[nb], B_, writes=[B_.o])
                    for mi in range(4):
                        ps, po = PSM.next()
                        mm(ps[:], po, [(Ab.t[:, c, mi * 128:(mi + 1) * 128], B_.t[:, c, :]) for c in range(32)],
                           reads=[Ab.o, B_.o])
                        Eb = EV.next()
                        k += 1
                        if k % 2:
                            S.op("act", "activation", dict(out=Eb.t[:], in_=ps[:], func=AF.Copy), reads=[po], writes=[Eb.o])
                        else:
                            S.op("dve", "tensor_copy", dict(out=Eb.t[:], in_=ps[:]), reads=[po], writes=[Eb.o])
                        S.dma("pool", dst[ap * 4 + mi, :, nb * 512:(nb + 1) * 512], Eb.t[:], Eb, reads=[Eb.o])
        return fn

    def p8v(st):
        A = [mkbuf(st, [128, 32, 512], BF16, "A") for _ in range(2)]
        B_ = mkbuf(st, [128, 32, 512], BF16, "B")
        EV = Ring([mkbuf(st, [128, 512], BF16, "EV") for _ in range(4)])
        PSM = Ring(mkps(st, 4))
        S.dma("sp", B_.t[:], MNT[0], B_, writes=[B_.o])
        for ap in range(2):
            load_w(A[ap], w_ckv[2 + ap], 32)
        for ap in range(2):
            for mi in range(4):
                ps, po = PSM.next()
                mm(ps[:], po, [(B_.t[:, c, mi * 128:(mi + 1) * 128], A[ap].t[:, c, :]) for c in range(32)],
                   reads=[A[ap].o, B_.o])
                Eb = EV.next()
                S.op("act", "activation", dict(out=Eb.t[:], in_=ps[:], func=AF.Copy), reads=[po], writes=[Eb.o])
                S.dma("pool", VC[mi * 128:(mi + 1) * 128, ap * 512:(ap + 1) * 512], Eb.t[:], Eb, reads=[Eb.o])

    def p8c(st):
        ON, EP = consts(st)
        KCb = mkbuf(st, [128, 8, 512], BF16, "KCb")
        QCb = mkbuf(st, [128, 8, TOWN], BF16, "QCb")
        VCb = mkbuf(st, [128, 4, 1024], BF16, "VCb")
        S.dma("sp", KCb.t[:], KCT.rearrange("b d m -> d b m"), KCb, writes=[KCb.o])
        S.dma("sp", QCb.t[:], QCT.rearrange("b d t -> d b t"), QCb, writes=[QCb.o])
        S.dma("sp", VCb.t[:], VC.rearrange("(c p) n -> p c n", p=128), VCb, writes=[VCb.o])
        ER = Ring([mkbuf(st, [128, 512], BF16, "E") for _ in range(4)])
        RZ = Ring([mkbuf(st, [128, 512], F32, "RZ") for _ in range(2)])
        OB = Ring([mkbuf(st, [128, 512], BF16, "OB") for _ in range(4)])
        pss = mkps(st, 8)
        PSS = Ring(pss[0:2])
        PSO = Ring(pss[2:6])
        PSZ = Ring(pss[6:8])
        for nb in range(NPO):
            mc0 = 0 if nb < 4 else 2
            for hc in range(4):
                Es = []
                for mc in range(2):
                    ps, po = PSS.next()
                    ms = slice((mc0 + mc) * 128, (mc0 + mc + 1) * 128)
                    mm(ps[:], po, [(KCb.t[:, hc * 2 + db, ms], QCb.t[:, hc * 2 + db, nb * 512:(nb + 1) * 512]) for db in range(2)],
                       reads=[KCb.o, QCb.o])
                    Eb = ER.next()
                    S.op("act", "activation", dict(out=Eb.t[:], in_=ps[:], func=AF.Exp, scale=CA_SCALE), reads=[po], writes=[Eb.o])
                    Es.append(Eb)
                psz, poz = PSZ.next()
                mm(psz[:], poz, [(ON.t[:], Es[mc].t[:]) for mc in range(2)], reads=[ON.o, Es[0].o, Es[1].o])
                rz = RZ.next()
                S.op("dve", "reciprocal", dict(out=rz.t[:], in_=psz[:]), reads=[poz], writes=[rz.o])
                for dvb in range(2):
                    pso, poo = PSO.next()
                    cs = slice(hc * 256 + dvb * 128, hc * 256 + (dvb + 1) * 128)
                    mm(pso[:], poo, [(VCb.t[:, mc0 + mc, cs], Es[mc].t[:]) for mc in range(2)],
                       reads=[VCb.o, Es[0].o, Es[1].o])
                    ob = OB.next()
                    S.op("dve", "tensor_tensor", dict(out=ob.t[:], in0=pso[:], in1=rz.t[:], op=ALU.mult),
                         reads=[poo, rz.o], writes=[ob.o])
                    S.dma("pool", OCT[nb, :, hc * 2 + dvb, :], ob.t[:], ob, reads=[ob.o])

    def p9cd(st):
        SUBK = mkbuf(st, [128, 2, 128], BF16, "SUBK")
        S.dma("pool", SUBK.t[:], subkT, SUBK, writes=[SUBK.o])
        IDB = mkbuf(st, [128, 128], BF16, "IDB")
        S.dma("sp", IDB.t[:], ident, IDB, writes=[IDB.o])
        Q = mkbuf(st, [128, 16, 128], BF16, "Q16")
        SK = mkbuf(st, [128, 16, 128], F32, "SK")
        TOP = mkbuf(st, [128, 16, 16], F32, "TOP")
        B16 = mkbuf(st, [128, 8, 16], F32, "B16")
        JUNK = mkbuf(st, [128, 16], F32, "JUNK")
        NEGM = mkbuf(st, [128, 8], F32, "NEGM")
        ZS = mkbuf(st, [128, 8], F32, "ZS")
        LNZ = mkbuf(st, [128, 8], F32, "LNZ")
        BIAS = mkbuf(st, [128, 8], F32, "BIAS")
        TAUP = mkbuf(st, [128, 8], F32, "TAUP")
        S1B = mkbuf(st, [128, 8, 128], F32, "S1B")
        Xr = Ring([mkbuf(st, [128, 8, 2, 128], F32, "X") for _ in range(3)])
        Er = Ring([mkbuf(st, [128, 8, 256], BF16, "E") for _ in range(6)])
        WTSr = Ring([mkbuf(st, [128, 16, 128], BF16, "WTS") for _ in range(2)])
        pssk = mkps(st, 2)
        pstr = Ring(mkps(st, 2, F32, 256))
        A = [mkbuf(st, [128, 32, 512], BF16, "A") for _ in range(2)]
        Bb = Ring([mkbuf(st, [128, 32, 512], BF16, "B") for _ in range(2)])
        GA = Ring([mkbuf(st, [128, 512], BF16, "GA") for _ in range(4)])
        PSM = Ring(mkps(st, 4))
        LAG = 3

        def gen_c():
            for tt in range(24):
                nb, tsub = tt // 4, tt % 4
                S.dma("sp", Q.t[:], QPT[:, :, tt * 128:(tt + 1) * 128].rearrange("b d t -> d b t"), Q, writes=[Q.o])
                for half in range(2):
                    for h8 in range(8):
                        hc = half * 8 + h8
                        ps, po = pssk[h8 // 4]
                        mm(ps[:, (h8 % 4) * 128:(h8 % 4 + 1) * 128], po, [(Q.t[:, hc, :], SUBK.t[:, hc % 2, :])],
                           reads=[Q.o, SUBK.o])
                    for bk in range(2):
                        ps, po = pssk[bk]
                        c0 = half * 8 + bk * 4
                        S.op("act", "activation", dict(out=SK.t[:, c0:c0 + 4, :].rearrange("p a b -> p (a b)"), in_=ps[:],
                                                       func=AF.Copy), reads=[po], writes=[SK.o])
                yield
                X0, X1 = Xr.items[0], Xr.items[1]
                SKRv = X0.t[:].rearrange("p h i j -> p (h i) j")
                CNv = X1.t[:].rearrange("p h i j -> p h (i j)")
                for hc in range(16):
                    S.op("dve", "max", dict(out=TOP.t[:, hc, 0:8], in_=SK.t[:, hc, :]), reads=[SK.o], writes=[TOP.o], nosame=True)
                for hc in range(16):
                    S.op("dve", "match_replace", dict(out=SKRv[:, hc, :], in_to_replace=TOP.t[:, hc, 0:8], in_values=SK.t[:, hc, :],
                                                      imm_value=-1e30), reads=[SK.o, TOP.o], writes=[X0.o], nosame=(hc > 0))
                yield
                for hc in range(16):
                    S.op("dve", "max", dict(out=TOP.t[:, hc, 8:16], in_=SKRv[:, hc, :]), reads=[X0.o], writes=[TOP.o], nosame=(hc > 0))
                yield
                for hh in range(2):
                    for h4 in range(4):
                        h = hh * 4 + h4
                        a_ = TOP.t[:, 2 * h, :].unsqueeze(2).broadcast_to([128, 16, 16])
                        b_ = TOP.t[:, 2 * h + 1, :].unsqueeze(1).broadcast_to([128, 16, 16])
                        S.op("dve", "tensor_tensor", dict(out=CNv[:, h4, :].rearrange("p (a b) -> p a b", a=16), in0=a_, in1=b_, op=ALU.add),
                             reads=[TOP.o], writes=[X1.o], nosame=(h4 > 0))
                    for h4 in range(4):
                        h = hh * 4 + h4
                        S.op("dve", "max", dict(out=B16.t[:, h, 0:8], in_=CNv[:, h4, :]), reads=[X1.o], writes=[B16.o], nosame=(h4 > 0))
                    for h4 in range(4):
                        h = hh * 4 + h4
                        S.op("dve", "match_replace", dict(out=CNv[:, 4 + h4, :], in_to_replace=B16.t[:, h, 0:8], in_values=CNv[:, h4, :],
                                                          imm_value=-1e30), reads=[X1.o, B16.o], writes=[X1.o], nosame=(h4 > 0))
                    for h4 in range(4):
                        h = hh * 4 + h4
                        S.op("dve", "max", dict(out=B16.t[:, h, 8:16], in_=CNv[:, 4 + h4, :]), reads=[X1.o], writes=[B16.o], nosame=(h4 > 0))
                    yield
                S.op("dve", "tensor_scalar", dict(out=NEGM.t[:], in0=B16.t[:, :, 0], scalar1=-1.0, scalar2=0.0,
                                                  op0=ALU.mult, op1=ALU.add), reads=[B16.o], writes=[NEGM.o])
                S.op("dve", "memset", dict(ap=ZS.t[:], constant=0.0), writes=[ZS.o])
                for h in range(8):
                    S.op("act", "activation", dict(out=JUNK.t[:], in_=B16.t[:, h, :], func=AF.Exp, bias=NEGM.t[:, h:h + 1],
                                                   scale=1.0, accum_out=ZS.t[:, h:h + 1]),
                         reads=[B16.o, NEGM.o], writes=[JUNK.o, ZS.o], nosame=(h > 0))
                S.op("act", "activation", dict(out=LNZ.t[:], in_=ZS.t[:], func=AF.Ln), reads=[ZS.o], writes=[LNZ.o])
                S.op("dve", "tensor_tensor", dict(out=BIAS.t[:], in0=NEGM.t[:], in1=LNZ.t[:], op=ALU.subtract),
                     reads=[NEGM.o, LNZ.o], writes=[BIAS.o])
                S.op("dve", "tensor_tensor", dict(out=TAUP.t[:], in0=B16.t[:, :, 15], in1=BIAS.t[:], op=ALU.add),
                     reads=[B16.o, BIAS.o], writes=[TAUP.o])
                S.op("dve", "tensor_scalar", dict(out=TAUP.t[:], in0=TAUP.t[:], scalar1=1.0, scalar2=-2e-5,
                                                  op0=ALU.mult, op1=ALU.add), reads=[TAUP.o], writes=[TAUP.o])
                skv = SK.t[:].rearrange("p (h c) n -> p h c n", c=2)
                S.op("dve", "tensor_tensor", dict(out=S1B.t[:], in0=skv[:, :, 0, :],
                                                  in1=BIAS.t[:].unsqueeze(2).broadcast_to([128, 8, 128]), op=ALU.add),
                     reads=[SK.o, BIAS.o], writes=[S1B.o])
                s2 = skv[:, :, 1, :]
                yield
                pend = []
                wts = [None]

                def stage2(eb, Eb):
                    if eb % 8 == 0:
                        wts[0] = WTSr.next()
                    WTS = wts[0]
                    pt, pto = pstr.next()
                    for k in range(2):
                        mm(pt[:, k * 128:(k + 1) * 128], pto,
                           [(Eb.t[:, h, k * 128:(k + 1) * 128], IDB.t[:]) for h in range(8)], reads=[Eb.o, IDB.o])
                    cl = (eb % 8) * 2
                    S.op("dve", "tensor_copy", dict(out=WTS.t[:, cl:cl + 2, :].rearrange("p a b -> p (a b)"), in_=pt[:]),
                         reads=[pto], writes=[WTS.o], nosame=True)
                    if eb % 8 == 7:
                        c16 = ((eb // 8) % 2) * 16
                        S.dma("sp", WTp[nb, eb // 16, :, c16:c16 + 16, tsub * 128:(tsub + 1) * 128], WTS.t[:], WTS, reads=[WTS.o])

                def stage0(eb):
                    i0 = eb * 2
                    Xb, Eb = Xr.next(), Er.next()
                    S.op("dve", "tensor_tensor",
                         dict(out=Xb.t[:], in0=S1B.t[:, :, i0:i0 + 2].unsqueeze(3).broadcast_to([128, 8, 2, 128]),
                              in1=s2.unsqueeze(2).broadcast_to([128, 8, 2, 128]), op=ALU.add),
                         reads=[S1B.o, SK.o], writes=[Xb.o])
                    S.op("act", "activation", dict(out=Eb.t[:].rearrange("p h e -> p (h e)"),
                                                   in_=Xb.t[:].rearrange("p h i j -> p (h i j)"), func=AF.Exp),
                         reads=[Xb.o], writes=[Eb.o])
                    return Xb, Eb

                nxt = stage0(0)
                for eb in range(64):
                    Xb, Eb = nxt
                    if eb + 1 < 64:
                        nxt = stage0(eb + 1)
                    xv = Xb.t[:].rearrange("p h i j -> p h (i j)")
                    for h in range(8):
                        S.op("dve", "scalar_tensor_tensor",
                             dict(out=Eb.t[:, h, :], in0=xv[:, h, :], scalar=TAUP.t[:, h:h + 1], in1=Eb.t[:, h, :],
                                  op0=ALU.is_ge, op1=ALU.mult), reads=[Xb.o, TAUP.o, Eb.o], writes=[Eb.o], nosame=(h > 0))
                    pend.append((eb, Eb))
                    if len(pend) > LAG:
                        stage2(*pend.pop(0))
                    if eb % 2 == 1:
                        yield
                while pend:
                    stage2(*pend.pop(0))
                yield

        def gen_d():
            tiles = [(mb, nb, mi) for mb in range(32) for nb in range(NPO) for mi in range(4)]
            info = {}
            load_w(A[0], uT[0], 32)
            B_ = None
            for j in range(len(tiles) + 4):
                if j < len(tiles):
                    mb, nb, mi = tiles[j]
                    Ab = A[mb % 2]
                    if nb == 0 and mi == 0 and mb + 1 < 32:
                        load_w(A[(mb + 1) % 2], uT[mb + 1], 32)
                    if mi == 0:
                        B_ = Bb.next()
                        S.dma("sp", B_.t[:], HPT[nb], B_, writes=[B_.o])
                    ps, po = PSM.next()
                    mm(ps[:], po, [(Ab.t[:, c, mi * 128:(mi + 1) * 128], B_.t[:, c, :]) for c in range(32)],
                       reads=[Ab.o, B_.o])
                    info[j] = (ps, po, nb, mb * 4 + mi)
                if 0 <= j - 3 < len(tiles):
                    ps, po, nb_, ec = info[j - 3]
                    ga = GA.next()
                    S.op("act", "activation", dict(out=ga.t[:], in_=ps[:], func=AF.Copy), reads=[po], writes=[ga.o])
                    info[j - 3] = (ga, nb_, ec)
                if 0 <= j - 4 < len(tiles):
                    ga, nb_, ec = info.pop(j - 4)
                    S.dma("sp", GAT[nb_, ec // 32, :, ec % 32, :], ga.t[:], ga, reads=[ga.o])
                yield

        gc, gd = gen_c(), gen_d()
        alive = [True, True]
        while alive[0] or alive[1]:
            for i, g in enumerate((gc, gd)):
                for _ in range(1):
                    if alive[i]:
                        try:
                            next(g)
                        except StopIteration:
                            alive[i] = False

    def p9e(st):
        ACC = mkbuf(st, [128, 32, 512], F32, "ACC")
        A = Ring([mkbuf(st, [128, 32, 512], BF16, "A") for _ in range(2)])
        Bgs = [mkbuf(st, [128, 32, 512], BF16, "Bg") for _ in range(2)]
        Bw = Ring([mkbuf(st, [128, 8, 512], BF16, "Bw") for _ in range(1)])
        BgOs = [[Obj("bg%d_%d" % (i, q)) for q in range(4)] for i in range(2)]
        PSM = Ring(mkps(st, 8))
        panels = [(nb, kp) for nb in range(NPO) for kp in range(4)]

        def prep(p):
            nb, kp = panels[p]
            Bg, BgO = Bgs[p % 2], BgOs[p % 2]
            for q in range(4):
                c8 = slice(q * 8, (q + 1) * 8)
                S.dma("sp", Bg.t[:, c8, :], GAT[nb, kp, :, c8, :], BgO[q], writes=[BgO[q]])
            for q in range(4):
                bw = Bw.next()
                c8 = slice(q * 8, (q + 1) * 8)
                S.dma("sp", bw.t[:], WTp[nb, kp, :, c8, :], bw, writes=[bw.o])
                S.op("act", "activation", dict(out=Bg.t[:, c8, :], in_=Bg.t[:, c8, :], func=AF.Gelu),
                     reads=[BgO[q]], writes=[BgO[q]])
                S.op("dve", "tensor_tensor", dict(out=Bg.t[:, c8, :], in0=Bg.t[:, c8, :], in1=bw.t[:], op=ALU.mult),
                     reads=[BgO[q], bw.o], writes=[BgO[q]])

        prep(0)
        for p, (nb, kp) in enumerate(panels):
            if kp == 0:
                S.dma("sp", ACC.t[:], X2T[nb], ACC, writes=[ACC.o])
            if p + 1 < len(panels):
                prep(p + 1)
            Bg, BgO = Bgs[p % 2], BgOs[p % 2]
            for ap in range(8):
                Ab = A.next()
                load_w(Ab, ev[ap, kp], 32)
                for mi in range(4):
                    m = ap * 4 + mi
                    ps, po = PSM.next()
                    mm(ps[:], po, [(Ab.t[:, c, mi * 128:(mi + 1) * 128], Bg.t[:, c, :]) for c in range(32)],
                       reads=[Ab.o] + BgO)
                    S.op("dve", "tensor_tensor", dict(out=ACC.t[:, m, :], in0=ps[:], in1=ACC.t[:, m, :], op=ALU.add),
                         reads=[po, ACC.o], writes=[ACC.o], nosame=True)
            if kp == 3:
                S.dma("act", X3T[nb], ACC.t[:], ACC, reads=[ACC.o])

    run_phase(norm_phase([xT[j] for j in range(NPA)], g_mix, [HT[j] for j in range(NPA)], BF16))
    run_phase(p2)
    run_phase(p3)
    run_phase(p4)
    run_phase(p5)
    run_phase(p6)
    run_phase(resid_gemm(w_out, 32, MT, xT, X1T))
    run_phase(norm_phase([X1T[j] for j in range(NPO)], g_ca, [HCT[j] for j in range(NPO)], BF16))
    run_phase(norm_phase([memT], g_mem, [MNT[0]], BF16))
    run_phase(proj_fm(w_cq, 2, HCT, NPO, QCT))
    run_phase(proj_fm(w_ckv, 2, MNT, 1, KCT))
    run_phase(p8v)
    run_phase(p8c)
    run_phase(resid_gemm(w_co, 8, OCT, X1T, X2T))
    run_phase(norm_phase([X2T[j] for j in range(NPO)], g_ffn, [HPT[j] for j in range(NPO)], BF16))
    run_phase(proj_fm(w_pq, 4, HPT, NPO, QPT))
    run_phase(p9cd)
    run_phase(p9e)
    run_phase(norm_phase([X3T[j] for j in range(NPO)], g_fin, [yT[j] for j in range(NPO)], F32))
    outer.close()
    return nc, S.nops


def _panels(x2d, kc):
    n = x2d.shape[0] // 512
    return np.ascontiguousarray(x2d.reshape(n, 512, kc, 128).transpose(0, 3, 2, 1))


def _kpanel(m2d):
    k = m2d.shape[0] // 128
    return np.ascontiguousarray(m2d.reshape(k, 128, m2d.shape[1]).transpose(1, 0, 2))


def _wp(W, wc):
    W = np.asarray(W, np.float32)
    K, N = W.shape
    return np.ascontiguousarray(W.reshape(K // 128, 128, N // wc, wc).transpose(2, 1, 0, 3))


def _gain(g):
    return np.ascontiguousarray(np.asarray(g, np.float32).reshape(-1, 128).T)


def _consts(hf):
    own_p = hf * 2048 + np.arange(2048)
    oth_p = (1 - hf) * 2048 + np.arange(2048)
    own_s = hf * 1024 + np.arange(1024)
    oth_s = (1 - hf) * 1024 + np.arange(1024)
    pos_all = np.concatenate([own_p, own_s, oth_p, oth_s])
    d = np.arange(128)
    a, f = d // 64, d % 32
    inv = 10000.0 ** (-np.arange(0, 64, 2, dtype=np.float64) / 64)
    pa = np.where(a[:, None] == 0, pos_all[None, :] // 64, pos_all[None, :] % 64).astype(np.float64)
    ang = pa * inv[f][:, None]
    cosT = np.cos(ang).astype(np.float32)
    sinT = np.sin(ang).astype(np.float32)

    def dft(rows, cols, n, neg_sin):
        prod = (rows[:, None].astype(np.int64) * cols[None, :].astype(np.int64)) % n
        angm = 2.0 * np.pi * prod / n
        c = np.cos(angm) / np.sqrt(n)
        s = np.sin(angm) / np.sqrt(n)
        return c, (-s if neg_sin else s)

    kp_rows = np.concatenate([own_p, oth_p])
    c, s = dft(kp_rows, own_p, 4096, True)
    csp = np.stack([np.stack([_kpanel(mat[:, nb * 512:(nb + 1) * 512]) for nb in range(4)]) for mat in (c, s)]).astype(BF)
    ks_rows = np.concatenate([own_s, oth_s])
    c, s = dft(ks_rows, own_s, 2048, True)
    mat = np.concatenate([c, s], axis=0)
    css = np.stack([_kpanel(mat[:, nb * 512:(nb + 1) * 512]) for nb in range(2)]).astype(BF)
    return dict(cosT=cosT, sinT=sinT, csp=csp, css=css)


_CACHE = {}


def kernel(x_prompt, x_sample, mem_prompt, mem_sample, norm_mix, w_in, q_norm, k_norm, w_attn_br, w_four_br,
           w_gate, b_gate, w_out, norm_ca, mem_norm, w_cq, w_ckv, w_co, norm_ffn, w_pq, sub_keys, expert_u,
           expert_v, final_norm):
    f32 = lambda a: np.ascontiguousarray(np.asarray(a, np.float32))
    x_prompt, x_sample, mem_prompt, mem_sample = map(f32, (x_prompt, x_sample, mem_prompt, mem_sample))
    if "nc" not in _CACHE:
        _CACHE["nc"] = build()[0]
    nc = _CACHE["nc"]
    ch = np.arange(2048)
    prod = (ch[:, None] * ch[None, :]) % 2048
    angc = 2.0 * np.pi * prod / 2048
    cc = np.cos(angc) / np.sqrt(2048.0)
    sc = np.sin(angc) / np.sqrt(2048.0)
    ccs = np.stack([_kpanel(mat[:, cb * 512:(cb + 1) * 512]) for mat in (cc, sc) for cb in range(4)]).astype(BF)
    rm = np.zeros((128, 128), np.float32)
    for dout in range(128):
        if (dout % 64) < 32:
            rm[dout + 32, dout] = -1.0
        else:
            rm[dout - 32, dout] = 1.0
    shared = dict(
        w_in=_wp(w_in[0], 512), w_attn=_wp(w_attn_br[0], 256), w_four=_wp(w_four_br[0], 256), w_gate=_wp(w_gate[0], 256),
        w_out=_wp(w_out[0], 512), w_cq=_wp(w_cq[0], 512), w_ckv=_wp(w_ckv[0], 512), w_co=_wp(w_co[0], 512),
        w_pq=_wp(w_pq[0], 512),
        uT=np.ascontiguousarray(np.asarray(expert_u[0], np.float32).reshape(32, 512, 32, 128).transpose(0, 3, 2, 1)),
        ev=np.ascontiguousarray(np.asarray(expert_v[0], np.float32).reshape(4, 32, 128, 8, 512).transpose(3, 0, 2, 1, 4)),
        g_mix=_gain(norm_mix[0]), g_ca=_gain(norm_ca[0]), g_mem=_gain(mem_norm[0]), g_ffn=_gain(norm_ffn[0]),
        g_fin=_gain(final_norm), qn=f32(np.asarray(q_norm[0]).reshape(128, 1)), kn=f32(np.asarray(k_norm[0]).reshape(128, 1)),
        bg=_gain(b_gate[0]),
        subkT=np.ascontiguousarray(np.asarray(sub_keys[0], np.float32).transpose(2, 0, 1)),
        rmat=rm.astype(BF), ones=np.ones((128, 128), BF), ident=np.eye(128, dtype=np.float32).astype(BF), ccs=ccs,
    )
    pc = [_consts(0), _consts(1)]
    in_maps = []
    for c in range(8):
        b, hf = c // 2, c % 2
        op_ = slice(hf * 2048, (hf + 1) * 2048)
        tp_ = slice((1 - hf) * 2048, (2 - hf) * 2048)
        os_ = slice(hf * 1024, (hf + 1) * 1024)
        ts_ = slice((1 - hf) * 1024, (2 - hf) * 1024)
        xall = np.concatenate([x_prompt[b, op_], x_sample[b, os_], x_prompt[b, tp_], x_sample[b, ts_]], axis=0)
        mem = np.concatenate([mem_prompt[b], mem_sample[b]], axis=0)
        m = dict(shared)
        m.update(pc[hf])
        m["xT"] = _panels(xall, 32)
        m["memT"] = _panels(mem, 32)[0]
        in_maps.append(m)
    res = run_bass_kernel_spmd(nc, in_maps, core_ids=list(range(8)))
    y_prompt = np.empty_like(x_prompt)
    y_sample = np.empty_like(x_sample)
    for c in range(8):
        b, hf = c // 2, c % 2
        yT = np.asarray(res.results[c]["yT"], np.float32)
        y = yT.transpose(0, 3, 2, 1).reshape(TOWN, D)
        y_prompt[b, hf * 2048:(hf + 1) * 2048] = y[0:2048]
        y_sample[b, hf * 1024:(hf + 1) * 1024] = y[2048:3072]
    return (y_prompt, y_sample)
```
